# Optimizing a Trainium2 kernel written in Bass

```python
import jax, jax.numpy as jnp
from jax import lax
import numpy as np

D_MODEL = 1024
BATCH = 16
SEQ = 4096
DEPTH = 4

D_MIX = 2 * D_MODEL
D_MLSTM = D_MIX // 2
N_MLSTM_HEADS = 4
DV_HEAD = D_MLSTM // N_MLSTM_HEADS
DQK_HEAD = DV_HEAD // 2
D_QK = N_MLSTM_HEADS * DQK_HEAD
D_CONV = D_MIX - D_MLSTM
N_CONV_GROUPS = 16
CONV_WIDTH = 3
CHUNK = 64
EPS = 1e-6

SPLIT_SIZES = (D_QK, D_QK, D_MLSTM, D_MLSTM, D_MLSTM, N_MLSTM_HEADS, N_MLSTM_HEADS,
               D_CONV, D_CONV, D_CONV, D_CONV)
IN_COLS = 2 * D_QK + 3 * D_MLSTM + 2 * N_MLSTM_HEADS + 4 * D_CONV

kernel_name = "hymba_mlstm_shortconv_hybrid"


def _split_points():
    pts = []
    acc = 0
    for s in SPLIT_SIZES[:-1]:
        acc += s
        pts.append(acc)
    return tuple(pts)


def rms_norm(x, g):
    xf = x.astype(jnp.float32)
    y = xf * lax.rsqrt(jnp.mean(xf * xf, axis=-1, keepdims=True) + EPS)
    return (y * g.astype(jnp.float32)).astype(x.dtype)


def group_rms_norm(x, g, n_groups, out_dtype):
    shp = x.shape
    xf = x.astype(jnp.float32).reshape(shp[:-1] + (n_groups, shp[-1] // n_groups))
    xf = xf * lax.rsqrt(jnp.mean(xf * xf, axis=-1, keepdims=True) + EPS)
    return (xf.reshape(shp) * g.astype(jnp.float32)).astype(out_dtype)


def mlstm_chunkwise(q, k, v, log_i, log_f):
    B_, H_, S_, dk = q.shape
    dv = v.shape[-1]
    nc = S_ // CHUNK

    def to_chunks(t):
        t = t.reshape((B_, H_, nc, CHUNK) + t.shape[3:])
        return jnp.moveaxis(t, 2, 0)

    qc = to_chunks(q.astype(jnp.float32))
    kc = to_chunks(k.astype(jnp.float32))
    vc = to_chunks(v.astype(jnp.float32))
    lic = to_chunks(log_i)
    bc = lax.cumsum(to_chunks(log_f), axis=3)
    causal = jnp.tril(jnp.ones((CHUNK, CHUNK), dtype=bool))

    def step(carry, xs):
        C, n, m = carry
        qx, kx, vx, li, b = xs
        D = b[..., :, None] - b[..., None, :] + li[..., None, :]
        D = jnp.where(causal, D, -jnp.inf)
        inter = b + m[..., None]
        m_comb = jnp.maximum(inter, jnp.max(D, axis=-1))
        Dw = jnp.exp(D - m_comb[..., None])
        inter_w = jnp.exp(inter - m_comb)
        s = jnp.einsum('bhtd,bhsd->bhts', qx, kx) * Dw
        num = (jnp.einsum('bhts,bhsv->bhtv', s, vx)
               + inter_w[..., None] * jnp.einsum('bhtd,bhdv->bhtv', qx, C))
        den = jnp.sum(s, axis=-1) + inter_w * jnp.einsum('bhtd,bhd->bht', qx, n)
        h = num / jnp.maximum(jnp.abs(den), jnp.exp(-m_comb))[..., None]
        bL = b[..., -1]
        a = bL[..., None] - b + li
        m_new = jnp.maximum(bL + m, jnp.max(a, axis=-1))
        decay = jnp.exp(bL + m - m_new)
        w = jnp.exp(a - m_new[..., None])
        kw = kx * w[..., None]
        C_new = decay[..., None, None] * C + jnp.einsum('bhsd,bhsv->bhdv', kw, vx)
        n_new = decay[..., None] * n + jnp.sum(kw, axis=2)
        return (C_new, n_new, m_new), h

    init = (jnp.zeros((B_, H_, dk, dv), jnp.float32),
            jnp.zeros((B_, H_, dk), jnp.float32),
            jnp.zeros((B_, H_), jnp.float32))
    _, hs = lax.scan(step, init, (qc, kc, vc, lic, bc))
    return jnp.moveaxis(hs, 0, 2).reshape(B_, H_, S_, dv)


def causal_depthwise_conv(u, w):
    S_ = u.shape[1]
    up = jnp.pad(u, ((0, 0), (CONV_WIDTH - 1, 0), (0, 0)))
    y = w[0] * up[:, 0:S_]
    for j in range(1, CONV_WIDTH):
        y = y + w[j] * up[:, j:j + S_]
    return y


def hybrid_layer(x, g_pre, g_post, w_in, b_i, b_f, g_head, conv_w, g_conv, w_out):
    B_, S_, _ = x.shape
    h = rms_norm(x, g_pre)
    proj = jnp.einsum('bsd,de->bse', h, w_in)
    q, k, v, o, z_m, i_pre, f_pre, u, gate_b, gate_c, z_c = jnp.split(
        proj, _split_points(), axis=-1)

    def heads(t, dh):
        return t.reshape(B_, S_, N_MLSTM_HEADS, dh).transpose(0, 2, 1, 3)

    qh = heads(q, DQK_HEAD) * (DQK_HEAD ** -0.5)
    kh = heads(k, DQK_HEAD)
    vh = heads(v, DV_HEAD)
    log_i = (i_pre.astype(jnp.float32) + b_i.astype(jnp.float32)).transpose(0, 2, 1)
    log_f = jax.nn.log_sigmoid(
        f_pre.astype(jnp.float32) + b_f.astype(jnp.float32)).transpose(0, 2, 1)
    hm = mlstm_chunkwise(qh, kh, vh, log_i, log_f)
    hm = hm.transpose(0, 2, 1, 3).reshape(B_, S_, D_MLSTM)
    hm = group_rms_norm(hm, g_head, N_MLSTM_HEADS, x.dtype)
    y_m = jax.nn.silu(z_m) * jax.nn.sigmoid(o) * hm

    conv = causal_depthwise_conv(gate_c * u, conv_w)
    y_c = jax.nn.silu(z_c) * group_rms_norm(gate_b * conv, g_conv, N_CONV_GROUPS, x.dtype)

    mix = jnp.concatenate([y_m, y_c], axis=-1)
    out = jnp.einsum('bse,ed->bsd', mix, w_out)
    return x + rms_norm(out, g_post)


def setup_inputs(seed: int = 0) -> dict:
    key = jax.random.key(seed)
    ks = jax.random.split(key, 11)
    x = jax.random.normal(ks[0], (BATCH, SEQ, D_MODEL), jnp.float32)
    norm_pre = 1.0 + 0.02 * jax.random.normal(ks[1], (DEPTH, D_MODEL), jnp.float32)
    norm_post = 1.0 + 0.02 * jax.random.normal(ks[2], (DEPTH, D_MODEL), jnp.float32)
    w_in = jax.random.normal(ks[3], (DEPTH, D_MODEL, IN_COLS), jnp.float32) * (D_MODEL ** -0.5)
    b_igate = 0.1 * jax.random.normal(ks[4], (DEPTH, N_MLSTM_HEADS), jnp.float32)
    b_fgate = (jnp.linspace(3.0, 6.0, N_MLSTM_HEADS, dtype=jnp.float32)[None, :]
               + 0.1 * jax.random.normal(ks[5], (DEPTH, N_MLSTM_HEADS), jnp.float32))
    head_norm = 1.0 + 0.02 * jax.random.normal(ks[6], (DEPTH, D_MLSTM), jnp.float32)
    conv_w = jax.random.normal(ks[7], (DEPTH, CONV_WIDTH, D_CONV), jnp.float32) * (CONV_WIDTH ** -0.5)
    conv_norm = 1.0 + 0.02 * jax.random.normal(ks[8], (DEPTH, D_CONV), jnp.float32)
    w_out = jax.random.normal(ks[9], (DEPTH, D_MIX, D_MODEL), jnp.float32) * (D_MIX ** -0.5)
    return {"x": x, "norm_pre": norm_pre, "norm_post": norm_post, "w_in": w_in,
            "b_igate": b_igate, "b_fgate": b_fgate, "head_norm": head_norm,
            "conv_w": conv_w, "conv_norm": conv_norm, "w_out": w_out}


def reference(x, norm_pre, norm_post, w_in, b_igate, b_fgate, head_norm, conv_w, conv_norm, w_out):
    for l in range(DEPTH):
        x = hybrid_layer(x, norm_pre[l], norm_post[l], w_in[l], b_igate[l], b_fgate[l],
                         head_norm[l], conv_w[l], conv_norm[l], w_out[l])
    return x
```

```python
import numpy as np
import concourse.bass as bass
import concourse.mybir as mybir
from concourse.bass_utils import run_bass_kernel_spmd

F32 = mybir.dt.float32
BF16 = mybir.dt.bfloat16
AF = mybir.ActivationFunctionType
ALU = mybir.AluOpType
AX = mybir.AxisListType

EPS = 1e-6
NT = 512
NB = 4
NSLAB = 56
NCORES = 8
S_EPOCH = 30000


class Buf:
    __slots__ = ("name", "writers", "readers", "dsem", "dcount", "excl")

    def __init__(self, name):
        self.name = name
        self.excl = False
        self.writers = []
        self.readers = []
        self.dsem = None
        self.dcount = 0


class Eng:
    def __init__(self, S, name, h):
        self.S = S
        self.name = name
        self.h = h
        self.sems = []
        self.count = 0
        self.seen = {}

    def cur_sem(self):
        if not self.sems or self.count >= S_EPOCH:
            self.sems.append(self.S.nc.alloc_semaphore("%s_e%d" % (self.name, len(self.sems))))
            self.count = 0
        return self.sems[-1]


class Sched:
    def __init__(self, nc):
        self.nc = nc
        self.pe = Eng(self, "pe", nc.tensor)
        self.act = Eng(self, "act", nc.scalar)
        self.dve = Eng(self, "dve", nc.vector)
        self.pool = Eng(self, "pool", nc.gpsimd)
        self.sp = Eng(self, "sp", nc.sync)
        self.nbuf = 0
        self.n_ins = 0
        self.n_wait = 0

    def buf(self, name=None):
        self.nbuf += 1
        return Buf(name or "b%d" % self.nbuf)

    def _wait(self, eng, tok):
        sem, val, src = tok
        key = id(sem)
        if eng.seen.get(key, 0) >= val:
            return
        eng.h.wait_ge(sem, val)
        eng.seen[key] = val
        self.n_wait += 1

    def _deps(self, reads, writes):
        deps = []
        for b in reads:
            deps.extend(b.writers)
            if b.excl:
                deps.extend(b.readers)
        for b in writes:
            deps.extend(b.writers)
            deps.extend(b.readers)
        return deps

    def _commit(self, tok, reads, writes):
        for b in reads:
            b.readers.append(tok)
        for b in writes:
            b.writers = [tok]
            b.readers = []

    def op(self, eng, fn, reads=(), writes=()):
        for tok in self._deps(reads, writes):
            if tok[2] is eng and eng is self.pe:
                continue
            self._wait(eng, tok)
        sem = eng.cur_sem()
        ins = fn()
        ins.then_inc(sem, 1)
        eng.count += 1
        tok = (sem, eng.count, eng)
        self.n_ins += 1
        self._commit(tok, reads, writes)
        return tok

    def dma(self, eng, out, in_, sb, reads=(), writes=(), **kw):
        for tok in self._deps(reads, writes):
            self._wait(eng, tok)
        if sb.dsem is None:
            sb.dsem = self.nc.alloc_semaphore("d_%s" % sb.name)
        ins = eng.h.dma_start(out=out, in_=in_, **kw)
        ins.then_inc(sb.dsem, 16)
        sb.dcount += 16
        tok = (sb.dsem, sb.dcount, None)
        self.n_ins += 1
        self._commit(tok, reads, writes)
        return tok

    def wait_all(self, eng, bufs):
        for b in bufs:
            for tok in b.writers + b.readers:
                self._wait(eng, tok)


PV_GPRE, PV_GPOST, PV_GHEAD, PV_GCONV, PV_CONVW, PV_PER = 0, 8, 16, 24, 32, 56
C_IDENT, C_ONES, C_MASK, C_INDA, C_INDB, C_TOT = 0, 128, 256, 384, 512, 1536


def build(L, NTILES, TPS, dbg=None):
    nc = bass.Bass("TRN2", target_bir_lowering=False)
    S = Sched(nc)
    NTOK = NTILES * NT

    x_d = nc.dram_tensor("x", [NTOK, 1024], F32, kind="ExternalInput").ap()
    ws_d = nc.dram_tensor("ws", [L, NSLAB, 128, 1024], F32, kind="ExternalInput").ap()
    wv_d = nc.dram_tensor("wv", [L, 4, 128, 2048], F32, kind="ExternalInput").ap()
    wo_d = nc.dram_tensor("wo", [L, 8, 128, 2048], F32, kind="ExternalInput").ap()
    wg_d = nc.dram_tensor("wg", [128, L * 64], F32, kind="ExternalInput").ap()
    pv_d = nc.dram_tensor("pv", [128, L * PV_PER], F32, kind="ExternalInput").ap()
    gb_d = nc.dram_tensor("gb", [4, L * 2], F32, kind="ExternalInput").ap()
    cst_d = nc.dram_tensor("cst", [128, C_TOT], F32, kind="ExternalInput").ap()
    y_d = nc.dram_tensor("y", [NTOK, 1024], F32, kind="ExternalOutput").ap()
    wsb_d = nc.dram_tensor("wsb", [L, NSLAB, 128, 1024], BF16).ap()
    wvb_d = nc.dram_tensor("wvb", [L, 4, 128, 2048], BF16).ap()
    wob_d = nc.dram_tensor("wob", [L, 8, 128, 2048], BF16).ap()
    dbg_d = {}
    if dbg:
        for nm, shp in dbg.items():
            dbg_d[nm] = nc.dram_tensor("dbg_" + nm, list(shp), F32, kind="ExternalOutput").ap()

    def sb(name, shape, dt=F32):
        return nc.alloc_sbuf_tensor("s_" + name, list(shape), dt)

    cF = sb("cF", [128, C_TOT]);            BcF = S.buf("cF")
    cB = sb("cB", [128, C_TOT], BF16);      BcB = S.buf("cB")
    maskb = sb("maskb", [128, 4, 128], BF16); Bmask = S.buf("mask")
    pv = sb("pv", [128, L * PV_PER]);       Bpv = S.buf("pv")
    gb = sb("gb", [4, L * 2]);              Bgb = S.buf("gb")
    nbf = sb("nbf", [4, L]);                Bnbf = S.buf("nbf")
    wgf = sb("wgf", [128, L * 64]);         Bwgf = S.buf("wgf")
    wgb = sb("wgb", [128, L * 64], BF16);   Bwgb = S.buf("wgb")

    xst = [sb("xst%d" % i, [128, 1024]) for i in range(2)]
    Bxst = [S.buf("xst%d" % i) for i in range(2)]
    ost = [sb("ost%d" % i, [128, 1024]) for i in range(2)]
    Bost = [S.buf("ost%d" % i) for i in range(2)]
    xT = sb("xT", [128, 8, NT]);            BxT = [S.buf("xT%d" % k) for k in range(8)]
    hT = sb("hT", [128, 8, NT], BF16);      BhT = [S.buf("hT%d" % k) for k in range(8)]
    NSQ = 3
    sq = [sb("sq%d" % i, [128, NT], BF16) for i in range(NSQ)]
    Bsq = [S.buf("sq%d" % i) for i in range(NSQ)]
    sd = sb("sd", [128, NT]);               Bsd = S.buf("sd")
    rstd = sb("rstd", [128, NT]);           Brstd = S.buf("rstd")
    NWS = 6
    wsl = [sb("wsl%d" % i, [128, 1024], BF16) for i in range(NWS)]
    Bwsl = [S.buf("wsl%d" % i) for i in range(NWS)]
    NW4 = 4
    w4k = [sb("w4k%d" % i, [128, 2048], BF16) for i in range(NW4)]
    Bw4k = [S.buf("w4k%d" % i) for i in range(NW4)]
    ARENA_B = max(4096 + 4096 + 4096 + NB * 4 * 257 * 2, 8 * NT * 4)
    arena = sb("arena", [128, ARENA_B // 2], BF16)
    qT = arena[:, 0:2048].rearrange("p (h t) -> p h t", h=4)
    kT = arena[:, 2048:4096].rearrange("p (h t) -> p h t", h=4)
    ktok = arena[:, 4096:6144].rearrange("p (b c) -> p b c", b=NB)
    vw = arena[:, 6144:6144 + NB * 4 * 257].rearrange("p (b h v) -> p b h v", b=NB, h=4)
    osb = arena[:, 0:8 * NT * 2].bitcast(F32).rearrange("p (k t) -> p k t", k=8)
    BqT = [S.buf("qT%d" % h) for h in range(4)]
    BkT = [S.buf("kT%d" % h) for h in range(4)]
    Bktok = [S.buf("ktok%d" % b) for b in range(NB)]
    Bvw = [S.buf("vw%d" % b) for b in range(NB)]
    Bosb = [S.buf("osb%d" % k) for k in range(8)]
    arena_m = BqT + BkT + Bktok + Bvw
    g_t = sb("g_t", [4, NT]);   Bg = S.buf("g")
    sp_t = sb("sp_t", [4, NT]); Bsp = S.buf("sp")
    nb_t = sb("nb_t", [4, NT]); Bnb = S.buf("nb")
    w_t = sb("w_t", [4, NT]);   Bw = S.buf("w")
    th_t = sb("th_t", [4, NT]); Bth = S.buf("th")
    sm4 = sb("sm4", [4, 64]);   Bsm4 = S.buf("sm4")
    mst = sb("mst", [4, L]);    Bmst = [S.buf("mst%d" % l) for l in range(L)]
    wthr = sb("wthr", [128, 48]); Bwthr = S.buf("wthr")
    Sm = [sb("Sm%d" % i, [128, 512], BF16) for i in range(2)]
    BSm = [S.buf("Sm%d" % i) for i in range(2)]
    Cst = sb("Cst", [128, L, 4, 257])
    BC = [[S.buf("C%d_%d" % (l, h)) for h in range(4)] for l in range(L)]
    Cd = sb("Cd", [128, 4, 257]);           BCd = [S.buf("Cd%d" % h) for h in range(4)]
    Cb = sb("Cb", [128, 4, 257], BF16);     BCb = [S.buf("Cb%d" % h) for h in range(4)]
    numb = sb("numb", [128, NB, 4, 256], BF16); Bnumb = [S.buf("numb%d" % b) for b in range(NB)]
    junk = sb("junk", [128, 256], BF16);    Bjunk = S.buf("junk")
    sm16 = sb("sm16", [128, 16 * 8]);       Bsm16 = S.buf("sm16")
    Bssr = S.buf("ssr"); Bdn = S.buf("dn"); Bsc = S.buf("sc")
    sz = [sb("sz%d" % i, [128, NT], BF16) for i in range(2)]
    Bsz = [S.buf("sz%d" % i) for i in range(2)]
    mixT = sb("mixT", [128, 16, NT], BF16); Bmix = [S.buf("mix%d" % e) for e in range(16)]
    ub = [sb("ub%d" % i, [128, NT], BF16) for i in range(2)];  Bub = [S.buf("ub%d" % i) for i in range(2)]
    cue = [sb("cue%d" % i, [128, NT + 2], BF16) for i in range(2)]; Bcue = [S.buf("cue%d" % i) for i in range(2)]
    Bb = [sb("Bb%d" % i, [128, NT], BF16) for i in range(2)];  BBb = [S.buf("Bb%d" % i) for i in range(2)]
    dg = [sb("dg%d" % i, [128, 3, 128], BF16) for i in range(2)]; Bdg = [S.buf("dg%d" % i) for i in range(2)]
    tails = sb("tails", [128, L, 8, 2], BF16); Btail = [[S.buf("tl%d_%d" % (l, j)) for j in range(8)] for l in range(L)]
    gsd = sb("gsd", [16, NT]);  Bgsd = S.buf("gsd")
    gr = sb("gr", [16, NT]);    Bgr = S.buf("gr")
    ghi = sb("ghi", [16, NT], BF16); Bghi = S.buf("ghi")
    glo = sb("glo", [16, NT], BF16); Bglo = S.buf("glo")
    tmp = [sb("tmp%d" % i, [128, NT]) for i in range(2)]; Btmp = [S.buf("tmp%d" % i) for i in range(2)]

    PS = [nc.alloc_psum_tensor("ps%d" % i, [128, 512], F32) for i in range(8)]
    BPS = [S.buf("ps%d" % i) for i in range(8)]
    for _b in BPS:
        _b.excl = True
    import os as _os
    pstate = {"i": int(_os.environ.get("PS0", "0")), "pinned": set()}

    def psum(pin=False):
        while True:
            i = pstate["i"]
            pstate["i"] = (i + 1) % 8
            if i not in pstate["pinned"]:
                break
        if pin:
            pstate["pinned"].add(i)
        return i

    def unpin(i):
        pstate["pinned"].discard(i)

    rr = {"ws": 0, "w4": 0, "sq": 0, "ev": 0}

    def nxt(key, n):
        i = rr[key]
        rr[key] = (i + 1) % n
        return i

    identF = cF[:, C_IDENT:C_IDENT + 128]
    identB = cB[:, C_IDENT:C_IDENT + 128]
    onesB = cB[:, C_ONES:C_ONES + 128]
    ones4 = cF[0:4, C_ONES:C_ONES + 128]
    indA = cB[:, C_INDA:C_INDA + 128].rearrange("p (j g) -> p j g", j=8)
    indB = cB[0:16, C_INDB:C_INDB + 1024].rearrange("p (j c) -> p j c", j=8)

    pe, act, dve, pool, sp = S.pe, S.act, S.dve, S.pool, S.sp
    T = nc.tensor
    A = nc.scalar
    V = nc.vector
    G = nc.gpsimd

    def pvcol(l, off):
        return pv[:, l * PV_PER + off: l * PV_PER + off + 1]

    S.dma(sp, cF[:], cst_d[:, :], BcF, writes=[BcF])
    S.dma(sp, pv[:], pv_d[:, :], Bpv, writes=[Bpv])
    S.dma(sp, gb[:], gb_d[:, :], Bgb, writes=[Bgb])
    S.dma(sp, wgf[:], wg_d[:, :], Bwgf, writes=[Bwgf])
    S.op(dve, lambda: V.tensor_copy(out=cB[:], in_=cF[:]), reads=[BcF], writes=[BcB])
    for h in range(4):
        S.op(dve, lambda h=h: V.tensor_copy(out=maskb[:, h, :], in_=cF[:, C_MASK:C_MASK + 128]),
             reads=[BcF], writes=[Bmask])
    S.op(dve, lambda: V.tensor_copy(out=wgb[:], in_=wgf[:]), reads=[Bwgf], writes=[Bwgb])
    for l in range(L):
        S.op(dve, lambda l=l: V.tensor_scalar(out=nbf[:, l:l + 1], in0=gb[:, 2 * l + 1:2 * l + 2], scalar1=-1.0,
                                               scalar2=None, op0=ALU.mult), reads=[Bgb], writes=[Bnbf])

    Bwc = [S.buf("wconv%d" % l) for l in range(L)]

    def convert_layer(l):
        for s0 in range(0, NSLAB, 8):
            S.dma(pool, wsb_d[l, s0:s0 + 8].rearrange("s p f -> (s p) f"),
                  ws_d[l, s0:s0 + 8].rearrange("s p f -> (s p) f"), Bwc[l], writes=[Bwc[l]])
        S.dma(pool, wvb_d[l].rearrange("s p f -> (s p) f"), wv_d[l].rearrange("s p f -> (s p) f"), Bwc[l],
              writes=[Bwc[l]])
        S.dma(pool, wob_d[l].rearrange("s p f -> (s p) f"), wo_d[l].rearrange("s p f -> (s p) f"), Bwc[l],
              writes=[Bwc[l]])

    for l in range(L):
        convert_layer(l)

    def evac_eng():
        i = nxt("ev", 2)
        return i

    def copy_on(which, out, in_, reads, writes, scale=None):
        if which == 0:
            if scale is None:
                return S.op(act, lambda: A.activation(out=out, in_=in_, func=AF.Copy), reads=reads, writes=writes)
            return S.op(act, lambda: A.activation(out=out, in_=in_, func=AF.Copy, scale=scale), reads=reads,
                        writes=writes)
        if scale is None:
            return S.op(dve, lambda: V.tensor_copy(out=out, in_=in_), reads=reads, writes=writes)
        return S.op(dve, lambda: V.tensor_scalar(out=out, in0=in_, scalar1=scale, scalar2=None, op0=ALU.mult),
                    reads=reads, writes=writes)

    def load_slab(l, s):
        r = nxt("ws", NWS)
        S.dma(sp, wsl[r][:], wsb_d[l, s], Bwsl[r], reads=[Bwc[l]], writes=[Bwsl[r]])
        return r

    def slab_mm(l, s, M=128):
        r = load_slab(l, s)
        b = psum()
        for k in range(8):
            S.op(pe, lambda k=k: T.matmul(PS[b][0:M, :], lhsT=wsl[r][:, k * 128:k * 128 + M], rhs=hT[:, k, :],
                                          start=(k == 0), stop=(k == 7)),
                 reads=[Bwsl[r], BhT[k]], writes=[BPS[b]])
        return b

    dbg_bufs = []

    def dump(nm, src_ap, bufs):
        if nm in dbg_d:
            t = S.buf("dbgtmp_" + nm)
            S.dma(pool, dbg_d[nm], src_ap, t, reads=bufs, writes=[t])
            dbg_bufs.append(t)

    def layer(l, ti, first_in_seq):
        bss = psum(pin=True)
        for k in range(8):
            i = nxt("sq", NSQ)
            S.op(act, lambda: A.activation(out=sq[i][:], in_=xT[:, k, :], func=AF.Square),
                 reads=[BxT[k]], writes=[Bsq[i]])
            S.op(pe, lambda: T.matmul(PS[bss][:], lhsT=onesB, rhs=sq[i][:], start=(k == 0), stop=(k == 7)),
                 reads=[Bsq[i], BcB], writes=[BPS[bss]])
        S.op(act, lambda: A.activation(out=sd[:], in_=PS[bss][:], func=AF.Sqrt, scale=1.0 / 1024, bias=EPSB),
             reads=[BPS[bss], Beps], writes=[Bsd])
        unpin(bss)
        S.op(dve, lambda: V.reciprocal(out=rstd[:], in_=sd[:]), reads=[Bsd], writes=[Brstd])
        for k in range(8):
            S.op(dve, lambda: V.scalar_tensor_tensor(out=hT[:, k, :], in0=xT[:, k, :], scalar=pvcol(l, PV_GPRE + k),
                                                     in1=rstd[:], op0=ALU.mult, op1=ALU.mult),
                 reads=[BxT[k], Brstd, Bpv], writes=[BhT[k]])
        if l == 0 and ti == 0:
            dump("hT", hT[:, 0, :], [BhT[0]])

        bi = psum()
        bf = psum()
        for (bb, c0) in ((bi, 0), (bf, 4)):
            for k in range(8):
                S.op(pe, lambda: T.matmul(PS[bb][0:4, :], lhsT=wgb[:, l * 64 + k * 8 + c0: l * 64 + k * 8 + c0 + 4],
                                          rhs=hT[:, k, :], start=(k == 0), stop=(k == 7)),
                     reads=[Bwgb, BhT[k]], writes=[BPS[bb]])
        S.op(dve, lambda: V.tensor_scalar(out=g_t[:], in0=PS[bi][0:4, :], scalar1=gb[:, 2 * l:2 * l + 1], scalar2=None,
                                          op0=ALU.add), reads=[BPS[bi], Bgb], writes=[Bg])
        S.op(act, lambda: A.activation(out=sp_t[:], in_=PS[bf][0:4, :], func=AF.Exp, scale=-1.0, bias=nbf[:, l:l + 1]),
             reads=[BPS[bf], Bnbf], writes=[Bsp])
        S.op(act, lambda: A.activation(out=sp_t[:], in_=sp_t[:], func=AF.Ln, bias=ONEB[0:4, :]), reads=[Bsp, Beps],
             writes=[Bsp])
        for blk in range(NB):
            S.op(dve, lambda: V.tensor_tensor_scan(out=nb_t[:, blk * 128:(blk + 1) * 128], data0=ones4,
                                                   data1=sp_t[:, blk * 128:(blk + 1) * 128], initial=0.0,
                                                   op0=ALU.mult, op1=ALU.add),
                 reads=[Bsp, BcF], writes=[Bnb])
        S.op(dve, lambda: V.tensor_tensor(out=g_t[:], in0=g_t[:], in1=nb_t[:], op=ALU.add), reads=[Bg, Bnb],
             writes=[Bg])
        S.op(dve, lambda: V.tensor_reduce(out=sm4[:, 0:4], in_=g_t[:].rearrange("p (b t) -> p b t", b=NB),
                                          op=ALU.max, axis=AX.X), reads=[Bg], writes=[Bsm4])
        for blk in range(NB):
            mprev = mst[:, l:l + 1] if blk == 0 else sm4[:, 12 + blk - 1:12 + blk]
            S.op(dve, lambda: V.tensor_tensor(out=sm4[:, 4 + blk:5 + blk], in0=mprev, in1=sm4[:, blk:blk + 1],
                                              op=ALU.max), reads=[Bsm4, Bmst[l]], writes=[Bsm4])
            S.op(dve, lambda: V.tensor_tensor(out=sm4[:, 8 + blk:9 + blk], in0=mprev, in1=sm4[:, 4 + blk:5 + blk],
                                              op=ALU.subtract), reads=[Bsm4, Bmst[l]], writes=[Bsm4])
            S.op(dve, lambda: V.tensor_tensor(out=sm4[:, 12 + blk:13 + blk], in0=sm4[:, 4 + blk:5 + blk],
                                              in1=nb_t[:, blk * 128 + 127:blk * 128 + 128], op=ALU.subtract),
                 reads=[Bsm4, Bnb], writes=[Bsm4])
        S.op(dve, lambda: V.tensor_copy(out=mst[:, l:l + 1], in_=sm4[:, 15:16]), reads=[Bsm4], writes=[Bmst[l]])
        S.op(dve, lambda: V.tensor_scalar(out=sm4[:, 16:20], in0=sm4[:, 4:8], scalar1=-1.0, scalar2=None,
                                          op0=ALU.mult), reads=[Bsm4], writes=[Bsm4])
        for blk in range(NB):
            sl = slice(blk * 128, (blk + 1) * 128)
            S.op(act, lambda: A.activation(out=w_t[:, sl], in_=g_t[:, sl], func=AF.Exp,
                                           bias=sm4[:, 16 + blk:17 + blk]), reads=[Bg, Bsm4], writes=[Bw])
            S.op(act, lambda: A.activation(out=th_t[:, sl], in_=nb_t[:, sl], func=AF.Exp,
                                           bias=sm4[:, 16 + blk:17 + blk]), reads=[Bnb, Bsm4], writes=[Bth])
        S.op(act, lambda: A.activation(out=sm4[:, 20:24], in_=sm4[:, 8:12], func=AF.Exp), reads=[Bsm4],
             writes=[Bsm4])
        for blk in range(NB):
            S.op(dve, lambda: V.tensor_scalar(out=sm4[:, 32 + blk * 4:36 + blk * 4], in0=cF[0:4, C_IDENT:C_IDENT + 4],
                                              scalar1=sm4[:, 20 + blk:21 + blk], scalar2=None, op0=ALU.mult),
                 reads=[Bsm4, BcF], writes=[Bsm4])
        bw = psum()
        for blk in range(NB):
            sl = slice(blk * 128, (blk + 1) * 128)
            S.op(pe, lambda: T.transpose(out=PS[bw][:, blk * 8:blk * 8 + 4], in_=w_t[:, sl],
                                         identity=cF[0:4, C_IDENT:C_IDENT + 4]), reads=[Bw, BcF], writes=[BPS[bw]])
            S.op(pe, lambda: T.transpose(out=PS[bw][:, blk * 8 + 4:blk * 8 + 8], in_=th_t[:, sl],
                                         identity=cF[0:4, C_IDENT:C_IDENT + 4]), reads=[Bth, BcF], writes=[BPS[bw]])
        S.op(pe, lambda: T.matmul(PS[bw][:, 32:48], lhsT=ones4, rhs=sm4[:, 32:48], start=True, stop=True),
             reads=[Bsm4, BcF], writes=[BPS[bw]])
        S.op(dve, lambda: V.tensor_copy(out=wthr[:, 0:48], in_=PS[bw][:, 0:48]), reads=[BPS[bw]], writes=[Bwthr])
        if l == 0 and ti == 0:
            dump("wthr", wthr[:, :], [Bwthr])

        for h in range(4):
            b = slab_mm(l, h)
            S.op(act, lambda: A.activation(out=qT[:, h, :], in_=PS[b][:], func=AF.Copy, scale=float(128 ** -0.5)),
                 reads=[BPS[b]], writes=[BqT[h]] + Bosb)
        for h in range(4):
            b = slab_mm(l, 4 + h)
            S.op(dve, lambda: V.tensor_copy(out=kT[:, h, :], in_=PS[b][:]), reads=[BPS[b]], writes=[BkT[h]] + Bosb)
        for blk in range(NB):
            b = psum()
            pb = PS[b][:].bitcast(BF16)
            for h in range(4):
                S.op(pe, lambda: T.transpose(out=pb[:, h * 128:(h + 1) * 128], in_=kT[:, h, blk * 128:(blk + 1) * 128],
                                             identity=identB), reads=[BkT[h], BcB], writes=[BPS[b]])
            copy_on(blk % 2, ktok[:, blk, :], pb[:, 0:512], [BPS[b]], [Bktok[blk]] + Bosb)
        S.op(dve, lambda: V.tensor_copy(out=vw[:, :, :, 256],
                                        in_=wthr[:, 0:32].rearrange("p (b e) -> p b e", e=8)[:, :, 0:4]),
             reads=[Bwthr], writes=Bvw + Bosb)
        for h in range(4):
            r = nxt("w4", NW4)
            S.dma(sp, w4k[r][:], wvb_d[l, h], Bw4k[r], reads=[Bwc[l]], writes=[Bw4k[r]])
            for blk in range(NB):
                b = psum()
                for k in range(8):
                    S.op(pe, lambda: T.matmul(PS[b][:, 0:256], lhsT=hT[:, k, blk * 128:(blk + 1) * 128],
                                              rhs=w4k[r][:, k * 256:(k + 1) * 256], start=(k == 0), stop=(k == 7)),
                         reads=[Bw4k[r], BhT[k]], writes=[BPS[b]])
                S.op(dve, lambda: V.tensor_scalar(out=vw[:, blk, h, 0:256], in0=PS[b][:, 0:256],
                                                  scalar1=wthr[:, blk * 8 + h:blk * 8 + h + 1], scalar2=None,
                                                  op0=ALU.mult), reads=[BPS[b], Bwthr], writes=[Bvw[blk]])

        for blk in range(NB):
            sl = slice(blk * 128, (blk + 1) * 128)
            for h in range(4):
                S.op(dve, lambda: V.tensor_scalar(out=Cd[:, h, :], in0=Cst[:, l, h, :],
                                                  scalar1=wthr[:, 32 + blk * 4 + h:33 + blk * 4 + h], scalar2=None,
                                                  op0=ALU.mult), reads=[BC[l][h], Bwthr], writes=[BCd[h]])
                S.op(act, lambda: A.activation(out=Cb[:, h, :], in_=Cd[:, h, :], func=AF.Copy), reads=[BCd[h]],
                     writes=[BCb[h]])
            bS = psum()
            for h in range(4):
                S.op(pe, lambda: T.matmul(PS[bS][:, h * 128:(h + 1) * 128], lhsT=kT[:, h, sl], rhs=qT[:, h, sl],
                                          start=True, stop=True), reads=[BkT[h], BqT[h]], writes=[BPS[bS]])
            si = blk % 2
            S.op(dve, lambda: V.tensor_tensor(out=Sm[si][:], in0=PS[bS][:], in1=maskb[:].rearrange("p h t -> p (h t)"),
                                              op=ALU.mult), reads=[BPS[bS], Bmask], writes=[BSm[si]])
            bN = [psum(), psum()]
            bD = psum()
            for h in range(4):
                on = PS[bN[h // 2]][:, (h % 2) * 256:(h % 2) * 256 + 256]
                S.op(pe, lambda: T.matmul(on, lhsT=Sm[si][:, h * 128:(h + 1) * 128], rhs=vw[:, blk, h, 0:256],
                                          start=True, stop=False), reads=[BSm[si], Bvw[blk]], writes=[BPS[bN[h // 2]]])
                S.op(pe, lambda: T.matmul(on, lhsT=qT[:, h, sl], rhs=Cb[:, h, 0:256], start=False, stop=True),
                     reads=[BqT[h], BCb[h]], writes=[BPS[bN[h // 2]]])
                S.op(pe, lambda: T.matmul(PS[bD][:, h:h + 1], lhsT=Sm[si][:, h * 128:(h + 1) * 128],
                                          rhs=vw[:, blk, h, 256:257], start=True, stop=False),
                     reads=[BSm[si], Bvw[blk]], writes=[BPS[bD]])
                S.op(pe, lambda: T.matmul(PS[bD][:, h:h + 1], lhsT=qT[:, h, sl], rhs=Cb[:, h, 256:257], start=False,
                                          stop=True), reads=[BqT[h], BCb[h]], writes=[BPS[bD]])
            bC = [psum(), psum()]
            for h in range(4):
                S.op(pe, lambda: T.matmul(PS[bC[h // 2]][:, (h % 2) * 256:(h % 2) * 256 + 256],
                                          lhsT=ktok[:, blk, h * 128:(h + 1) * 128], rhs=vw[:, blk, h, 0:256],
                                          start=True, stop=True), reads=[Bktok[blk], Bvw[blk]],
                     writes=[BPS[bC[h // 2]]])
                S.op(pe, lambda: T.matmul(PS[bD][:, 4 + h:5 + h], lhsT=ktok[:, blk, h * 128:(h + 1) * 128],
                                          rhs=vw[:, blk, h, 256:257], start=True, stop=True),
                     reads=[Bktok[blk], Bvw[blk]], writes=[BPS[bD]])
            for h in range(4):
                S.op(dve, lambda: V.tensor_tensor(out=Cst[:, l, h, 0:256], in0=Cd[:, h, 0:256],
                                                  in1=PS[bC[h // 2]][:, (h % 2) * 256:(h % 2) * 256 + 256],
                                                  op=ALU.add), reads=[BCd[h], BPS[bC[h // 2]]], writes=[BC[l][h]])
            S.op(dve, lambda: V.tensor_tensor(out=Cst[:, l, :, 256], in0=Cd[:, :, 256], in1=PS[bD][:, 4:8],
                                              op=ALU.add), reads=BCd + [BPS[bD]], writes=BC[l])
            S.op(act, lambda: A.activation(out=sm16[:, blk * 4:blk * 4 + 4], in_=PS[bD][:, 0:4], func=AF.Abs),
                 reads=[BPS[bD]], writes=[Bdn])
            S.op(dve, lambda: V.tensor_tensor(out=sm16[:, blk * 4:blk * 4 + 4], in0=sm16[:, blk * 4:blk * 4 + 4],
                                              in1=wthr[:, blk * 8 + 4:blk * 8 + 8], op=ALU.max),
                 reads=[Bdn, Bwthr], writes=[Bdn])
            for h in range(4):
                S.op(act, lambda: A.activation(out=junk[:], in_=PS[bN[h // 2]][:, (h % 2) * 256:(h % 2) * 256 + 256],
                                               func=AF.Square,
                                               accum_out=sm16[:, 16 + blk * 4 + h:17 + blk * 4 + h]),
                     reads=[BPS[bN[h // 2]]], writes=[Bjunk, Bssr])
            for pr in range(2):
                S.op(dve, lambda: V.tensor_copy(out=numb[:, blk, 2 * pr:2 * pr + 2, :].rearrange("p a v -> p (a v)"),
                                                in_=PS[bN[pr]][:]), reads=[BPS[bN[pr]]], writes=[Bnumb[blk]])
        S.op(dve, lambda: V.reciprocal(out=sm16[:, 32:48], in_=sm16[:, 0:16]), reads=[Bdn], writes=[Bsm16])
        S.op(dve, lambda: V.tensor_tensor(out=sm16[:, 48:64], in0=sm16[:, 32:48], in1=sm16[:, 32:48], op=ALU.mult),
             reads=[Bsm16], writes=[Bsm16])
        S.op(dve, lambda: V.tensor_tensor(out=sm16[:, 64:80], in0=sm16[:, 48:64], in1=sm16[:, 16:32], op=ALU.mult),
             reads=[Bsm16, Bssr], writes=[Bsm16])
        S.op(act, lambda: A.activation(out=sm16[:, 80:96], in_=sm16[:, 64:80], func=AF.Sqrt, scale=1.0 / 256,
                                       bias=EPSB), reads=[Bsm16, Beps], writes=[Bsm16])
        S.op(dve, lambda: V.reciprocal(out=sm16[:, 96:112], in_=sm16[:, 80:96]), reads=[Bsm16], writes=[Bsm16])
        S.op(dve, lambda: V.tensor_tensor(out=sm16[:, 112:128], in0=sm16[:, 96:112], in1=sm16[:, 32:48],
                                          op=ALU.mult), reads=[Bsm16], writes=[Bsc])
        for blk in range(NB):
            S.op(dve, lambda: V.tensor_tensor(out=numb[:, blk, :, :], in0=numb[:, blk, :, :],
                                              in1=sm16[:, 112 + blk * 4:116 + blk * 4].unsqueeze(2).broadcast_to(
                                                  [128, 4, 256]), op=ALU.mult),
                 reads=[Bnumb[blk], Bsc], writes=[Bnumb[blk]])
        if l == 0 and ti == 0:
            dump("sm16", sm16[:, :], [Bsm16, Bsc, Bssr, Bdn])

        for e in range(8):
            b = slab_mm(l, 8 + e)
            S.op(act, lambda: A.activation(out=mixT[:, e, :], in_=PS[b][:], func=AF.Sigmoid), reads=[BPS[b]],
                 writes=[Bmix[e]])
        for e in range(8):
            b = slab_mm(l, 16 + e)
            i = e % 2
            S.op(act, lambda: A.activation(out=sz[i][:], in_=PS[b][:], func=AF.Silu), reads=[BPS[b]], writes=[Bsz[i]])
            S.op(dve, lambda: V.tensor_tensor(out=mixT[:, e, :], in0=mixT[:, e, :], in1=sz[i][:], op=ALU.mult),
                 reads=[Bmix[e], Bsz[i]], writes=[Bmix[e]])
        for e in range(8):
            h, half = e // 2, e % 2
            b = psum()
            pb = PS[b][:].bitcast(BF16)
            for blk in range(NB):
                S.op(pe, lambda: T.transpose(out=pb[:, blk * 128:(blk + 1) * 128],
                                             in_=numb[:, blk, h, half * 128:(half + 1) * 128], identity=identB),
                     reads=[Bnumb[blk], BcB], writes=[BPS[b]])
            S.op(dve, lambda: V.scalar_tensor_tensor(out=mixT[:, e, :], in0=pb[:, 0:512],
                                                     scalar=pvcol(l, PV_GHEAD + e), in1=mixT[:, e, :], op0=ALU.mult,
                                                     op1=ALU.mult), reads=[BPS[b], Bmix[e], Bpv], writes=[Bmix[e]])
        if l == 0 and ti == 0:
            dump("ym", mixT[:, 0, :], [Bmix[0]])

        bg = psum(pin=True)
        for j in range(8):
            i = j % 2
            S.op(pool, lambda: G.tensor_copy(out=cue[i][:, 0:2], in_=tails[:, l, j, :]), reads=[Btail[l][j]],
                 writes=[Bcue[i]])
            for tap in range(3):
                S.op(pool, lambda: G.tensor_scalar(out=dg[i][:, tap, :], in0=identB,
                                                   scalar1=pvcol(l, PV_CONVW + j * 3 + tap), scalar2=None,
                                                   op0=ALU.mult), reads=[BcB, Bpv], writes=[Bdg[i]])
            bu = slab_mm(l, 24 + 4 * j + 0)
            S.op(act, lambda: A.activation(out=ub[i][:], in_=PS[bu][:], func=AF.Copy), reads=[BPS[bu]],
                 writes=[Bub[i]])
            bc = slab_mm(l, 24 + 4 * j + 1)
            S.op(dve, lambda: V.tensor_tensor(out=cue[i][:, 2:NT + 2], in0=PS[bc][:], in1=ub[i][:], op=ALU.mult),
                 reads=[BPS[bc], Bub[i]], writes=[Bcue[i]])
            S.op(pool, lambda: G.tensor_copy(out=tails[:, l, j, :], in_=cue[i][:, NT:NT + 2]), reads=[Bcue[i]],
                 writes=[Btail[l][j]])
            by = psum()
            for tap in range(3):
                S.op(pe, lambda: T.matmul(PS[by][:], lhsT=dg[i][:, tap, :], rhs=cue[i][:, tap:tap + NT],
                                          start=(tap == 0), stop=(tap == 2)), reads=[Bdg[i], Bcue[i]],
                     writes=[BPS[by]])
            bB = slab_mm(l, 24 + 4 * j + 2)
            S.op(act, lambda: A.activation(out=Bb[i][:], in_=PS[bB][:], func=AF.Copy), reads=[BPS[bB]],
                 writes=[BBb[i]])
            S.op(dve, lambda: V.tensor_tensor(out=mixT[:, 8 + j, :], in0=PS[by][:], in1=Bb[i][:], op=ALU.mult),
                 reads=[BPS[by], BBb[i]], writes=[Bmix[8 + j]])
            qi = nxt("sq", NSQ)
            S.op(act, lambda: A.activation(out=sq[qi][:], in_=mixT[:, 8 + j, :], func=AF.Square),
                 reads=[Bmix[8 + j]], writes=[Bsq[qi]])
            S.op(pe, lambda: T.matmul(PS[bg][0:16, :], lhsT=indA[:, j, :], rhs=sq[qi][:], start=(j == 0),
                                      stop=(j == 7)), reads=[Bsq[qi], BcB], writes=[BPS[bg]])
            bz = slab_mm(l, 24 + 4 * j + 3)
            S.op(act, lambda: A.activation(out=sz[i][:], in_=PS[bz][:], func=AF.Silu), reads=[BPS[bz]],
                 writes=[Bsz[i]])
            S.op(dve, lambda: V.tensor_tensor(out=mixT[:, 8 + j, :], in0=mixT[:, 8 + j, :], in1=sz[i][:],
                                              op=ALU.mult), reads=[Bmix[8 + j], Bsz[i]], writes=[Bmix[8 + j]])
        S.op(act, lambda: A.activation(out=gsd[:], in_=PS[bg][0:16, :], func=AF.Sqrt, scale=1.0 / 64,
                                       bias=EPSB[0:16, :]), reads=[BPS[bg], Beps], writes=[Bgsd])
        unpin(bg)
        S.op(dve, lambda: V.reciprocal(out=gr[:], in_=gsd[:]), reads=[Bgsd], writes=[Bgr])
        S.op(dve, lambda: V.tensor_copy(out=ghi[:], in_=gr[:]), reads=[Bgr], writes=[Bghi])
        S.op(dve, lambda: V.tensor_tensor(out=glo[:], in0=gr[:], in1=ghi[:], op=ALU.subtract), reads=[Bgr, Bghi],
             writes=[Bglo])
        for j in range(8):
            b = psum()
            S.op(pe, lambda: T.matmul(PS[b][:], lhsT=indB[:, j, :], rhs=ghi[:], start=True, stop=False),
                 reads=[Bghi, BcB], writes=[BPS[b]])
            S.op(pe, lambda: T.matmul(PS[b][:], lhsT=indB[:, j, :], rhs=glo[:], start=False, stop=True),
                 reads=[Bglo, BcB], writes=[BPS[b]])
            S.op(dve, lambda: V.scalar_tensor_tensor(out=mixT[:, 8 + j, :], in0=mixT[:, 8 + j, :],
                                                     scalar=pvcol(l, PV_GCONV + j), in1=PS[b][:], op0=ALU.mult,
                                                     op1=ALU.mult), reads=[Bmix[8 + j], BPS[b], Bpv],
                 writes=[Bmix[8 + j]])
        if l == 0 and ti == 0:
            dump("yc", mixT[:, 8, :], [Bmix[8]])

        bss = psum(pin=True)
        for dc in range(8):
            r = nxt("w4", NW4)
            S.dma(sp, w4k[r][:], wob_d[l, dc], Bw4k[r], reads=[Bwc[l]], writes=[Bw4k[r]])
            b = psum()
            for e in range(16):
                S.op(pe, lambda: T.matmul(PS[b][:], lhsT=w4k[r][:, e * 128:(e + 1) * 128], rhs=mixT[:, e, :],
                                          start=(e == 0), stop=(e == 15)), reads=[Bw4k[r], Bmix[e]],
                     writes=[BPS[b]])
            S.op(act, lambda: A.activation(out=osb[:, dc, :], in_=PS[b][:], func=AF.Copy), reads=[BPS[b]],
                 writes=[Bosb[dc]] + arena_m)
            qi = nxt("sq", NSQ)
            S.op(dve, lambda: V.tensor_tensor(out=sq[qi][:], in0=PS[b][:], in1=osb[:, dc, :], op=ALU.mult),
                 reads=[BPS[b], Bosb[dc]], writes=[Bsq[qi]])
            S.op(pe, lambda: T.matmul(PS[bss][:], lhsT=onesB, rhs=sq[qi][:], start=(dc == 0), stop=(dc == 7)),
                 reads=[Bsq[qi], BcB], writes=[BPS[bss]])
        S.op(act, lambda: A.activation(out=sd[:], in_=PS[bss][:], func=AF.Sqrt, scale=1.0 / 1024, bias=EPSB),
             reads=[BPS[bss], Beps], writes=[Bsd])
        unpin(bss)
        S.op(dve, lambda: V.reciprocal(out=rstd[:], in_=sd[:]), reads=[Bsd], writes=[Brstd])
        for k in range(8):
            i = k % 2
            S.op(dve, lambda: V.scalar_tensor_tensor(out=tmp[i][:], in0=osb[:, k, :], scalar=pvcol(l, PV_GPOST + k),
                                                     in1=rstd[:], op0=ALU.mult, op1=ALU.mult),
                 reads=[Bosb[k], Brstd, Bpv], writes=[Btmp[i]])
            S.op(dve, lambda: V.tensor_tensor(out=xT[:, k, :], in0=xT[:, k, :], in1=tmp[i][:], op=ALU.add),
                 reads=[BxT[k], Btmp[i]], writes=[BxT[k]])

    epsb = sb("epsb", [128, 2])
    Beps = S.buf("eps")
    S.op(dve, lambda: V.memset(epsb[:, 0:1], EPS), writes=[Beps])
    S.op(dve, lambda: V.memset(epsb[:, 1:2], 1.0), writes=[Beps])
    EPSB = epsb[:, 0:1]
    ONEB = epsb[:, 1:2]

    for ti in range(NTILES):
        tis = ti % TPS
        tok0 = ti * NT
        if tis == 0:
            S.op(pool, lambda: G.memset(Cst[:].rearrange("p l h v -> p (l h v)"), 0.0),
                 writes=[b for bl in BC for b in bl])
            S.op(pool, lambda: G.memset(mst[:], 0.0), writes=Bmst)
            S.op(pool, lambda: G.memset(tails[:].rearrange("p l j t -> p (l j t)"), 0.0),
                 writes=[b for bl in Btail for b in bl])
        for blk in range(NB):
            i = blk % 2
            S.dma(sp, xst[i][:], x_d[tok0 + blk * 128: tok0 + (blk + 1) * 128, :], Bxst[i], writes=[Bxst[i]])
            for half in range(2):
                b = psum()
                for kk in range(4):
                    k = half * 4 + kk
                    S.op(pe, lambda: T.transpose(out=PS[b][:, kk * 128:(kk + 1) * 128],
                                                 in_=xst[i][:, k * 128:(k + 1) * 128], identity=identF),
                         reads=[Bxst[i], BcF], writes=[BPS[b]])
                copy_on(half, xT[:, half * 4:half * 4 + 4, blk * 128:(blk + 1) * 128],
                        PS[b][:].rearrange("p (a t) -> p a t", a=4), [BPS[b]], BxT[half * 4:half * 4 + 4])
        for l in range(L):
            layer(l, ti, tis == 0)
        for blk in range(NB):
            i = blk % 2
            for half in range(2):
                b = psum()
                for kk in range(4):
                    k = half * 4 + kk
                    S.op(pe, lambda: T.transpose(out=PS[b][:, kk * 128:(kk + 1) * 128],
                                                 in_=xT[:, k, blk * 128:(blk + 1) * 128], identity=identF),
                         reads=[BxT[k], BcF], writes=[BPS[b]])
                copy_on(half, ost[i][:, half * 512:(half + 1) * 512], PS[b][:], [BPS[b]], [Bost[i]])
            S.dma(pool, y_d[tok0 + blk * 128: tok0 + (blk + 1) * 128, :], ost[i][:], Bost[i], reads=[Bost[i]])
    S.wait_all(pool, Bost)
    S.wait_all(sp, Bost + dbg_bufs)
    return nc, S


def _slab_cols():
    cols = []
    for h in range(4):
        cols.append(0 + 128 * h)
    for h in range(4):
        cols.append(512 + 128 * h)
    for e in range(8):
        cols.append(2048 + 128 * e)
    for e in range(8):
        cols.append(3072 + 128 * e)
    for j in range(8):
        cols.append(4104 + 128 * j)
        cols.append(6152 + 128 * j)
        cols.append(5128 + 128 * j)
        cols.append(7176 + 128 * j)
    return cols


def _consts():
    c = np.zeros((128, C_TOT), np.float32)
    c[:, C_IDENT:C_IDENT + 128] = np.eye(128, dtype=np.float32)
    c[:, C_ONES:C_ONES + 128] = 1.0
    s = np.arange(128)
    c[:, C_MASK:C_MASK + 128] = (s[:, None] <= s[None, :]).astype(np.float32)
    indA = np.zeros((128, 8, 16), np.float32)
    indB = np.zeros((128, 8, 128), np.float32)
    for j in range(8):
        for p in range(128):
            g = 2 * j + (1 if p >= 64 else 0)
            indA[p, j, g] = 1.0
            indB[g, j, p] = 1.0
    c[:, C_INDA:C_INDA + 128] = indA.reshape(128, 128)
    c[:, C_INDB:C_INDB + 1024] = indB.reshape(128, 1024)
    return c


def _prep_layers(layers, norm_pre, norm_post, w_in, b_igate, b_fgate, head_norm, conv_w, conv_norm, w_out):
    L = len(layers)
    cols = np.array(_slab_cols())
    colidx = (cols[:, None] + np.arange(128)[None, :])
    ws = np.empty((L, NSLAB, 128, 1024), np.float32)
    wv = np.empty((L, 4, 128, 2048), np.float32)
    wo = np.empty((L, 8, 128, 2048), np.float32)
    wg = np.empty((128, L * 64), np.float32)
    pv = np.empty((128, L * PV_PER), np.float32)
    gb = np.empty((4, L * 2), np.float32)
    for li, l in enumerate(layers):
        W = np.asarray(w_in[l]).reshape(8, 128, 8200)
        ws[li] = W[:, :, colidx].transpose(2, 1, 0, 3).reshape(NSLAB, 128, 1024)
        wv[li] = W[:, :, 1024:2048].reshape(8, 128, 4, 256).transpose(2, 1, 0, 3).reshape(4, 128, 2048)
        wo[li] = np.asarray(w_out[l]).reshape(16, 128, 8, 128).transpose(2, 1, 0, 3).reshape(8, 128, 2048)
        wg[:, li * 64:(li + 1) * 64] = W[:, :, 4096:4104].transpose(1, 0, 2).reshape(128, 64)
        o = li * PV_PER
        pv[:, o + PV_GPRE:o + PV_GPRE + 8] = np.asarray(norm_pre[l]).reshape(8, 128).T
        pv[:, o + PV_GPOST:o + PV_GPOST + 8] = np.asarray(norm_post[l]).reshape(8, 128).T
        pv[:, o + PV_GHEAD:o + PV_GHEAD + 8] = np.asarray(head_norm[l]).reshape(8, 128).T
        pv[:, o + PV_GCONV:o + PV_GCONV + 8] = np.asarray(conv_norm[l]).reshape(8, 128).T
        pv[:, o + PV_CONVW:o + PV_CONVW + 24] = np.asarray(conv_w[l]).reshape(3, 8, 128).transpose(2, 1, 0).reshape(128, 24)
        gb[:, 2 * li] = np.asarray(b_igate[l])
        gb[:, 2 * li + 1] = np.asarray(b_fgate[l])
    return dict(ws=ws, wv=wv, wo=wo, wg=wg, pv=pv, gb=gb, cst=_consts())


_PROG_CACHE = {}


def _get_prog(L, NTILES, TPS):
    key = (L, NTILES, TPS)
    if key not in _PROG_CACHE:
        _PROG_CACHE[key] = build(L, NTILES, TPS)[0]
    return _PROG_CACHE[key]


FUSED = False


def kernel(x, norm_pre, norm_post, w_in, b_igate, b_fgate, head_norm, conv_w, conv_norm, w_out):
    x = np.asarray(x, dtype=np.float32)
    Bt, Sq, D = x.shape
    per = Bt // NCORES
    TPS = Sq // NT
    NTILES = per * TPS
    xs = [np.ascontiguousarray(x[c * per:(c + 1) * per].reshape(per * Sq, D)) for c in range(NCORES)]
    groups = [list(range(4))] if FUSED else [[l] for l in range(4)]
    for layers in groups:
        prm = _prep_layers(layers, norm_pre, norm_post, w_in, b_igate, b_fgate, head_norm, conv_w, conv_norm, w_out)
        nc = _get_prog(len(layers), NTILES, TPS)
        in_maps = [dict(prm, x=xs[c]) for c in range(NCORES)]
        res = run_bass_kernel_spmd(nc, in_maps, core_ids=list(range(NCORES)))
        xs = [np.asarray(res.results[c]["y"], dtype=np.float32) for c in range(NCORES)]
    out = np.stack([xc.reshape(per, Sq, D) for xc in xs], axis=0).reshape(Bt, Sq, D)
    return out
```

```python
import numpy as np
import concourse.bass as bass
import concourse.mybir as mybir
from concourse.bass_utils import run_bass_kernel_spmd

F32 = mybir.dt.float32
BF16 = mybir.dt.bfloat16
AF = mybir.ActivationFunctionType
ALU = mybir.AluOpType
AX = mybir.AxisListType

EPS = 1e-6
NT = 512
NB = 4
NSLAB = 56
NCORES = 8
S_EPOCH = 30000


class Buf:
    __slots__ = ("name", "writers", "readers", "dsem", "dcount", "excl")

    def __init__(self, name):
        self.name = name
        self.excl = False
        self.writers = []
        self.readers = []
        self.dsem = None
        self.dcount = 0


class Eng:
    def __init__(self, S, name, h):
        self.S = S
        self.name = name
        self.h = h
        self.sems = []
        self.count = 0
        self.seen = {}

    def cur_sem(self):
        if not self.sems or self.count >= S_EPOCH:
            self.sems.append(self.S.nc.alloc_semaphore("%s_e%d" % (self.name, len(self.sems))))
            self.count = 0
        return self.sems[-1]


class Sched:
    def __init__(self, nc):
        self.nc = nc
        self.pe = Eng(self, "pe", nc.tensor)
        self.act = Eng(self, "act", nc.scalar)
        self.dve = Eng(self, "dve", nc.vector)
        self.pool = Eng(self, "pool", nc.gpsimd)
        self.sp = Eng(self, "sp", nc.sync)
        self.nbuf = 0
        self.n_ins = 0
        self.n_wait = 0

    def buf(self, name=None):
        self.nbuf += 1
        return Buf(name or "b%d" % self.nbuf)

    def _wait(self, eng, tok):
        sem, val, src = tok
        key = id(sem)
        if eng.seen.get(key, 0) >= val:
            return
        eng.h.wait_ge(sem, val)
        eng.seen[key] = val
        self.n_wait += 1

    def _deps(self, reads, writes):
        deps = []
        for b in reads:
            deps.extend(b.writers)
            if b.excl:
                deps.extend(b.readers)
        for b in writes:
            deps.extend(b.writers)
            deps.extend(b.readers)
        return deps

    def _commit(self, tok, reads, writes):
        for b in reads:
            b.readers.append(tok)
        for b in writes:
            b.writers = [tok]
            b.readers = []

    def op(self, eng, fn, reads=(), writes=()):
        for tok in self._deps(reads, writes):
            if tok[2] is eng and eng is self.pe:
                continue
            self._wait(eng, tok)
        sem = eng.cur_sem()
        ins = fn()
        ins.then_inc(sem, 1)
        eng.count += 1
        tok = (sem, eng.count, eng)
        self.n_ins += 1
        self._commit(tok, reads, writes)
        return tok

    def dma(self, eng, out, in_, sb, reads=(), writes=(), **kw):
        for tok in self._deps(reads, writes):
            self._wait(eng, tok)
        if sb.dsem is None:
            sb.dsem = self.nc.alloc_semaphore("d_%s" % sb.name)
        ins = eng.h.dma_start(out=out, in_=in_, **kw)
        ins.then_inc(sb.dsem, 16)
        sb.dcount += 16
        tok = (sb.dsem, sb.dcount, None)
        self.n_ins += 1
        self._commit(tok, reads, writes)
        return tok

    def wait_all(self, eng, bufs):
        for b in bufs:
            for tok in b.writers + b.readers:
                self._wait(eng, tok)


PV_GPRE, PV_GPOST, PV_GHEAD, PV_GCONV, PV_CONVW, PV_PER = 0, 8, 16, 24, 32, 56
C_IDENT, C_ONES, C_MASK, C_INDA, C_INDB, C_TOT = 0, 128, 256, 384, 512, 1536


def build(L, NTILES, TPS, dbg=None):
    nc = bass.Bass("TRN2", target_bir_lowering=False)
    S = Sched(nc)
    NTOK = NTILES * NT

    x_d = nc.dram_tensor("x", [NTOK, 1024], F32, kind="ExternalInput").ap()
    ws_d = nc.dram_tensor("ws", [L, NSLAB, 128, 1024], F32, kind="ExternalInput").ap()
    wv_d = nc.dram_tensor("wv", [L, 4, 128, 2048], F32, kind="ExternalInput").ap()
    wo_d = nc.dram_tensor("wo", [L, 8, 128, 2048], F32, kind="ExternalInput").ap()
    wg_d = nc.dram_tensor("wg", [128, L * 64], F32, kind="ExternalInput").ap()
    pv_d = nc.dram_tensor("pv", [128, L * PV_PER], F32, kind="ExternalInput").ap()
    gb_d = nc.dram_tensor("gb", [4, L * 2], F32, kind="ExternalInput").ap()
    cst_d = nc.dram_tensor("cst", [128, C_TOT], F32, kind="ExternalInput").ap()
    y_d = nc.dram_tensor("y", [NTOK, 1024], F32, kind="ExternalOutput").ap()
    wsb_d = nc.dram_tensor("wsb", [L, NSLAB, 128, 1024], BF16).ap()
    wvb_d = nc.dram_tensor("wvb", [L, 4, 128, 2048], BF16).ap()
    wob_d = nc.dram_tensor("wob", [L, 8, 128, 2048], BF16).ap()
    dbg_d = {}
    if dbg:
        for nm, shp in dbg.items():
            dbg_d[nm] = nc.dram_tensor("dbg_" + nm, list(shp), F32, kind="ExternalOutput").ap()

    def sb(name, shape, dt=F32):
        return nc.alloc_sbuf_tensor("s_" + name, list(shape), dt)

    cF = sb("cF", [128, C_TOT]);            BcF = S.buf("cF")
    cB = sb("cB", [128, C_TOT], BF16);      BcB = S.buf("cB")
    maskb = sb("maskb", [128, 4, 128], BF16); Bmask = S.buf("mask")
    pv = sb("pv", [128, L * PV_PER]);       Bpv = S.buf("pv")
    gb = sb("gb", [4, L * 2]);              Bgb = S.buf("gb")
    nbf = sb("nbf", [4, L]);                Bnbf = S.buf("nbf")
    wgf = sb("wgf", [128, L * 64]);         Bwgf = S.buf("wgf")
    wgb = sb("wgb", [128, L * 64], BF16);   Bwgb = S.buf("wgb")

    xst = [sb("xst%d" % i, [128, 1024]) for i in range(2)]
    Bxst = [S.buf("xst%d" % i) for i in range(2)]
    ost = [sb("ost%d" % i, [128, 1024]) for i in range(2)]
    Bost = [S.buf("ost%d" % i) for i in range(2)]
    xT = sb("xT", [128, 8, NT]);            BxT = [S.buf("xT%d" % k) for k in range(8)]
    hT = sb("hT", [128, 8, NT], BF16);      BhT = [S.buf("hT%d" % k) for k in range(8)]
    NSQ = 3
    sq = [sb("sq%d" % i, [128, NT], BF16) for i in range(NSQ)]
    Bsq = [S.buf("sq%d" % i) for i in range(NSQ)]
    sd = sb("sd", [128, NT]);               Bsd = S.buf("sd")
    rstd = sb("rstd", [128, NT]);           Brstd = S.buf("rstd")
    NWS = 6
    wsl = [sb("wsl%d" % i, [128, 1024], BF16) for i in range(NWS)]
    Bwsl = [S.buf("wsl%d" % i) for i in range(NWS)]
    NW4 = 4
    w4k = [sb("w4k%d" % i, [128, 2048], BF16) for i in range(NW4)]
    Bw4k = [S.buf("w4k%d" % i) for i in range(NW4)]
    ARENA_B = max(4096 + 4096 + 4096 + NB * 4 * 257 * 2, 8 * NT * 4)
    arena = sb("arena", [128, ARENA_B // 2], BF16)
    qT = arena[:, 0:2048].rearrange("p (h t) -> p h t", h=4)
    kT = arena[:, 2048:4096].rearrange("p (h t) -> p h t", h=4)
    ktok = arena[:, 4096:6144].rearrange("p (b c) -> p b c", b=NB)
    vw = arena[:, 6144:6144 + NB * 4 * 257].rearrange("p (b h v) -> p b h v", b=NB, h=4)
    osb = arena[:, 0:8 * NT * 2].bitcast(F32).rearrange("p (k t) -> p k t", k=8)
    BqT = [S.buf("qT%d" % h) for h in range(4)]
    BkT = [S.buf("kT%d" % h) for h in range(4)]
    Bktok = [S.buf("ktok%d" % b) for b in range(NB)]
    Bvw = [S.buf("vw%d" % b) for b in range(NB)]
    Bosb = [S.buf("osb%d" % k) for k in range(8)]
    arena_m = BqT + BkT + Bktok + Bvw
    g_t = sb("g_t", [4, NT]);   Bg = S.buf("g")
    sp_t = sb("sp_t", [4, NT]); Bsp = S.buf("sp")
    nb_t = sb("nb_t", [4, NT]); Bnb = S.buf("nb")
    w_t = sb("w_t", [4, NT]);   Bw = S.buf("w")
    th_t = sb("th_t", [4, NT]); Bth = S.buf("th")
    sm4 = sb("sm4", [4, 64]);   Bsm4 = S.buf("sm4")
    mst = sb("mst", [4, L]);    Bmst = [S.buf("mst%d" % l) for l in range(L)]
    wthr = sb("wthr", [128, 48]); Bwthr = S.buf("wthr")
    Sm = [sb("Sm%d" % i, [128, 512], BF16) for i in range(2)]
    BSm = [S.buf("Sm%d" % i) for i in range(2)]
    Cst = sb("Cst", [128, L, 4, 257])
    BC = [[S.buf("C%d_%d" % (l, h)) for h in range(4)] for l in range(L)]
    Cd = sb("Cd", [128, 4, 257]);           BCd = [S.buf("Cd%d" % h) for h in range(4)]
    Cb = sb("Cb", [128, 4, 257], BF16);     BCb = [S.buf("Cb%d" % h) for h in range(4)]
    numb = sb("numb", [128, NB, 4, 256], BF16); Bnumb = [S.buf("numb%d" % b) for b in range(NB)]
    junk = sb("junk", [128, 256], BF16);    Bjunk = S.buf("junk")
    sm16 = sb("sm16", [128, 16 * 8]);       Bsm16 = S.buf("sm16")
    Bssr = S.buf("ssr"); Bdn = S.buf("dn"); Bsc = S.buf("sc")
    sz = [sb("sz%d" % i, [128, NT], BF16) for i in range(2)]
    Bsz = [S.buf("sz%d" % i) for i in range(2)]
    mixT = sb("mixT", [128, 16, NT], BF16); Bmix = [S.buf("mix%d" % e) for e in range(16)]
    ub = [sb("ub%d" % i, [128, NT], BF16) for i in range(2)];  Bub = [S.buf("ub%d" % i) for i in range(2)]
    cue = [sb("cue%d" % i, [128, NT + 2], BF16) for i in range(2)]; Bcue = [S.buf("cue%d" % i) for i in range(2)]
    Bb = [sb("Bb%d" % i, [128, NT], BF16) for i in range(2)];  BBb = [S.buf("Bb%d" % i) for i in range(2)]
    dg = [sb("dg%d" % i, [128, 3, 128], BF16) for i in range(2)]; Bdg = [S.buf("dg%d" % i) for i in range(2)]
    tails = sb("tails", [128, L, 8, 2], BF16); Btail = [[S.buf("tl%d_%d" % (l, j)) for j in range(8)] for l in range(L)]
    gsd = sb("gsd", [16, NT]);  Bgsd = S.buf("gsd")
    gr = sb("gr", [16, NT]);    Bgr = S.buf("gr")
    ghi = sb("ghi", [16, NT], BF16); Bghi = S.buf("ghi")
    glo = sb("glo", [16, NT], BF16); Bglo = S.buf("glo")
    tmp = [sb("tmp%d" % i, [128, NT]) for i in range(2)]; Btmp = [S.buf("tmp%d" % i) for i in range(2)]

    PS = [nc.alloc_psum_tensor("ps%d" % i, [128, 512], F32) for i in range(8)]
    BPS = [S.buf("ps%d" % i) for i in range(8)]
    for _b in BPS:
        _b.excl = True
    import os as _os
    pstate = {"i": int(_os.environ.get("PS0", "0")), "pinned": set()}

    def psum(pin=False):
        while True:
            i = pstate["i"]
            pstate["i"] = (i + 1) % 8
            if i not in pstate["pinned"]:
                break
        if pin:
            pstate["pinned"].add(i)
        return i

    def unpin(i):
        pstate["pinned"].discard(i)

    rr = {"ws": 0, "w4": 0, "sq": 0, "ev": 0}

    def nxt(key, n):
        i = rr[key]
        rr[key] = (i + 1) % n
        return i

    identF = cF[:, C_IDENT:C_IDENT + 128]
    identB = cB[:, C_IDENT:C_IDENT + 128]
    onesB = cB[:, C_ONES:C_ONES + 128]
    ones4 = cF[0:4, C_ONES:C_ONES + 128]
    indA = cB[:, C_INDA:C_INDA + 128].rearrange("p (j g) -> p j g", j=8)
    indB = cB[0:16, C_INDB:C_INDB + 1024].rearrange("p (j c) -> p j c", j=8)

    pe, act, dve, pool, sp = S.pe, S.act, S.dve, S.pool, S.sp
    T = nc.tensor
    A = nc.scalar
    V = nc.vector
    G = nc.gpsimd

    def pvcol(l, off):
        return pv[:, l * PV_PER + off: l * PV_PER + off + 1]

    S.dma(sp, cF[:], cst_d[:, :], BcF, writes=[BcF])
    S.dma(sp, pv[:], pv_d[:, :], Bpv, writes=[Bpv])
    S.dma(sp, gb[:], gb_d[:, :], Bgb, writes=[Bgb])
    S.dma(sp, wgf[:], wg_d[:, :], Bwgf, writes=[Bwgf])
    S.op(dve, lambda: V.tensor_copy(out=cB[:], in_=cF[:]), reads=[BcF], writes=[BcB])
    for h in range(4):
        S.op(dve, lambda h=h: V.tensor_copy(out=maskb[:, h, :], in_=cF[:, C_MASK:C_MASK + 128]),
             reads=[BcF], writes=[Bmask])
    S.op(dve, lambda: V.tensor_copy(out=wgb[:], in_=wgf[:]), reads=[Bwgf], writes=[Bwgb])
    for l in range(L):
        S.op(dve, lambda l=l: V.tensor_scalar(out=nbf[:, l:l + 1], in0=gb[:, 2 * l + 1:2 * l + 2], scalar1=-1.0,
                                               scalar2=None, op0=ALU.mult), reads=[Bgb], writes=[Bnbf])

    Bwc = [S.buf("wconv%d" % l) for l in range(L)]

    def convert_layer(l):
        for s0 in range(0, NSLAB, 8):
            S.dma(pool, wsb_d[l, s0:s0 + 8].rearrange("s p f -> (s p) f"),
                  ws_d[l, s0:s0 + 8].rearrange("s p f -> (s p) f"), Bwc[l], writes=[Bwc[l]])
        S.dma(pool, wvb_d[l].rearrange("s p f -> (s p) f"), wv_d[l].rearrange("s p f -> (s p) f"), Bwc[l],
              writes=[Bwc[l]])
        S.dma(pool, wob_d[l].rearrange("s p f -> (s p) f"), wo_d[l].rearrange("s p f -> (s p) f"), Bwc[l],
              writes=[Bwc[l]])

    for l in range(L):
        convert_layer(l)

    def evac_eng():
        i = nxt("ev", 2)
        return i

    def copy_on(which, out, in_, reads, writes, scale=None):
        if which == 0:
            if scale is None:
                return S.op(act, lambda: A.activation(out=out, in_=in_, func=AF.Copy), reads=reads, writes=writes)
            return S.op(act, lambda: A.activation(out=out, in_=in_, func=AF.Copy, scale=scale), reads=reads,
                        writes=writes)
        if scale is None:
            return S.op(dve, lambda: V.tensor_copy(out=out, in_=in_), reads=reads, writes=writes)
        return S.op(dve, lambda: V.tensor_scalar(out=out, in0=in_, scalar1=scale, scalar2=None, op0=ALU.mult),
                    reads=reads, writes=writes)

    def load_slab(l, s):
        r = nxt("ws", NWS)
        S.dma(sp, wsl[r][:], wsb_d[l, s], Bwsl[r], reads=[Bwc[l]], writes=[Bwsl[r]])
        return r

    def slab_mm(l, s, M=128):
        r = load_slab(l, s)
        b = psum()
        for k in range(8):
            S.op(pe, lambda k=k: T.matmul(PS[b][0:M, :], lhsT=wsl[r][:, k * 128:k * 128 + M], rhs=hT[:, k, :],
                                          start=(k == 0), stop=(k == 7)),
                 reads=[Bwsl[r], BhT[k]], writes=[BPS[b]])
        return b

    dbg_bufs = []

    def dump(nm, src_ap, bufs):
        if nm in dbg_d:
            t = S.buf("dbgtmp_" + nm)
            S.dma(pool, dbg_d[nm], src_ap, t, reads=bufs, writes=[t])
            dbg_bufs.append(t)

    def layer(l, ti, first_in_seq):
        bss = psum(pin=True)
        for k in range(8):
            i = nxt("sq", NSQ)
            S.op(act, lambda: A.activation(out=sq[i][:], in_=xT[:, k, :], func=AF.Square),
                 reads=[BxT[k]], writes=[Bsq[i]])
            S.op(pe, lambda: T.matmul(PS[bss][:], lhsT=onesB, rhs=sq[i][:], start=(k == 0), stop=(k == 7)),
                 reads=[Bsq[i], BcB], writes=[BPS[bss]])
        S.op(act, lambda: A.activation(out=sd[:], in_=PS[bss][:], func=AF.Sqrt, scale=1.0 / 1024, bias=EPSB),
             reads=[BPS[bss], Beps], writes=[Bsd])
        unpin(bss)
        S.op(dve, lambda: V.reciprocal(out=rstd[:], in_=sd[:]), reads=[Bsd], writes=[Brstd])
        for k in range(8):
            S.op(dve, lambda: V.scalar_tensor_tensor(out=hT[:, k, :], in0=xT[:, k, :], scalar=pvcol(l, PV_GPRE + k),
                                                     in1=rstd[:], op0=ALU.mult, op1=ALU.mult),
                 reads=[BxT[k], Brstd, Bpv], writes=[BhT[k]])
        if l == 0 and ti == 0:
            dump("hT", hT[:, 0, :], [BhT[0]])

        bi = psum()
        bf = psum()
        for (bb, c0) in ((bi, 0), (bf, 4)):
            for k in range(8):
                S.op(pe, lambda: T.matmul(PS[bb][0:4, :], lhsT=wgb[:, l * 64 + k * 8 + c0: l * 64 + k * 8 + c0 + 4],
                                          rhs=hT[:, k, :], start=(k == 0), stop=(k == 7)),
                     reads=[Bwgb, BhT[k]], writes=[BPS[bb]])
        S.op(dve, lambda: V.tensor_scalar(out=g_t[:], in0=PS[bi][0:4, :], scalar1=gb[:, 2 * l:2 * l + 1], scalar2=None,
                                          op0=ALU.add), reads=[BPS[bi], Bgb], writes=[Bg])
        S.op(act, lambda: A.activation(out=sp_t[:], in_=PS[bf][0:4, :], func=AF.Exp, scale=-1.0, bias=nbf[:, l:l + 1]),
             reads=[BPS[bf], Bnbf], writes=[Bsp])
        S.op(act, lambda: A.activation(out=sp_t[:], in_=sp_t[:], func=AF.Ln, bias=ONEB[0:4, :]), reads=[Bsp, Beps],
             writes=[Bsp])
        for blk in range(NB):
            S.op(dve, lambda: V.tensor_tensor_scan(out=nb_t[:, blk * 128:(blk + 1) * 128], data0=ones4,
                                                   data1=sp_t[:, blk * 128:(blk + 1) * 128], initial=0.0,
                                                   op0=ALU.mult, op1=ALU.add),
                 reads=[Bsp, BcF], writes=[Bnb])
        S.op(dve, lambda: V.tensor_tensor(out=g_t[:], in0=g_t[:], in1=nb_t[:], op=ALU.add), reads=[Bg, Bnb],
             writes=[Bg])
        S.op(dve, lambda: V.tensor_reduce(out=sm4[:, 0:4], in_=g_t[:].rearrange("p (b t) -> p b t", b=NB),
                                          op=ALU.max, axis=AX.X), reads=[Bg], writes=[Bsm4])
        for blk in range(NB):
            mprev = mst[:, l:l + 1] if blk == 0 else sm4[:, 12 + blk - 1:12 + blk]
            S.op(dve, lambda: V.tensor_tensor(out=sm4[:, 4 + blk:5 + blk], in0=mprev, in1=sm4[:, blk:blk + 1],
                                              op=ALU.max), reads=[Bsm4, Bmst[l]], writes=[Bsm4])
            S.op(dve, lambda: V.tensor_tensor(out=sm4[:, 8 + blk:9 + blk], in0=mprev, in1=sm4[:, 4 + blk:5 + blk],
                                              op=ALU.subtract), reads=[Bsm4, Bmst[l]], writes=[Bsm4])
            S.op(dve, lambda: V.tensor_tensor(out=sm4[:, 12 + blk:13 + blk], in0=sm4[:, 4 + blk:5 + blk],
                                              in1=nb_t[:, blk * 128 + 127:blk * 128 + 128], op=ALU.subtract),
                 reads=[Bsm4, Bnb], writes=[Bsm4])
        S.op(dve, lambda: V.tensor_copy(out=mst[:, l:l + 1], in_=sm4[:, 15:16]), reads=[Bsm4], writes=[Bmst[l]])
        S.op(dve, lambda: V.tensor_scalar(out=sm4[:, 16:20], in0=sm4[:, 4:8], scalar1=-1.0, scalar2=None,
                                          op0=ALU.mult), reads=[Bsm4], writes=[Bsm4])
        for blk in range(NB):
            sl = slice(blk * 128, (blk + 1) * 128)
            S.op(act, lambda: A.activation(out=w_t[:, sl], in_=g_t[:, sl], func=AF.Exp,
                                           bias=sm4[:, 16 + blk:17 + blk]), reads=[Bg, Bsm4], writes=[Bw])
            S.op(act, lambda: A.activation(out=th_t[:, sl], in_=nb_t[:, sl], func=AF.Exp,
                                           bias=sm4[:, 16 + blk:17 + blk]), reads=[Bnb, Bsm4], writes=[Bth])
        S.op(act, lambda: A.activation(out=sm4[:, 20:24], in_=sm4[:, 8:12], func=AF.Exp), reads=[Bsm4],
             writes=[Bsm4])
        for blk in range(NB):
            S.op(dve, lambda: V.tensor_scalar(out=sm4[:, 32 + blk * 4:36 + blk * 4], in0=cF[0:4, C_IDENT:C_IDENT + 4],
                                              scalar1=sm4[:, 20 + blk:21 + blk], scalar2=None, op0=ALU.mult),
                 reads=[Bsm4, BcF], writes=[Bsm4])
        bw = psum()
        for blk in range(NB):
            sl = slice(blk * 128, (blk + 1) * 128)
            S.op(pe, lambda: T.transpose(out=PS[bw][:, blk * 8:blk * 8 + 4], in_=w_t[:, sl],
                                         identity=cF[0:4, C_IDENT:C_IDENT + 4]), reads=[Bw, BcF], writes=[BPS[bw]])
            S.op(pe, lambda: T.transpose(out=PS[bw][:, blk * 8 + 4:blk * 8 + 8], in_=th_t[:, sl],
                                         identity=cF[0:4, C_IDENT:C_IDENT + 4]), reads=[Bth, BcF], writes=[BPS[bw]])
        S.op(pe, lambda: T.matmul(PS[bw][:, 32:48], lhsT=ones4, rhs=sm4[:, 32:48], start=True, stop=True),
             reads=[Bsm4, BcF], writes=[BPS[bw]])
        S.op(dve, lambda: V.tensor_copy(out=wthr[:, 0:48], in_=PS[bw][:, 0:48]), reads=[BPS[bw]], writes=[Bwthr])
        if l == 0 and ti == 0:
            dump("wthr", wthr[:, :], [Bwthr])

        for h in range(4):
            b = slab_mm(l, h)
            S.op(act, lambda: A.activation(out=qT[:, h, :], in_=PS[b][:], func=AF.Copy, scale=float(128 ** -0.5)),
                 reads=[BPS[b]], writes=[BqT[h]] + Bosb)
        for h in range(4):
            b = slab_mm(l, 4 + h)
            S.op(dve, lambda: V.tensor_copy(out=kT[:, h, :], in_=PS[b][:]), reads=[BPS[b]], writes=[BkT[h]] + Bosb)
        for blk in range(NB):
            b = psum()
            pb = PS[b][:].bitcast(BF16)
            for h in range(4):
                S.op(pe, lambda: T.transpose(out=pb[:, h * 128:(h + 1) * 128], in_=kT[:, h, blk * 128:(blk + 1) * 128],
                                             identity=identB), reads=[BkT[h], BcB], writes=[BPS[b]])
            copy_on(blk % 2, ktok[:, blk, :], pb[:, 0:512], [BPS[b]], [Bktok[blk]] + Bosb)
        S.op(dve, lambda: V.tensor_copy(out=vw[:, :, :, 256],
                                        in_=wthr[:, 0:32].rearrange("p (b e) -> p b e", e=8)[:, :, 0:4]),
             reads=[Bwthr], writes=Bvw + Bosb)
        for h in range(4):
            r = nxt("w4", NW4)
            S.dma(sp, w4k[r][:], wvb_d[l, h], Bw4k[r], reads=[Bwc[l]], writes=[Bw4k[r]])
            for blk in range(NB):
                b = psum()
                for k in range(8):
                    S.op(pe, lambda: T.matmul(PS[b][:, 0:256], lhsT=hT[:, k, blk * 128:(blk + 1) * 128],
                                              rhs=w4k[r][:, k * 256:(k + 1) * 256], start=(k == 0), stop=(k == 7)),
                         reads=[Bw4k[r], BhT[k]], writes=[BPS[b]])
                S.op(dve, lambda: V.tensor_scalar(out=vw[:, blk, h, 0:256], in0=PS[b][:, 0:256],
                                                  scalar1=wthr[:, blk * 8 + h:blk * 8 + h + 1], scalar2=None,
                                                  op0=ALU.mult), reads=[BPS[b], Bwthr], writes=[Bvw[blk]])

        for blk in range(NB):
            sl = slice(blk * 128, (blk + 1) * 128)
            for h in range(4):
                S.op(dve, lambda: V.tensor_scalar(out=Cd[:, h, :], in0=Cst[:, l, h, :],
                                                  scalar1=wthr[:, 32 + blk * 4 + h:33 + blk * 4 + h], scalar2=None,
                                                  op0=ALU.mult), reads=[BC[l][h], Bwthr], writes=[BCd[h]])
                S.op(act, lambda: A.activation(out=Cb[:, h, :], in_=Cd[:, h, :], func=AF.Copy), reads=[BCd[h]],
                     writes=[BCb[h]])
            bS = psum()
            for h in range(4):
                S.op(pe, lambda: T.matmul(PS[bS][:, h * 128:(h + 1) * 128], lhsT=kT[:, h, sl], rhs=qT[:, h, sl],
                                          start=True, stop=True), reads=[BkT[h], BqT[h]], writes=[BPS[bS]])
            si = blk % 2
            S.op(dve, lambda: V.tensor_tensor(out=Sm[si][:], in0=PS[bS][:], in1=maskb[:].rearrange("p h t -> p (h t)"),
                                              op=ALU.mult), reads=[BPS[bS], Bmask], writes=[BSm[si]])
            bN = [psum(), psum()]
            bD = psum()
            for h in range(4):
                on = PS[bN[h // 2]][:, (h % 2) * 256:(h % 2) * 256 + 256]
                S.op(pe, lambda: T.matmul(on, lhsT=Sm[si][:, h * 128:(h + 1) * 128], rhs=vw[:, blk, h, 0:256],
                                          start=True, stop=False), reads=[BSm[si], Bvw[blk]], writes=[BPS[bN[h // 2]]])
                S.op(pe, lambda: T.matmul(on, lhsT=qT[:, h, sl], rhs=Cb[:, h, 0:256], start=False, stop=True),
                     reads=[BqT[h], BCb[h]], writes=[BPS[bN[h // 2]]])
                S.op(pe, lambda: T.matmul(PS[bD][:, h:h + 1], lhsT=Sm[si][:, h * 128:(h + 1) * 128],
                                          rhs=vw[:, blk, h, 256:257], start=True, stop=False),
                     reads=[BSm[si], Bvw[blk]], writes=[BPS[bD]])
                S.op(pe, lambda: T.matmul(PS[bD][:, h:h + 1], lhsT=qT[:, h, sl], rhs=Cb[:, h, 256:257], start=False,
                                          stop=True), reads=[BqT[h], BCb[h]], writes=[BPS[bD]])
            bC = [psum(), psum()]
            for h in range(4):
                S.op(pe, lambda: T.matmul(PS[bC[h // 2]][:, (h % 2) * 256:(h % 2) * 256 + 256],
                                          lhsT=ktok[:, blk, h * 128:(h + 1) * 128], rhs=vw[:, blk, h, 0:256],
                                          start=True, stop=True), reads=[Bktok[blk], Bvw[blk]],
                     writes=[BPS[bC[h // 2]]])
                S.op(pe, lambda: T.matmul(PS[bD][:, 4 + h:5 + h], lhsT=ktok[:, blk, h * 128:(h + 1) * 128],
                                          rhs=vw[:, blk, h, 256:257], start=True, stop=True),
                     reads=[Bktok[blk], Bvw[blk]], writes=[BPS[bD]])
            for h in range(4):
                S.op(dve, lambda: V.tensor_tensor(out=Cst[:, l, h, 0:256], in0=Cd[:, h, 0:256],
                                                  in1=PS[bC[h // 2]][:, (h % 2) * 256:(h % 2) * 256 + 256],
                                                  op=ALU.add), reads=[BCd[h], BPS[bC[h // 2]]], writes=[BC[l][h]])
            S.op(dve, lambda: V.tensor_tensor(out=Cst[:, l, :, 256], in0=Cd[:, :, 256], in1=PS[bD][:, 4:8],
                                              op=ALU.add), reads=BCd + [BPS[bD]], writes=BC[l])
            S.op(act, lambda: A.activation(out=sm16[:, blk * 4:blk * 4 + 4], in_=PS[bD][:, 0:4], func=AF.Abs),
                 reads=[BPS[bD]], writes=[Bdn])
            S.op(dve, lambda: V.tensor_tensor(out=sm16[:, blk * 4:blk * 4 + 4], in0=sm16[:, blk * 4:blk * 4 + 4],
                                              in1=wthr[:, blk * 8 + 4:blk * 8 + 8], op=ALU.max),
                 reads=[Bdn, Bwthr], writes=[Bdn])
            for h in range(4):
                S.op(act, lambda: A.activation(out=junk[:], in_=PS[bN[h // 2]][:, (h % 2) * 256:(h % 2) * 256 + 256],
                                               func=AF.Square,
                                               accum_out=sm16[:, 16 + blk * 4 + h:17 + blk * 4 + h]),
                     reads=[BPS[bN[h // 2]]], writes=[Bjunk, Bssr])
            for pr in range(2):
                S.op(dve, lambda: V.tensor_copy(out=numb[:, blk, 2 * pr:2 * pr + 2, :].rearrange("p a v -> p (a v)"),
                                                in_=PS[bN[pr]][:]), reads=[BPS[bN[pr]]], writes=[Bnumb[blk]])
        S.op(dve, lambda: V.reciprocal(out=sm16[:, 32:48], in_=sm16[:, 0:16]), reads=[Bdn], writes=[Bsm16])
        S.op(dve, lambda: V.tensor_tensor(out=sm16[:, 48:64], in0=sm16[:, 32:48], in1=sm16[:, 32:48], op=ALU.mult),
             reads=[Bsm16], writes=[Bsm16])
        S.op(dve, lambda: V.tensor_tensor(out=sm16[:, 64:80], in0=sm16[:, 48:64], in1=sm16[:, 16:32], op=ALU.mult),
             reads=[Bsm16, Bssr], writes=[Bsm16])
        S.op(act, lambda: A.activation(out=sm16[:, 80:96], in_=sm16[:, 64:80], func=AF.Sqrt, scale=1.0 / 256,
                                       bias=EPSB), reads=[Bsm16, Beps], writes=[Bsm16])
        S.op(dve, lambda: V.reciprocal(out=sm16[:, 96:112], in_=sm16[:, 80:96]), reads=[Bsm16], writes=[Bsm16])
        S.op(dve, lambda: V.tensor_tensor(out=sm16[:, 112:128], in0=sm16[:, 96:112], in1=sm16[:, 32:48],
                                          op=ALU.mult), reads=[Bsm16], writes=[Bsc])
        for blk in range(NB):
            S.op(dve, lambda: V.tensor_tensor(out=numb[:, blk, :, :], in0=numb[:, blk, :, :],
                                              in1=sm16[:, 112 + blk * 4:116 + blk * 4].unsqueeze(2).broadcast_to(
                                                  [128, 4, 256]), op=ALU.mult),
                 reads=[Bnumb[blk], Bsc], writes=[Bnumb[blk]])
        if l == 0 and ti == 0:
            dump("sm16", sm16[:, :], [Bsm16, Bsc, Bssr, Bdn])

        for e in range(8):
            b = slab_mm(l, 8 + e)
            S.op(act, lambda: A.activation(out=mixT[:, e, :], in_=PS[b][:], func=AF.Sigmoid), reads=[BPS[b]],
                 writes=[Bmix[e]])
        for e in range(8):
            b = slab_mm(l, 16 + e)
            i = e % 2
            S.op(act, lambda: A.activation(out=sz[i][:], in_=PS[b][:], func=AF.Silu), reads=[BPS[b]], writes=[Bsz[i]])
            S.op(dve, lambda: V.tensor_tensor(out=mixT[:, e, :], in0=mixT[:, e, :], in1=sz[i][:], op=ALU.mult),
                 reads=[Bmix[e], Bsz[i]], writes=[Bmix[e]])
        for e in range(8):
            h, half = e // 2, e % 2
            b = psum()
            pb = PS[b][:].bitcast(BF16)
            for blk in range(NB):
                S.op(pe, lambda: T.transpose(out=pb[:, blk * 128:(blk + 1) * 128],
                                             in_=numb[:, blk, h, half * 128:(half + 1) * 128], identity=identB),
                     reads=[Bnumb[blk], BcB], writes=[BPS[b]])
            S.op(dve, lambda: V.scalar_tensor_tensor(out=mixT[:, e, :], in0=pb[:, 0:512],
                                                     scalar=pvcol(l, PV_GHEAD + e), in1=mixT[:, e, :], op0=ALU.mult,
                                                     op1=ALU.mult), reads=[BPS[b], Bmix[e], Bpv], writes=[Bmix[e]])
        if l == 0 and ti == 0:
            dump("ym", mixT[:, 0, :], [Bmix[0]])

        bg = psum(pin=True)
        for j in range(8):
            i = j % 2
            S.op(pool, lambda: G.tensor_copy(out=cue[i][:, 0:2], in_=tails[:, l, j, :]), reads=[Btail[l][j]],
                 writes=[Bcue[i]])
            for tap in range(3):
                S.op(pool, lambda: G.tensor_scalar(out=dg[i][:, tap, :], in0=identB,
                                                   scalar1=pvcol(l, PV_CONVW + j * 3 + tap), scalar2=None,
                                                   op0=ALU.mult), reads=[BcB, Bpv], writes=[Bdg[i]])
            bu = slab_mm(l, 24 + 4 * j + 0)
            S.op(act, lambda: A.activation(out=ub[i][:], in_=PS[bu][:], func=AF.Copy), reads=[BPS[bu]],
                 writes=[Bub[i]])
            bc = slab_mm(l, 24 + 4 * j + 1)
            S.op(dve, lambda: V.tensor_tensor(out=cue[i][:, 2:NT + 2], in0=PS[bc][:], in1=ub[i][:], op=ALU.mult),
                 reads=[BPS[bc], Bub[i]], writes=[Bcue[i]])
            S.op(pool, lambda: G.tensor_copy(out=tails[:, l, j, :], in_=cue[i][:, NT:NT + 2]), reads=[Bcue[i]],
                 writes=[Btail[l][j]])
            by = psum()
            for tap in range(3):
                S.op(pe, lambda: T.matmul(PS[by][:], lhsT=dg[i][:, tap, :], rhs=cue[i][:, tap:tap + NT],
                                          start=(tap == 0), stop=(tap == 2)), reads=[Bdg[i], Bcue[i]],
                     writes=[BPS[by]])
            bB = slab_mm(l, 24 + 4 * j + 2)
            S.op(act, lambda: A.activation(out=Bb[i][:], in_=PS[bB][:], func=AF.Copy), reads=[BPS[bB]],
                 writes=[BBb[i]])
            S.op(dve, lambda: V.tensor_tensor(out=mixT[:, 8 + j, :], in0=PS[by][:], in1=Bb[i][:], op=ALU.mult),
                 reads=[BPS[by], BBb[i]], writes=[Bmix[8 + j]])
            qi = nxt("sq", NSQ)
            S.op(act, lambda: A.activation(out=sq[qi][:], in_=mixT[:, 8 + j, :], func=AF.Square),
                 reads=[Bmix[8 + j]], writes=[Bsq[qi]])
            S.op(pe, lambda: T.matmul(PS[bg][0:16, :], lhsT=indA[:, j, :], rhs=sq[qi][:], start=(j == 0),
                                      stop=(j == 7)), reads=[Bsq[qi], BcB], writes=[BPS[bg]])
            bz = slab_mm(l, 24 + 4 * j + 3)
            S.op(act, lambda: A.activation(out=sz[i][:], in_=PS[bz][:], func=AF.Silu), reads=[BPS[bz]],
                 writes=[Bsz[i]])
            S.op(dve, lambda: V.tensor_tensor(out=mixT[:, 8 + j, :], in0=mixT[:, 8 + j, :], in1=sz[i][:],
                                              op=ALU.mult), reads=[Bmix[8 + j], Bsz[i]], writes=[Bmix[8 + j]])
        S.op(act, lambda: A.activation(out=gsd[:], in_=PS[bg][0:16, :], func=AF.Sqrt, scale=1.0 / 64,
                                       bias=EPSB[0:16, :]), reads=[BPS[bg], Beps], writes=[Bgsd])
        unpin(bg)
        S.op(dve, lambda: V.reciprocal(out=gr[:], in_=gsd[:]), reads=[Bgsd], writes=[Bgr])
        S.op(dve, lambda: V.tensor_copy(out=ghi[:], in_=gr[:]), reads=[Bgr], writes=[Bghi])
        S.op(dve, lambda: V.tensor_tensor(out=glo[:], in0=gr[:], in1=ghi[:], op=ALU.subtract), reads=[Bgr, Bghi],
             writes=[Bglo])
        for j in range(8):
            b = psum()
            S.op(pe, lambda: T.matmul(PS[b][:], lhsT=indB[:, j, :], rhs=ghi[:], start=True, stop=False),
                 reads=[Bghi, BcB], writes=[BPS[b]])
            S.op(pe, lambda: T.matmul(PS[b][:], lhsT=indB[:, j, :], rhs=glo[:], start=False, stop=True),
                 reads=[Bglo, BcB], writes=[BPS[b]])
            S.op(dve, lambda: V.scalar_tensor_tensor(out=mixT[:, 8 + j, :], in0=mixT[:, 8 + j, :],
                                                     scalar=pvcol(l, PV_GCONV + j), in1=PS[b][:], op0=ALU.mult,
                                                     op1=ALU.mult), reads=[Bmix[8 + j], BPS[b], Bpv],
                 writes=[Bmix[8 + j]])
        if l == 0 and ti == 0:
            dump("yc", mixT[:, 8, :], [Bmix[8]])

        bss = psum(pin=True)
        for dc in range(8):
            r = nxt("w4", NW4)
            S.dma(sp, w4k[r][:], wob_d[l, dc], Bw4k[r], reads=[Bwc[l]], writes=[Bw4k[r]])
            b = psum()
            for e in range(16):
                S.op(pe, lambda: T.matmul(PS[b][:], lhsT=w4k[r][:, e * 128:(e + 1) * 128], rhs=mixT[:, e, :],
                                          start=(e == 0), stop=(e == 15)), reads=[Bw4k[r], Bmix[e]],
                     writes=[BPS[b]])
            S.op(act, lambda: A.activation(out=osb[:, dc, :], in_=PS[b][:], func=AF.Copy), reads=[BPS[b]],
                 writes=[Bosb[dc]] + arena_m)
            qi = nxt("sq", NSQ)
            S.op(dve, lambda: V.tensor_tensor(out=sq[qi][:], in0=PS[b][:], in1=osb[:, dc, :], op=ALU.mult),
                 reads=[BPS[b], Bosb[dc]], writes=[Bsq[qi]])
            S.op(pe, lambda: T.matmul(PS[bss][:], lhsT=onesB, rhs=sq[qi][:], start=(dc == 0), stop=(dc == 7)),
                 reads=[Bsq[qi], BcB], writes=[BPS[bss]])
        S.op(act, lambda: A.activation(out=sd[:], in_=PS[bss][:], func=AF.Sqrt, scale=1.0 / 1024, bias=EPSB),
             reads=[BPS[bss], Beps], writes=[Bsd])
        unpin(bss)
        S.op(dve, lambda: V.reciprocal(out=rstd[:], in_=sd[:]), reads=[Bsd], writes=[Brstd])
        for k in range(8):
            i = k % 2
            S.op(dve, lambda: V.scalar_tensor_tensor(out=tmp[i][:], in0=osb[:, k, :], scalar=pvcol(l, PV_GPOST + k),
                                                     in1=rstd[:], op0=ALU.mult, op1=ALU.mult),
                 reads=[Bosb[k], Brstd, Bpv], writes=[Btmp[i]])
            S.op(dve, lambda: V.tensor_tensor(out=xT[:, k, :], in0=xT[:, k, :], in1=tmp[i][:], op=ALU.add),
                 reads=[BxT[k], Btmp[i]], writes=[BxT[k]])

    epsb = sb("epsb", [128, 2])
    Beps = S.buf("eps")
    S.op(dve, lambda: V.memset(epsb[:, 0:1], EPS), writes=[Beps])
    S.op(dve, lambda: V.memset(epsb[:, 1:2], 1.0), writes=[Beps])
    EPSB = epsb[:, 0:1]
    ONEB = epsb[:, 1:2]

    for ti in range(NTILES):
        tis = ti % TPS
        tok0 = ti * NT
        if tis == 0:
            S.op(pool, lambda: G.memset(Cst[:].rearrange("p l h v -> p (l h v)"), 0.0),
                 writes=[b for bl in BC for b in bl])
            S.op(pool, lambda: G.memset(mst[:], 0.0), writes=Bmst)
            S.op(pool, lambda: G.memset(tails[:].rearrange("p l j t -> p (l j t)"), 0.0),
                 writes=[b for bl in Btail for b in bl])
        for blk in range(NB):
            i = blk % 2
            S.dma(sp, xst[i][:], x_d[tok0 + blk * 128: tok0 + (blk + 1) * 128, :], Bxst[i], writes=[Bxst[i]])
            for half in range(2):
                b = psum()
                for kk in range(4):
                    k = half * 4 + kk
                    S.op(pe, lambda: T.transpose(out=PS[b][:, kk * 128:(kk + 1) * 128],
                                                 in_=xst[i][:, k * 128:(k + 1) * 128], identity=identF),
                         reads=[Bxst[i], BcF], writes=[BPS[b]])
                copy_on(half, xT[:, half * 4:half * 4 + 4, blk * 128:(blk + 1) * 128],
                        PS[b][:].rearrange("p (a t) -> p a t", a=4), [BPS[b]], BxT[half * 4:half * 4 + 4])
        for l in range(L):
            layer(l, ti, tis == 0)
        for blk in range(NB):
            i = blk % 2
            for half in range(2):
                b = psum()
                for kk in range(4):
                    k = half * 4 + kk
                    S.op(pe, lambda: T.transpose(out=PS[b][:, kk * 128:(kk + 1) * 128],
                                                 in_=xT[:, k, blk * 128:(blk + 1) * 128], identity=identF),
                         reads=[BxT[k], BcF], writes=[BPS[b]])
                copy_on(half, ost[i][:, half * 512:(half + 1) * 512], PS[b][:], [BPS[b]], [Bost[i]])
            S.dma(pool, y_d[tok0 + blk * 128: tok0 + (blk + 1) * 128, :], ost[i][:], Bost[i], reads=[Bost[i]])
    S.wait_all(pool, Bost)
    S.wait_all(sp, Bost + dbg_bufs)
    return nc, S


def _slab_cols():
    cols = []
    for h in range(4):
        cols.append(0 + 128 * h)
    for h in range(4):
        cols.append(512 + 128 * h)
    for e in range(8):
        cols.append(2048 + 128 * e)
    for e in range(8):
        cols.append(3072 + 128 * e)
    for j in range(8):
        cols.append(4104 + 128 * j)
        cols.append(6152 + 128 * j)
        cols.append(5128 + 128 * j)
        cols.append(7176 + 128 * j)
    return cols


def _consts():
    c = np.zeros((128, C_TOT), np.float32)
    c[:, C_IDENT:C_IDENT + 128] = np.eye(128, dtype=np.float32)
    c[:, C_ONES:C_ONES + 128] = 1.0
    s = np.arange(128)
    c[:, C_MASK:C_MASK + 128] = (s[:, None] <= s[None, :]).astype(np.float32)
    indA = np.zeros((128, 8, 16), np.float32)
    indB = np.zeros((128, 8, 128), np.float32)
    for j in range(8):
        for p in range(128):
            g = 2 * j + (1 if p >= 64 else 0)
            indA[p, j, g] = 1.0
            indB[g, j, p] = 1.0
    c[:, C_INDA:C_INDA + 128] = indA.reshape(128, 128)
    c[:, C_INDB:C_INDB + 1024] = indB.reshape(128, 1024)
    return c


def _prep_layers(layers, norm_pre, norm_post, w_in, b_igate, b_fgate, head_norm, conv_w, conv_norm, w_out):
    L = len(layers)
    cols = np.array(_slab_cols())
    colidx = (cols[:, None] + np.arange(128)[None, :])
    ws = np.empty((L, NSLAB, 128, 1024), np.float32)
    wv = np.empty((L, 4, 128, 2048), np.float32)
    wo = np.empty((L, 8, 128, 2048), np.float32)
    wg = np.empty((128, L * 64), np.float32)
    pv = np.empty((128, L * PV_PER), np.float32)
    gb = np.empty((4, L * 2), np.float32)
    for li, l in enumerate(layers):
        W = np.asarray(w_in[l]).reshape(8, 128, 8200)
        ws[li] = W[:, :, colidx].transpose(2, 1, 0, 3).reshape(NSLAB, 128, 1024)
        wv[li] = W[:, :, 1024:2048].reshape(8, 128, 4, 256).transpose(2, 1, 0, 3).reshape(4, 128, 2048)
        wo[li] = np.asarray(w_out[l]).reshape(16, 128, 8, 128).transpose(2, 1, 0, 3).reshape(8, 128, 2048)
        wg[:, li * 64:(li + 1) * 64] = W[:, :, 4096:4104].transpose(1, 0, 2).reshape(128, 64)
        o = li * PV_PER
        pv[:, o + PV_GPRE:o + PV_GPRE + 8] = np.asarray(norm_pre[l]).reshape(8, 128).T
        pv[:, o + PV_GPOST:o + PV_GPOST + 8] = np.asarray(norm_post[l]).reshape(8, 128).T
        pv[:, o + PV_GHEAD:o + PV_GHEAD + 8] = np.asarray(head_norm[l]).reshape(8, 128).T
        pv[:, o + PV_GCONV:o + PV_GCONV + 8] = np.asarray(conv_norm[l]).reshape(8, 128).T
        pv[:, o + PV_CONVW:o + PV_CONVW + 24] = np.asarray(conv_w[l]).reshape(3, 8, 128).transpose(2, 1, 0).reshape(128, 24)
        gb[:, 2 * li] = np.asarray(b_igate[l])
        gb[:, 2 * li + 1] = np.asarray(b_fgate[l])
    return dict(ws=ws, wv=wv, wo=wo, wg=wg, pv=pv, gb=gb, cst=_consts())


_PROG_CACHE = {}


def _get_prog(L, NTILES, TPS):
    key = (L, NTILES, TPS)
    if key not in _PROG_CACHE:
        _PROG_CACHE[key] = build(L, NTILES, TPS)[0]
    return _PROG_CACHE[key]


FUSED = True


def kernel(x, norm_pre, norm_post, w_in, b_igate, b_fgate, head_norm, conv_w, conv_norm, w_out):
    x = np.asarray(x, dtype=np.float32)
    Bt, Sq, D = x.shape
    per = Bt // NCORES
    TPS = Sq // NT
    NTILES = per * TPS
    xs = [np.ascontiguousarray(x[c * per:(c + 1) * per].reshape(per * Sq, D)) for c in range(NCORES)]
    groups = [list(range(4))] if FUSED else [[l] for l in range(4)]
    for layers in groups:
        prm = _prep_layers(layers, norm_pre, norm_post, w_in, b_igate, b_fgate, head_norm, conv_w, conv_norm, w_out)
        nc = _get_prog(len(layers), NTILES, TPS)
        in_maps = [dict(prm, x=xs[c]) for c in range(NCORES)]
        res = run_bass_kernel_spmd(nc, in_maps, core_ids=list(range(NCORES)))
        xs = [np.asarray(res.results[c]["y"], dtype=np.float32) for c in range(NCORES)]
    out = np.stack([xc.reshape(per, Sq, D) for xc in xs], axis=0).reshape(Bt, Sq, D)
    return out
```

```python
import numpy as np
import concourse.bass as bass
import concourse.mybir as mybir
from concourse.bass_utils import run_bass_kernel_spmd

F32 = mybir.dt.float32
BF16 = mybir.dt.bfloat16
AF = mybir.ActivationFunctionType
ALU = mybir.AluOpType
AX = mybir.AxisListType

EPS = 1e-6
NT = 512
NB = 4
NSLAB = 56
NCORES = 8
S_EPOCH = 30000


class Buf:
    __slots__ = ("name", "writers", "readers", "dsem", "dcount", "excl")

    def __init__(self, name):
        self.name = name
        self.excl = False
        self.writers = []
        self.readers = []
        self.dsem = None
        self.dcount = 0


class Eng:
    def __init__(self, S, name, h):
        self.S = S
        self.name = name
        self.h = h
        self.sems = []
        self.count = 0
        self.seen = {}

    def cur_sem(self):
        if not self.sems or self.count >= S_EPOCH:
            self.sems.append(self.S.nc.alloc_semaphore("%s_e%d" % (self.name, len(self.sems))))
            self.count = 0
        return self.sems[-1]


class Sched:
    def __init__(self, nc):
        self.nc = nc
        self.pe = Eng(self, "pe", nc.tensor)
        self.act = Eng(self, "act", nc.scalar)
        self.dve = Eng(self, "dve", nc.vector)
        self.pool = Eng(self, "pool", nc.gpsimd)
        self.sp = Eng(self, "sp", nc.sync)
        self.nbuf = 0
        self.n_ins = 0
        self.n_wait = 0

    def buf(self, name=None):
        self.nbuf += 1
        return Buf(name or "b%d" % self.nbuf)

    def _wait(self, eng, tok):
        sem, val, src = tok
        key = id(sem)
        if eng.seen.get(key, 0) >= val:
            return
        eng.h.wait_ge(sem, val)
        eng.seen[key] = val
        self.n_wait += 1

    def _deps(self, reads, writes):
        deps = []
        for b in reads:
            deps.extend(b.writers)
            if b.excl:
                deps.extend(b.readers)
        for b in writes:
            deps.extend(b.writers)
            deps.extend(b.readers)
        return deps

    def _commit(self, tok, reads, writes):
        for b in reads:
            b.readers.append(tok)
        for b in writes:
            b.writers = [tok]
            b.readers = []

    def op(self, eng, fn, reads=(), writes=()):
        for tok in self._deps(reads, writes):
            if tok[2] is eng and eng is self.pe:
                continue
            self._wait(eng, tok)
        sem = eng.cur_sem()
        ins = fn()
        ins.then_inc(sem, 1)
        eng.count += 1
        tok = (sem, eng.count, eng)
        self.n_ins += 1
        self._commit(tok, reads, writes)
        return tok

    def dma(self, eng, out, in_, sb, reads=(), writes=(), **kw):
        for tok in self._deps(reads, writes):
            self._wait(eng, tok)
        if sb.dsem is None:
            sb.dsem = self.nc.alloc_semaphore("d_%s" % sb.name)
        ins = eng.h.dma_start(out=out, in_=in_, **kw)
        ins.then_inc(sb.dsem, 16)
        sb.dcount += 16
        tok = (sb.dsem, sb.dcount, None)
        self.n_ins += 1
        self._commit(tok, reads, writes)
        return tok

    def wait_all(self, eng, bufs):
        for b in bufs:
            for tok in b.writers + b.readers:
                self._wait(eng, tok)


PV_GPRE, PV_GPOST, PV_GHEAD, PV_GCONV, PV_CONVW, PV_PER = 0, 8, 16, 24, 32, 56
C_IDENT, C_ONES, C_MASK, C_INDA, C_INDB, C_TOT = 0, 128, 256, 384, 512, 1536


def build(L, NTILES, TPS, dbg=None):
    nc = bass.Bass("TRN2", target_bir_lowering=False)
    S = Sched(nc)
    NTOK = NTILES * NT

    x_d = nc.dram_tensor("x", [NTOK, 1024], F32, kind="ExternalInput").ap()
    ws_d = nc.dram_tensor("ws", [L, NSLAB, 128, 1024], F32, kind="ExternalInput").ap()
    wv_d = nc.dram_tensor("wv", [L, 4, 128, 2048], F32, kind="ExternalInput").ap()
    wo_d = nc.dram_tensor("wo", [L, 8, 128, 2048], F32, kind="ExternalInput").ap()
    wg_d = nc.dram_tensor("wg", [128, L * 64], F32, kind="ExternalInput").ap()
    pv_d = nc.dram_tensor("pv", [128, L * PV_PER], F32, kind="ExternalInput").ap()
    gb_d = nc.dram_tensor("gb", [4, L * 2], F32, kind="ExternalInput").ap()
    cst_d = nc.dram_tensor("cst", [128, C_TOT], F32, kind="ExternalInput").ap()
    y_d = nc.dram_tensor("y", [NTOK, 1024], F32, kind="ExternalOutput").ap()
    wsb_d = nc.dram_tensor("wsb", [L, NSLAB, 128, 1024], BF16).ap()
    wvb_d = nc.dram_tensor("wvb", [L, 4, 128, 2048], BF16).ap()
    wob_d = nc.dram_tensor("wob", [L, 8, 128, 2048], BF16).ap()
    dbg_d = {}
    if dbg:
        for nm, shp in dbg.items():
            dbg_d[nm] = nc.dram_tensor("dbg_" + nm, list(shp), F32, kind="ExternalOutput").ap()

    def sb(name, shape, dt=F32):
        return nc.alloc_sbuf_tensor("s_" + name, list(shape), dt)

    cF = sb("cF", [128, C_TOT]);            BcF = S.buf("cF")
    cB = sb("cB", [128, C_TOT], BF16);      BcB = S.buf("cB")
    maskb = sb("maskb", [128, 4, 128], BF16); Bmask = S.buf("mask")
    pv = sb("pv", [128, L * PV_PER]);       Bpv = S.buf("pv")
    gb = sb("gb", [4, L * 2]);              Bgb = S.buf("gb")
    nbf = sb("nbf", [4, L]);                Bnbf = S.buf("nbf")
    wgf = sb("wgf", [128, L * 64]);         Bwgf = S.buf("wgf")
    wgb = sb("wgb", [128, L * 64], BF16);   Bwgb = S.buf("wgb")

    xst = [sb("xst%d" % i, [128, 1024]) for i in range(2)]
    Bxst = [S.buf("xst%d" % i) for i in range(2)]
    ost = [sb("ost%d" % i, [128, 1024]) for i in range(2)]
    Bost = [S.buf("ost%d" % i) for i in range(2)]
    xT = sb("xT", [128, 8, NT]);            BxT = [S.buf("xT%d" % k) for k in range(8)]
    hT = sb("hT", [128, 8, NT], BF16);      BhT = [S.buf("hT%d" % k) for k in range(8)]
    NSQ = 3
    sq = [sb("sq%d" % i, [128, NT], BF16) for i in range(NSQ)]
    Bsq = [S.buf("sq%d" % i) for i in range(NSQ)]
    sd = sb("sd", [128, NT]);               Bsd = S.buf("sd")
    rstd = sb("rstd", [128, NT]);           Brstd = S.buf("rstd")
    NWS = 6
    wsl = [sb("wsl%d" % i, [128, 1024], BF16) for i in range(NWS)]
    Bwsl = [S.buf("wsl%d" % i) for i in range(NWS)]
    NW4 = 4
    w4k = [sb("w4k%d" % i, [128, 2048], BF16) for i in range(NW4)]
    Bw4k = [S.buf("w4k%d" % i) for i in range(NW4)]
    ARENA_B = max(4096 + 4096 + 4096 + NB * 4 * 257 * 2, 8 * NT * 4)
    arena = sb("arena", [128, ARENA_B // 2], BF16)
    qT = arena[:, 0:2048].rearrange("p (h t) -> p h t", h=4)
    kT = arena[:, 2048:4096].rearrange("p (h t) -> p h t", h=4)
    ktok = arena[:, 4096:6144].rearrange("p (b c) -> p b c", b=NB)
    vw = arena[:, 6144:6144 + NB * 4 * 257].rearrange("p (b h v) -> p b h v", b=NB, h=4)
    osb = arena[:, 0:8 * NT * 2].bitcast(F32).rearrange("p (k t) -> p k t", k=8)
    BqT = [S.buf("qT%d" % h) for h in range(4)]
    BkT = [S.buf("kT%d" % h) for h in range(4)]
    Bktok = [S.buf("ktok%d" % b) for b in range(NB)]
    Bvw = [S.buf("vw%d" % b) for b in range(NB)]
    Bosb = [S.buf("osb%d" % k) for k in range(8)]
    arena_m = BqT + BkT + Bktok + Bvw
    g_t = sb("g_t", [4, NT]);   Bg = S.buf("g")
    sp_t = sb("sp_t", [4, NT]); Bsp = S.buf("sp")
    nb_t = sb("nb_t", [4, NT]); Bnb = S.buf("nb")
    w_t = sb("w_t", [4, NT]);   Bw = S.buf("w")
    th_t = sb("th_t", [4, NT]); Bth = S.buf("th")
    sm4 = sb("sm4", [4, 64]);   Bsm4 = S.buf("sm4")
    mst = sb("mst", [4, L]);    Bmst = [S.buf("mst%d" % l) for l in range(L)]
    wthr = sb("wthr", [128, 48]); Bwthr = S.buf("wthr")
    Sm = [sb("Sm%d" % i, [128, 512], BF16) for i in range(2)]
    BSm = [S.buf("Sm%d" % i) for i in range(2)]
    Cst = sb("Cst", [128, L, 4, 257])
    BC = [[S.buf("C%d_%d" % (l, h)) for h in range(4)] for l in range(L)]
    Cd = sb("Cd", [128, 4, 257]);           BCd = [S.buf("Cd%d" % h) for h in range(4)]
    Cb = sb("Cb", [128, 4, 257], BF16);     BCb = [S.buf("Cb%d" % h) for h in range(4)]
    numb = sb("numb", [128, NB, 4, 256], BF16); Bnumb = [S.buf("numb%d" % b) for b in range(NB)]
    junk = sb("junk", [128, 256], BF16);    Bjunk = S.buf("junk")
    sm16 = sb("sm16", [128, 16 * 8]);       Bsm16 = S.buf("sm16")
    Bssr = S.buf("ssr"); Bdn = S.buf("dn"); Bsc = S.buf("sc")
    sz = [sb("sz%d" % i, [128, NT], BF16) for i in range(2)]
    Bsz = [S.buf("sz%d" % i) for i in range(2)]
    mixT = sb("mixT", [128, 16, NT], BF16); Bmix = [S.buf("mix%d" % e) for e in range(16)]
    ub = [sb("ub%d" % i, [128, NT], BF16) for i in range(2)];  Bub = [S.buf("ub%d" % i) for i in range(2)]
    cue = [sb("cue%d" % i, [128, NT + 2], BF16) for i in range(2)]; Bcue = [S.buf("cue%d" % i) for i in range(2)]
    Bb = [sb("Bb%d" % i, [128, NT], BF16) for i in range(2)];  BBb = [S.buf("Bb%d" % i) for i in range(2)]
    dg = sb("dg", [128, 8, 3, 128], BF16); Bdg = [S.buf("dg%d" % j) for j in range(8)]
    tails = sb("tails", [128, L, 8, 2], BF16); Btail = [[S.buf("tl%d_%d" % (l, j)) for j in range(8)] for l in range(L)]
    gsd = sb("gsd", [16, NT]);  Bgsd = S.buf("gsd")
    gr = sb("gr", [16, NT]);    Bgr = S.buf("gr")
    ghi = sb("ghi", [16, NT], BF16); Bghi = S.buf("ghi")
    glo = sb("glo", [16, NT], BF16); Bglo = S.buf("glo")
    tmp = [sb("tmp%d" % i, [128, NT]) for i in range(2)]; Btmp = [S.buf("tmp%d" % i) for i in range(2)]

    PS = [nc.alloc_psum_tensor("ps%d" % i, [128, 512], F32) for i in range(8)]
    BPS = [S.buf("ps%d" % i) for i in range(8)]
    for _b in BPS:
        _b.excl = True
    import os as _os
    pstate = {"i": int(_os.environ.get("PS0", "0")), "pinned": set()}

    POOLS = {'s': [0, 1, 2], 'c': [3, 4, 5, 6, 7], 'a': list(range(8))}
    pidx = {'s': 0, 'c': 0, 'a': 0}

    def psum(pool='a', pin=False):
        lst = POOLS[pool]
        while True:
            i = lst[pidx[pool] % len(lst)]
            pidx[pool] += 1
            if i not in pstate["pinned"]:
                break
        if pin:
            pstate["pinned"].add(i)
        return i

    def unpin(i):
        pstate["pinned"].discard(i)

    rr = {"ws": 0, "w4": 0, "sq": 0, "ev": 0}

    def nxt(key, n):
        i = rr[key]
        rr[key] = (i + 1) % n
        return i

    identF = cF[:, C_IDENT:C_IDENT + 128]
    identB = cB[:, C_IDENT:C_IDENT + 128]
    onesB = cB[:, C_ONES:C_ONES + 128]
    ones4 = cF[0:4, C_ONES:C_ONES + 128]
    indA = cB[:, C_INDA:C_INDA + 128].rearrange("p (j g) -> p j g", j=8)
    indB = cB[0:16, C_INDB:C_INDB + 1024].rearrange("p (j c) -> p j c", j=8)

    pe, act, dve, pool, sp = S.pe, S.act, S.dve, S.pool, S.sp
    T = nc.tensor
    A = nc.scalar
    V = nc.vector
    G = nc.gpsimd

    def pvcol(l, off):
        return pv[:, l * PV_PER + off: l * PV_PER + off + 1]

    S.dma(sp, cF[:], cst_d[:, :], BcF, writes=[BcF])
    S.dma(sp, pv[:], pv_d[:, :], Bpv, writes=[Bpv])
    S.dma(sp, gb[:], gb_d[:, :], Bgb, writes=[Bgb])
    S.dma(sp, wgf[:], wg_d[:, :], Bwgf, writes=[Bwgf])
    S.op(dve, lambda: V.tensor_copy(out=cB[:], in_=cF[:]), reads=[BcF], writes=[BcB])
    for h in range(4):
        S.op(dve, lambda h=h: V.tensor_copy(out=maskb[:, h, :], in_=cF[:, C_MASK:C_MASK + 128]),
             reads=[BcF], writes=[Bmask])
    S.op(dve, lambda: V.tensor_copy(out=wgb[:], in_=wgf[:]), reads=[Bwgf], writes=[Bwgb])
    for l in range(L):
        S.op(dve, lambda l=l: V.tensor_scalar(out=nbf[:, l:l + 1], in0=gb[:, 2 * l + 1:2 * l + 2], scalar1=-1.0,
                                               scalar2=None, op0=ALU.mult), reads=[Bgb], writes=[Bnbf])

    Bwc = [S.buf("wconv%d" % l) for l in range(L)]

    def convert_layer(l):
        for s0 in range(0, NSLAB, 8):
            S.dma(pool, wsb_d[l, s0:s0 + 8].rearrange("s p f -> (s p) f"),
                  ws_d[l, s0:s0 + 8].rearrange("s p f -> (s p) f"), Bwc[l], writes=[Bwc[l]])
        S.dma(pool, wvb_d[l].rearrange("s p f -> (s p) f"), wv_d[l].rearrange("s p f -> (s p) f"), Bwc[l],
              writes=[Bwc[l]])
        S.dma(pool, wob_d[l].rearrange("s p f -> (s p) f"), wo_d[l].rearrange("s p f -> (s p) f"), Bwc[l],
              writes=[Bwc[l]])

    for l in range(L):
        convert_layer(l)

    def evac_eng():
        i = nxt("ev", 2)
        return i

    def copy_on(which, out, in_, reads, writes, scale=None):
        if which == 0:
            if scale is None:
                return S.op(act, lambda: A.activation(out=out, in_=in_, func=AF.Copy), reads=reads, writes=writes)
            return S.op(act, lambda: A.activation(out=out, in_=in_, func=AF.Copy, scale=scale), reads=reads,
                        writes=writes)
        if scale is None:
            return S.op(dve, lambda: V.tensor_copy(out=out, in_=in_), reads=reads, writes=writes)
        return S.op(dve, lambda: V.tensor_scalar(out=out, in0=in_, scalar1=scale, scalar2=None, op0=ALU.mult),
                    reads=reads, writes=writes)

    def load_slab(l, s):
        r = nxt("ws", NWS)
        S.dma(sp, wsl[r][:], wsb_d[l, s], Bwsl[r], reads=[Bwc[l]], writes=[Bwsl[r]])
        return r

    def slab_mm(l, s, M=128):
        r = load_slab(l, s)
        b = psum('s')
        for k in range(8):
            S.op(pe, lambda k=k: T.matmul(PS[b][0:M, :], lhsT=wsl[r][:, k * 128:k * 128 + M], rhs=hT[:, k, :],
                                          start=(k == 0), stop=(k == 7)),
                 reads=[Bwsl[r], BhT[k]], writes=[BPS[b]])
        return b

    dbg_bufs = []

    def dump(nm, src_ap, bufs):
        if nm in dbg_d:
            t = S.buf("dbgtmp_" + nm)
            S.dma(pool, dbg_d[nm], src_ap, t, reads=bufs, writes=[t])
            dbg_bufs.append(t)

    def layer(l, ti, first_in_seq):
        for j in range(8):
            for tap in range(3):
                S.op(pool, lambda: G.tensor_scalar(out=dg[:, j, tap, :], in0=identB,
                                                   scalar1=pvcol(l, PV_CONVW + j * 3 + tap), scalar2=None,
                                                   op0=ALU.mult), reads=[BcB, Bpv], writes=[Bdg[j]])
        bss = psum('c', pin=True)
        for k in range(8):
            i = nxt("sq", NSQ)
            S.op(act, lambda: A.activation(out=sq[i][:], in_=xT[:, k, :], func=AF.Square),
                 reads=[BxT[k]], writes=[Bsq[i]])
            S.op(pe, lambda: T.matmul(PS[bss][:], lhsT=onesB, rhs=sq[i][:], start=(k == 0), stop=(k == 7)),
                 reads=[Bsq[i], BcB], writes=[BPS[bss]])
        S.op(act, lambda: A.activation(out=sd[:], in_=PS[bss][:], func=AF.Sqrt, scale=1.0 / 1024, bias=EPSB),
             reads=[BPS[bss], Beps], writes=[Bsd])
        unpin(bss)
        S.op(dve, lambda: V.reciprocal(out=rstd[:], in_=sd[:]), reads=[Bsd], writes=[Brstd])
        for k in range(8):
            S.op(dve, lambda: V.scalar_tensor_tensor(out=hT[:, k, :], in0=xT[:, k, :], scalar=pvcol(l, PV_GPRE + k),
                                                     in1=rstd[:], op0=ALU.mult, op1=ALU.mult),
                 reads=[BxT[k], Brstd, Bpv], writes=[BhT[k]])

        bi = psum('c')
        bf = psum('c')
        for (bb, c0) in ((bi, 0), (bf, 4)):
            for k in range(8):
                S.op(pe, lambda: T.matmul(PS[bb][0:4, :], lhsT=wgb[:, l * 64 + k * 8 + c0: l * 64 + k * 8 + c0 + 4],
                                          rhs=hT[:, k, :], start=(k == 0), stop=(k == 7)),
                     reads=[Bwgb, BhT[k]], writes=[BPS[bb]])
        S.op(dve, lambda: V.tensor_scalar(out=g_t[:], in0=PS[bi][0:4, :], scalar1=gb[:, 2 * l:2 * l + 1], scalar2=None,
                                          op0=ALU.add), reads=[BPS[bi], Bgb], writes=[Bg])
        S.op(act, lambda: A.activation(out=sp_t[:], in_=PS[bf][0:4, :], func=AF.Exp, scale=-1.0, bias=nbf[:, l:l + 1]),
             reads=[BPS[bf], Bnbf], writes=[Bsp])
        S.op(act, lambda: A.activation(out=sp_t[:], in_=sp_t[:], func=AF.Ln, bias=ONEB[0:4, :]), reads=[Bsp, Beps],
             writes=[Bsp])
        for blk in range(NB):
            S.op(dve, lambda: V.tensor_tensor_scan(out=nb_t[:, blk * 128:(blk + 1) * 128], data0=ones4,
                                                   data1=sp_t[:, blk * 128:(blk + 1) * 128], initial=0.0,
                                                   op0=ALU.mult, op1=ALU.add),
                 reads=[Bsp, BcF], writes=[Bnb])
        S.op(dve, lambda: V.tensor_tensor(out=g_t[:], in0=g_t[:], in1=nb_t[:], op=ALU.add), reads=[Bg, Bnb],
             writes=[Bg])
        S.op(dve, lambda: V.tensor_reduce(out=sm4[:, 0:4], in_=g_t[:].rearrange("p (b t) -> p b t", b=NB),
                                          op=ALU.max, axis=AX.X), reads=[Bg], writes=[Bsm4])
        for blk in range(NB):
            mprev = mst[:, l:l + 1] if blk == 0 else sm4[:, 12 + blk - 1:12 + blk]
            S.op(dve, lambda: V.tensor_tensor(out=sm4[:, 4 + blk:5 + blk], in0=mprev, in1=sm4[:, blk:blk + 1],
                                              op=ALU.max), reads=[Bsm4, Bmst[l]], writes=[Bsm4])
            S.op(dve, lambda: V.tensor_tensor(out=sm4[:, 8 + blk:9 + blk], in0=mprev, in1=sm4[:, 4 + blk:5 + blk],
                                              op=ALU.subtract), reads=[Bsm4, Bmst[l]], writes=[Bsm4])
            S.op(dve, lambda: V.tensor_tensor(out=sm4[:, 12 + blk:13 + blk], in0=sm4[:, 4 + blk:5 + blk],
                                              in1=nb_t[:, blk * 128 + 127:blk * 128 + 128], op=ALU.subtract),
                 reads=[Bsm4, Bnb], writes=[Bsm4])
        S.op(dve, lambda: V.tensor_copy(out=mst[:, l:l + 1], in_=sm4[:, 15:16]), reads=[Bsm4], writes=[Bmst[l]])
        S.op(dve, lambda: V.tensor_scalar(out=sm4[:, 16:20], in0=sm4[:, 4:8], scalar1=-1.0, scalar2=None,
                                          op0=ALU.mult), reads=[Bsm4], writes=[Bsm4])
        for blk in range(NB):
            sl = slice(blk * 128, (blk + 1) * 128)
            S.op(act, lambda: A.activation(out=w_t[:, sl], in_=g_t[:, sl], func=AF.Exp,
                                           bias=sm4[:, 16 + blk:17 + blk]), reads=[Bg, Bsm4], writes=[Bw])
            S.op(act, lambda: A.activation(out=th_t[:, sl], in_=nb_t[:, sl], func=AF.Exp,
                                           bias=sm4[:, 16 + blk:17 + blk]), reads=[Bnb, Bsm4], writes=[Bth])
        S.op(act, lambda: A.activation(out=sm4[:, 20:24], in_=sm4[:, 8:12], func=AF.Exp), reads=[Bsm4],
             writes=[Bsm4])
        for blk in range(NB):
            S.op(dve, lambda: V.tensor_scalar(out=sm4[:, 32 + blk * 4:36 + blk * 4], in0=cF[0:4, C_IDENT:C_IDENT + 4],
                                              scalar1=sm4[:, 20 + blk:21 + blk], scalar2=None, op0=ALU.mult),
                 reads=[Bsm4, BcF], writes=[Bsm4])

        for h in range(4):
            b = slab_mm(l, h)
            S.op(act, lambda: A.activation(out=qT[:, h, :], in_=PS[b][:], func=AF.Copy, scale=float(128 ** -0.5)),
                 reads=[BPS[b]], writes=[BqT[h]] + Bosb)
        for h in range(4):
            b = slab_mm(l, 4 + h)
            S.op(dve, lambda: V.tensor_copy(out=kT[:, h, :], in_=PS[b][:]), reads=[BPS[b]], writes=[BkT[h]] + Bosb)
        for blk in range(NB):
            b = psum('c')
            pb = PS[b][:].bitcast(BF16)
            for h in range(4):
                S.op(pe, lambda: T.transpose(out=pb[:, h * 128:(h + 1) * 128], in_=kT[:, h, blk * 128:(blk + 1) * 128],
                                             identity=identB), reads=[BkT[h], BcB], writes=[BPS[b]])
            copy_on(blk % 2, ktok[:, blk, :], pb[:, 0:512], [BPS[b]], [Bktok[blk]] + Bosb)
        bw = psum('c')
        for blk in range(NB):
            sl = slice(blk * 128, (blk + 1) * 128)
            S.op(pe, lambda: T.transpose(out=PS[bw][:, blk * 8:blk * 8 + 4], in_=w_t[:, sl],
                                         identity=cF[0:4, C_IDENT:C_IDENT + 4]), reads=[Bw, BcF], writes=[BPS[bw]])
            S.op(pe, lambda: T.transpose(out=PS[bw][:, blk * 8 + 4:blk * 8 + 8], in_=th_t[:, sl],
                                         identity=cF[0:4, C_IDENT:C_IDENT + 4]), reads=[Bth, BcF], writes=[BPS[bw]])
        S.op(pe, lambda: T.matmul(PS[bw][:, 32:48], lhsT=ones4, rhs=sm4[:, 32:48], start=True, stop=True),
             reads=[Bsm4, BcF], writes=[BPS[bw]])
        S.op(dve, lambda: V.tensor_copy(out=wthr[:, 0:48], in_=PS[bw][:, 0:48]), reads=[BPS[bw]], writes=[Bwthr])
        S.op(dve, lambda: V.tensor_copy(out=vw[:, :, :, 256],
                                        in_=wthr[:, 0:32].rearrange("p (b e) -> p b e", e=8)[:, :, 0:4]),
             reads=[Bwthr], writes=Bvw + Bosb)
        for h in range(4):
            r = nxt("w4", NW4)
            S.dma(sp, w4k[r][:], wvb_d[l, h], Bw4k[r], reads=[Bwc[l]], writes=[Bw4k[r]])
            for blk in range(NB):
                b = psum('s')
                for k in range(8):
                    S.op(pe, lambda: T.matmul(PS[b][:, 0:256], lhsT=hT[:, k, blk * 128:(blk + 1) * 128],
                                              rhs=w4k[r][:, k * 256:(k + 1) * 256], start=(k == 0), stop=(k == 7)),
                         reads=[Bw4k[r], BhT[k]], writes=[BPS[b]])
                S.op(dve, lambda: V.tensor_scalar(out=vw[:, blk, h, 0:256], in0=PS[b][:, 0:256],
                                                  scalar1=wthr[:, blk * 8 + h:blk * 8 + h + 1], scalar2=None,
                                                  op0=ALU.mult), reads=[BPS[b], Bwthr], writes=[Bvw[blk]])

        def slab_o(e):
            b = slab_mm(l, 8 + e)
            S.op(act, lambda: A.activation(out=mixT[:, e, :], in_=PS[b][:], func=AF.Sigmoid), reads=[BPS[b]],
                 writes=[Bmix[e]])

        def slab_z(e):
            b = slab_mm(l, 16 + e)
            i = e % 2
            S.op(act, lambda: A.activation(out=sz[i][:], in_=PS[b][:], func=AF.Silu), reads=[BPS[b]], writes=[Bsz[i]])
            S.op(dve, lambda: V.tensor_tensor(out=mixT[:, e, :], in0=mixT[:, e, :], in1=sz[i][:], op=ALU.mult),
                 reads=[Bmix[e], Bsz[i]], writes=[Bmix[e]])

        fill = [(lambda e=e: slab_o(e)) for e in range(8)] + [(lambda e=e: slab_z(e)) for e in range(8)]

        def filler(n):
            for _ in range(n):
                if fill:
                    fill.pop(0)()

        for blk in range(NB):
            sl = slice(blk * 128, (blk + 1) * 128)
            for h in range(4):
                S.op(dve, lambda: V.tensor_scalar(out=Cd[:, h, :], in0=Cst[:, l, h, :],
                                                  scalar1=wthr[:, 32 + blk * 4 + h:33 + blk * 4 + h], scalar2=None,
                                                  op0=ALU.mult), reads=[BC[l][h], Bwthr], writes=[BCd[h]])
                S.op(act, lambda: A.activation(out=Cb[:, h, :], in_=Cd[:, h, :], func=AF.Copy), reads=[BCd[h]],
                     writes=[BCb[h]])
            bS = psum('c')
            for h in range(4):
                S.op(pe, lambda: T.matmul(PS[bS][:, h * 128:(h + 1) * 128], lhsT=kT[:, h, sl], rhs=qT[:, h, sl],
                                          start=True, stop=True), reads=[BkT[h], BqT[h]], writes=[BPS[bS]])
            si = blk % 2
            S.op(dve, lambda: V.tensor_tensor(out=Sm[si][:], in0=PS[bS][:], in1=maskb[:].rearrange("p h t -> p (h t)"),
                                              op=ALU.mult), reads=[BPS[bS], Bmask], writes=[BSm[si]])
            filler(2)
            bN = [psum('c'), psum('c')]
            bD = psum('c')
            for h in range(4):
                on = PS[bN[h // 2]][:, (h % 2) * 256:(h % 2) * 256 + 256]
                S.op(pe, lambda: T.matmul(on, lhsT=Sm[si][:, h * 128:(h + 1) * 128], rhs=vw[:, blk, h, 0:256],
                                          start=True, stop=False), reads=[BSm[si], Bvw[blk]], writes=[BPS[bN[h // 2]]])
                S.op(pe, lambda: T.matmul(on, lhsT=qT[:, h, sl], rhs=Cb[:, h, 0:256], start=False, stop=True),
                     reads=[BqT[h], BCb[h]], writes=[BPS[bN[h // 2]]])
                S.op(pe, lambda: T.matmul(PS[bD][:, h:h + 1], lhsT=Sm[si][:, h * 128:(h + 1) * 128],
                                          rhs=vw[:, blk, h, 256:257], start=True, stop=False),
                     reads=[BSm[si], Bvw[blk]], writes=[BPS[bD]])
                S.op(pe, lambda: T.matmul(PS[bD][:, h:h + 1], lhsT=qT[:, h, sl], rhs=Cb[:, h, 256:257], start=False,
                                          stop=True), reads=[BqT[h], BCb[h]], writes=[BPS[bD]])
            bC = [bS, psum('c')]
            for h in range(4):
                S.op(pe, lambda: T.matmul(PS[bC[h // 2]][:, (h % 2) * 256:(h % 2) * 256 + 256],
                                          lhsT=ktok[:, blk, h * 128:(h + 1) * 128], rhs=vw[:, blk, h, 0:256],
                                          start=True, stop=True), reads=[Bktok[blk], Bvw[blk]],
                     writes=[BPS[bC[h // 2]]])
                S.op(pe, lambda: T.matmul(PS[bD][:, 4 + h:5 + h], lhsT=ktok[:, blk, h * 128:(h + 1) * 128],
                                          rhs=vw[:, blk, h, 256:257], start=True, stop=True),
                     reads=[Bktok[blk], Bvw[blk]], writes=[BPS[bD]])
            for h in range(4):
                S.op(dve, lambda: V.tensor_tensor(out=Cst[:, l, h, 0:256], in0=Cd[:, h, 0:256],
                                                  in1=PS[bC[h // 2]][:, (h % 2) * 256:(h % 2) * 256 + 256],
                                                  op=ALU.add), reads=[BCd[h], BPS[bC[h // 2]]], writes=[BC[l][h]])
            S.op(dve, lambda: V.tensor_tensor(out=Cst[:, l, :, 256], in0=Cd[:, :, 256], in1=PS[bD][:, 4:8],
                                              op=ALU.add), reads=BCd + [BPS[bD]], writes=BC[l])
            S.op(act, lambda: A.activation(out=sm16[:, blk * 4:blk * 4 + 4], in_=PS[bD][:, 0:4], func=AF.Abs),
                 reads=[BPS[bD]], writes=[Bdn])
            S.op(dve, lambda: V.tensor_tensor(out=sm16[:, blk * 4:blk * 4 + 4], in0=sm16[:, blk * 4:blk * 4 + 4],
                                              in1=wthr[:, blk * 8 + 4:blk * 8 + 8], op=ALU.max),
                 reads=[Bdn, Bwthr], writes=[Bdn])
            for h in range(4):
                S.op(act, lambda: A.activation(out=junk[:], in_=PS[bN[h // 2]][:, (h % 2) * 256:(h % 2) * 256 + 256],
                                               func=AF.Square,
                                               accum_out=sm16[:, 16 + blk * 4 + h:17 + blk * 4 + h]),
                     reads=[BPS[bN[h // 2]]], writes=[Bjunk, Bssr])
            for pr in range(2):
                S.op(dve, lambda: V.tensor_copy(out=numb[:, blk, 2 * pr:2 * pr + 2, :].rearrange("p a v -> p (a v)"),
                                                in_=PS[bN[pr]][:]), reads=[BPS[bN[pr]]], writes=[Bnumb[blk]])
            filler(2)
        S.op(dve, lambda: V.reciprocal(out=sm16[:, 32:48], in_=sm16[:, 0:16]), reads=[Bdn], writes=[Bsm16])
        S.op(dve, lambda: V.tensor_tensor(out=sm16[:, 48:64], in0=sm16[:, 32:48], in1=sm16[:, 32:48], op=ALU.mult),
             reads=[Bsm16], writes=[Bsm16])
        S.op(dve, lambda: V.tensor_tensor(out=sm16[:, 64:80], in0=sm16[:, 48:64], in1=sm16[:, 16:32], op=ALU.mult),
             reads=[Bsm16, Bssr], writes=[Bsm16])
        S.op(act, lambda: A.activation(out=sm16[:, 80:96], in_=sm16[:, 64:80], func=AF.Sqrt, scale=1.0 / 256,
                                       bias=EPSB), reads=[Bsm16, Beps], writes=[Bsm16])
        S.op(dve, lambda: V.reciprocal(out=sm16[:, 96:112], in_=sm16[:, 80:96]), reads=[Bsm16], writes=[Bsm16])
        S.op(dve, lambda: V.tensor_tensor(out=sm16[:, 112:128], in0=sm16[:, 96:112], in1=sm16[:, 32:48],
                                          op=ALU.mult), reads=[Bsm16], writes=[Bsc])
        for blk in range(NB):
            S.op(dve, lambda: V.tensor_tensor(out=numb[:, blk, :, :], in0=numb[:, blk, :, :],
                                              in1=sm16[:, 112 + blk * 4:116 + blk * 4].unsqueeze(2).broadcast_to(
                                                  [128, 4, 256]), op=ALU.mult),
                 reads=[Bnumb[blk], Bsc], writes=[Bnumb[blk]])
        filler(100)

        bg = psum('c', pin=True)

        def conv_a(j):
            i = j % 2
            S.op(pool, lambda: G.tensor_copy(out=cue[i][:, 0:2], in_=tails[:, l, j, :]), reads=[Btail[l][j]],
                 writes=[Bcue[i]])
            bu = slab_mm(l, 24 + 4 * j + 0)
            S.op(act, lambda: A.activation(out=ub[i][:], in_=PS[bu][:], func=AF.Copy), reads=[BPS[bu]],
                 writes=[Bub[i]])
            bc = slab_mm(l, 24 + 4 * j + 1)
            S.op(dve, lambda: V.tensor_tensor(out=cue[i][:, 2:NT + 2], in0=PS[bc][:], in1=ub[i][:], op=ALU.mult),
                 reads=[BPS[bc], Bub[i]], writes=[Bcue[i]])
            S.op(pool, lambda: G.tensor_copy(out=tails[:, l, j, :], in_=cue[i][:, NT:NT + 2]), reads=[Bcue[i]],
                 writes=[Btail[l][j]])
            bB = slab_mm(l, 24 + 4 * j + 2)
            S.op(act, lambda: A.activation(out=Bb[i][:], in_=PS[bB][:], func=AF.Copy), reads=[BPS[bB]],
                 writes=[BBb[i]])

        def conv_b(j):
            i = j % 2
            by = psum('c')
            for tap in range(3):
                S.op(pe, lambda: T.matmul(PS[by][:], lhsT=dg[:, j, tap, :], rhs=cue[i][:, tap:tap + NT],
                                          start=(tap == 0), stop=(tap == 2)), reads=[Bdg[j], Bcue[i]],
                     writes=[BPS[by]])
            S.op(dve, lambda: V.tensor_tensor(out=mixT[:, 8 + j, :], in0=PS[by][:], in1=Bb[i][:], op=ALU.mult),
                 reads=[BPS[by], BBb[i]], writes=[Bmix[8 + j]])
            qi = nxt("sq", NSQ)
            S.op(act, lambda: A.activation(out=sq[qi][:], in_=mixT[:, 8 + j, :], func=AF.Square),
                 reads=[Bmix[8 + j]], writes=[Bsq[qi]])
            bz = slab_mm(l, 24 + 4 * j + 3)
            S.op(act, lambda: A.activation(out=sz[i][:], in_=PS[bz][:], func=AF.Silu), reads=[BPS[bz]],
                 writes=[Bsz[i]])
            S.op(pe, lambda: T.matmul(PS[bg][0:16, :], lhsT=indA[:, j, :], rhs=sq[qi][:], start=(j == 0),
                                      stop=(j == 7)), reads=[Bsq[qi], BcB], writes=[BPS[bg]])
            S.op(dve, lambda: V.tensor_tensor(out=mixT[:, 8 + j, :], in0=mixT[:, 8 + j, :], in1=sz[i][:],
                                              op=ALU.mult), reads=[Bmix[8 + j], Bsz[i]], writes=[Bmix[8 + j]])

        def ym(e):
            h, half = e // 2, e % 2
            b = psum('c')
            pb = PS[b][:].bitcast(BF16)
            for blk in range(NB):
                S.op(pe, lambda: T.transpose(out=pb[:, blk * 128:(blk + 1) * 128],
                                             in_=numb[:, blk, h, half * 128:(half + 1) * 128], identity=identB),
                     reads=[Bnumb[blk], BcB], writes=[BPS[b]])
            S.op(dve, lambda: V.scalar_tensor_tensor(out=mixT[:, e, :], in0=pb[:, 0:512],
                                                     scalar=pvcol(l, PV_GHEAD + e), in1=mixT[:, e, :], op0=ALU.mult,
                                                     op1=ALU.mult), reads=[BPS[b], Bmix[e], Bpv], writes=[Bmix[e]])

        for j in range(8):
            conv_a(j)
            ym(j)
            conv_b(j)
        S.op(act, lambda: A.activation(out=gsd[:], in_=PS[bg][0:16, :], func=AF.Sqrt, scale=1.0 / 64,
                                       bias=EPSB[0:16, :]), reads=[BPS[bg], Beps], writes=[Bgsd])
        unpin(bg)
        S.op(dve, lambda: V.reciprocal(out=gr[:], in_=gsd[:]), reads=[Bgsd], writes=[Bgr])
        S.op(dve, lambda: V.tensor_copy(out=ghi[:], in_=gr[:]), reads=[Bgr], writes=[Bghi])
        S.op(dve, lambda: V.tensor_tensor(out=glo[:], in0=gr[:], in1=ghi[:], op=ALU.subtract), reads=[Bgr, Bghi],
             writes=[Bglo])
        for j in range(8):
            b = psum('c')
            S.op(pe, lambda: T.matmul(PS[b][:], lhsT=indB[:, j, :], rhs=ghi[:], start=True, stop=False),
                 reads=[Bghi, BcB], writes=[BPS[b]])
            S.op(pe, lambda: T.matmul(PS[b][:], lhsT=indB[:, j, :], rhs=glo[:], start=False, stop=True),
                 reads=[Bglo, BcB], writes=[BPS[b]])
            S.op(dve, lambda: V.scalar_tensor_tensor(out=mixT[:, 8 + j, :], in0=mixT[:, 8 + j, :],
                                                     scalar=pvcol(l, PV_GCONV + j), in1=PS[b][:], op0=ALU.mult,
                                                     op1=ALU.mult), reads=[Bmix[8 + j], BPS[b], Bpv],
                 writes=[Bmix[8 + j]])

        bss = psum('c', pin=True)
        for dc in range(8):
            r = nxt("w4", NW4)
            S.dma(sp, w4k[r][:], wob_d[l, dc], Bw4k[r], reads=[Bwc[l]], writes=[Bw4k[r]])
            b = psum('s')
            for e in range(16):
                S.op(pe, lambda: T.matmul(PS[b][:], lhsT=w4k[r][:, e * 128:(e + 1) * 128], rhs=mixT[:, e, :],
                                          start=(e == 0), stop=(e == 15)), reads=[Bw4k[r], Bmix[e]],
                     writes=[BPS[b]])
            S.op(act, lambda: A.activation(out=osb[:, dc, :], in_=PS[b][:], func=AF.Copy), reads=[BPS[b]],
                 writes=[Bosb[dc]] + arena_m)
            qi = nxt("sq", NSQ)
            S.op(dve, lambda: V.tensor_tensor(out=sq[qi][:], in0=PS[b][:], in1=osb[:, dc, :], op=ALU.mult),
                 reads=[BPS[b], Bosb[dc]], writes=[Bsq[qi]])
            S.op(pe, lambda: T.matmul(PS[bss][:], lhsT=onesB, rhs=sq[qi][:], start=(dc == 0), stop=(dc == 7)),
                 reads=[Bsq[qi], BcB], writes=[BPS[bss]])
        S.op(act, lambda: A.activation(out=sd[:], in_=PS[bss][:], func=AF.Sqrt, scale=1.0 / 1024, bias=EPSB),
             reads=[BPS[bss], Beps], writes=[Bsd])
        unpin(bss)
        S.op(dve, lambda: V.reciprocal(out=rstd[:], in_=sd[:]), reads=[Bsd], writes=[Brstd])
        for k in range(8):
            i = k % 2
            S.op(dve, lambda: V.scalar_tensor_tensor(out=tmp[i][:], in0=osb[:, k, :], scalar=pvcol(l, PV_GPOST + k),
                                                     in1=rstd[:], op0=ALU.mult, op1=ALU.mult),
                 reads=[Bosb[k], Brstd, Bpv], writes=[Btmp[i]])
            S.op(dve, lambda: V.tensor_tensor(out=xT[:, k, :], in0=xT[:, k, :], in1=tmp[i][:], op=ALU.add),
                 reads=[BxT[k], Btmp[i]], writes=[BxT[k]])

    epsb = sb("epsb", [128, 2])
    Beps = S.buf("eps")
    S.op(dve, lambda: V.memset(epsb[:, 0:1], EPS), writes=[Beps])
    S.op(dve, lambda: V.memset(epsb[:, 1:2], 1.0), writes=[Beps])
    EPSB = epsb[:, 0:1]
    ONEB = epsb[:, 1:2]

    for ti in range(NTILES):
        tis = ti % TPS
        tok0 = ti * NT
        if tis == 0:
            S.op(pool, lambda: G.memset(Cst[:].rearrange("p l h v -> p (l h v)"), 0.0),
                 writes=[b for bl in BC for b in bl])
            S.op(pool, lambda: G.memset(mst[:], 0.0), writes=Bmst)
            S.op(pool, lambda: G.memset(tails[:].rearrange("p l j t -> p (l j t)"), 0.0),
                 writes=[b for bl in Btail for b in bl])
        for blk in range(NB):
            i = blk % 2
            S.dma(sp, xst[i][:], x_d[tok0 + blk * 128: tok0 + (blk + 1) * 128, :], Bxst[i], writes=[Bxst[i]])
            for half in range(2):
                b = psum()
                for kk in range(4):
                    k = half * 4 + kk
                    S.op(pe, lambda: T.transpose(out=PS[b][:, kk * 128:(kk + 1) * 128],
                                                 in_=xst[i][:, k * 128:(k + 1) * 128], identity=identF),
                         reads=[Bxst[i], BcF], writes=[BPS[b]])
                copy_on(half, xT[:, half * 4:half * 4 + 4, blk * 128:(blk + 1) * 128],
                        PS[b][:].rearrange("p (a t) -> p a t", a=4), [BPS[b]], BxT[half * 4:half * 4 + 4])
        for l in range(L):
            layer(l, ti, tis == 0)
        for blk in range(NB):
            i = blk % 2
            for half in range(2):
                b = psum()
                for kk in range(4):
                    k = half * 4 + kk
                    S.op(pe, lambda: T.transpose(out=PS[b][:, kk * 128:(kk + 1) * 128],
                                                 in_=xT[:, k, blk * 128:(blk + 1) * 128], identity=identF),
                         reads=[BxT[k], BcF], writes=[BPS[b]])
                copy_on(half, ost[i][:, half * 512:(half + 1) * 512], PS[b][:], [BPS[b]], [Bost[i]])
            S.dma(pool, y_d[tok0 + blk * 128: tok0 + (blk + 1) * 128, :], ost[i][:], Bost[i], reads=[Bost[i]])
    S.wait_all(pool, Bost)
    S.wait_all(sp, Bost + dbg_bufs)
    return nc, S


def _slab_cols():
    cols = []
    for h in range(4):
        cols.append(0 + 128 * h)
    for h in range(4):
        cols.append(512 + 128 * h)
    for e in range(8):
        cols.append(2048 + 128 * e)
    for e in range(8):
        cols.append(3072 + 128 * e)
    for j in range(8):
        cols.append(4104 + 128 * j)
        cols.append(6152 + 128 * j)
        cols.append(5128 + 128 * j)
        cols.append(7176 + 128 * j)
    return cols


def _consts():
    c = np.zeros((128, C_TOT), np.float32)
    c[:, C_IDENT:C_IDENT + 128] = np.eye(128, dtype=np.float32)
    c[:, C_ONES:C_ONES + 128] = 1.0
    s = np.arange(128)
    c[:, C_MASK:C_MASK + 128] = (s[:, None] <= s[None, :]).astype(np.float32)
    indA = np.zeros((128, 8, 16), np.float32)
    indB = np.zeros((128, 8, 128), np.float32)
    for j in range(8):
        for p in range(128):
            g = 2 * j + (1 if p >= 64 else 0)
            indA[p, j, g] = 1.0
            indB[g, j, p] = 1.0
    c[:, C_INDA:C_INDA + 128] = indA.reshape(128, 128)
    c[:, C_INDB:C_INDB + 1024] = indB.reshape(128, 1024)
    return c


def _prep_layers(layers, norm_pre, norm_post, w_in, b_igate, b_fgate, head_norm, conv_w, conv_norm, w_out):
    L = len(layers)
    cols = np.array(_slab_cols())
    colidx = (cols[:, None] + np.arange(128)[None, :])
    ws = np.empty((L, NSLAB, 128, 1024), np.float32)
    wv = np.empty((L, 4, 128, 2048), np.float32)
    wo = np.empty((L, 8, 128, 2048), np.float32)
    wg = np.empty((128, L * 64), np.float32)
    pv = np.empty((128, L * PV_PER), np.float32)
    gb = np.empty((4, L * 2), np.float32)
    for li, l in enumerate(layers):
        W = np.asarray(w_in[l]).reshape(8, 128, 8200)
        ws[li] = W[:, :, colidx].transpose(2, 1, 0, 3).reshape(NSLAB, 128, 1024)
        wv[li] = W[:, :, 1024:2048].reshape(8, 128, 4, 256).transpose(2, 1, 0, 3).reshape(4, 128, 2048)
        wo[li] = np.asarray(w_out[l]).reshape(16, 128, 8, 128).transpose(2, 1, 0, 3).reshape(8, 128, 2048)
        wg[:, li * 64:(li + 1) * 64] = W[:, :, 4096:4104].transpose(1, 0, 2).reshape(128, 64)
        o = li * PV_PER
        pv[:, o + PV_GPRE:o + PV_GPRE + 8] = np.asarray(norm_pre[l]).reshape(8, 128).T
        pv[:, o + PV_GPOST:o + PV_GPOST + 8] = np.asarray(norm_post[l]).reshape(8, 128).T
        pv[:, o + PV_GHEAD:o + PV_GHEAD + 8] = np.asarray(head_norm[l]).reshape(8, 128).T
        pv[:, o + PV_GCONV:o + PV_GCONV + 8] = np.asarray(conv_norm[l]).reshape(8, 128).T
        pv[:, o + PV_CONVW:o + PV_CONVW + 24] = np.asarray(conv_w[l]).reshape(3, 8, 128).transpose(2, 1, 0).reshape(128, 24)
        gb[:, 2 * li] = np.asarray(b_igate[l])
        gb[:, 2 * li + 1] = np.asarray(b_fgate[l])
    return dict(ws=ws, wv=wv, wo=wo, wg=wg, pv=pv, gb=gb, cst=_consts())


_PROG_CACHE = {}


def _get_prog(L, NTILES, TPS):
    key = (L, NTILES, TPS)
    if key not in _PROG_CACHE:
        _PROG_CACHE[key] = build(L, NTILES, TPS)[0]
    return _PROG_CACHE[key]


FUSED = True


def kernel(x, norm_pre, norm_post, w_in, b_igate, b_fgate, head_norm, conv_w, conv_norm, w_out):
    x = np.asarray(x, dtype=np.float32)
    Bt, Sq, D = x.shape
    per = Bt // NCORES
    TPS = Sq // NT
    NTILES = per * TPS
    xs = [np.ascontiguousarray(x[c * per:(c + 1) * per].reshape(per * Sq, D)) for c in range(NCORES)]
    groups = [list(range(4))] if FUSED else [[l] for l in range(4)]
    for layers in groups:
        prm = _prep_layers(layers, norm_pre, norm_post, w_in, b_igate, b_fgate, head_norm, conv_w, conv_norm, w_out)
        nc = _get_prog(len(layers), NTILES, TPS)
        in_maps = [dict(prm, x=xs[c]) for c in range(NCORES)]
        res = run_bass_kernel_spmd(nc, in_maps, core_ids=list(range(NCORES)))
        xs = [np.asarray(res.results[c]["y"], dtype=np.float32) for c in range(NCORES)]
    out = np.stack([xc.reshape(per, Sq, D) for xc in xs], axis=0).reshape(Bt, Sq, D)
    return out
```

```python
import numpy as np
import concourse.bass as bass
import concourse.mybir as mybir
from concourse.bass_utils import run_bass_kernel_spmd

F32 = mybir.dt.float32
BF16 = mybir.dt.bfloat16
AF = mybir.ActivationFunctionType
ALU = mybir.AluOpType
AX = mybir.AxisListType

EPS = 1e-6
NT = 512
NB = 4
NSLAB = 56
NCORES = 8
S_EPOCH = 30000


class Buf:
    __slots__ = ("name", "writers", "readers", "dsem", "dcount", "excl")

    def __init__(self, name):
        self.name = name
        self.excl = False
        self.writers = []
        self.readers = []
        self.dsem = None
        self.dcount = 0


class Eng:
    def __init__(self, S, name, h):
        self.S = S
        self.name = name
        self.h = h
        self.sems = []
        self.count = 0
        self.seen = {}

    def cur_sem(self):
        if not self.sems or self.count >= S_EPOCH:
            self.sems.append(self.S.nc.alloc_semaphore("%s_e%d" % (self.name, len(self.sems))))
            self.count = 0
        return self.sems[-1]


class Sched:
    def __init__(self, nc):
        self.nc = nc
        self.pe = Eng(self, "pe", nc.tensor)
        self.act = Eng(self, "act", nc.scalar)
        self.dve = Eng(self, "dve", nc.vector)
        self.pool = Eng(self, "pool", nc.gpsimd)
        self.sp = Eng(self, "sp", nc.sync)
        self.nbuf = 0
        self.n_ins = 0
        self.n_wait = 0

    def buf(self, name=None):
        self.nbuf += 1
        return Buf(name or "b%d" % self.nbuf)

    def _wait(self, eng, tok):
        sem, val, src = tok
        key = id(sem)
        if eng.seen.get(key, 0) >= val:
            return
        eng.h.wait_ge(sem, val)
        eng.seen[key] = val
        self.n_wait += 1

    def _deps(self, reads, writes):
        deps = []
        for b in reads:
            deps.extend(b.writers)
            if b.excl:
                deps.extend(b.readers)
        for b in writes:
            deps.extend(b.writers)
            deps.extend(b.readers)
        return deps

    def _commit(self, tok, reads, writes):
        for b in reads:
            b.readers.append(tok)
        for b in writes:
            b.writers = [tok]
            b.readers = []

    def op(self, eng, fn, reads=(), writes=()):
        for tok in self._deps(reads, writes):
            if tok[2] is eng and eng is self.pe:
                continue
            self._wait(eng, tok)
        sem = eng.cur_sem()
        ins = fn()
        ins.then_inc(sem, 1)
        eng.count += 1
        tok = (sem, eng.count, eng)
        self.n_ins += 1
        self._commit(tok, reads, writes)
        return tok

    def dma(self, eng, out, in_, sb, reads=(), writes=(), **kw):
        for tok in self._deps(reads, writes):
            self._wait(eng, tok)
        if sb.dsem is None:
            sb.dsem = self.nc.alloc_semaphore("d_%s" % sb.name)
        ins = eng.h.dma_start(out=out, in_=in_, **kw)
        ins.then_inc(sb.dsem, 16)
        sb.dcount += 16
        tok = (sb.dsem, sb.dcount, None)
        self.n_ins += 1
        self._commit(tok, reads, writes)
        return tok

    def wait_all(self, eng, bufs):
        for b in bufs:
            for tok in b.writers + b.readers:
                self._wait(eng, tok)


PV_GPRE, PV_GPOST, PV_GHEAD, PV_GCONV, PV_CONVW, PV_PER = 0, 8, 16, 24, 32, 56
C_IDENT, C_ONES, C_MASK, C_INDA, C_INDB, C_TOT = 0, 128, 256, 384, 512, 1536


def build(L, NTILES, TPS, dbg=None):
    nc = bass.Bass("TRN2", target_bir_lowering=False)
    S = Sched(nc)
    NTOK = NTILES * NT

    x_d = nc.dram_tensor("x", [NTOK, 1024], F32, kind="ExternalInput").ap()
    ws_d = nc.dram_tensor("ws", [L, NSLAB, 128, 1024], F32, kind="ExternalInput").ap()
    wv_d = nc.dram_tensor("wv", [L, 4, 128, 2048], F32, kind="ExternalInput").ap()
    wo_d = nc.dram_tensor("wo", [L, 8, 128, 2048], F32, kind="ExternalInput").ap()
    wg_d = nc.dram_tensor("wg", [128, L * 64], F32, kind="ExternalInput").ap()
    pv_d = nc.dram_tensor("pv", [128, L * PV_PER], F32, kind="ExternalInput").ap()
    gb_d = nc.dram_tensor("gb", [4, L * 2], F32, kind="ExternalInput").ap()
    cst_d = nc.dram_tensor("cst", [128, C_TOT], F32, kind="ExternalInput").ap()
    y_d = nc.dram_tensor("y", [NTOK, 1024], F32, kind="ExternalOutput").ap()
    wsb_d = nc.dram_tensor("wsb", [L, NSLAB, 128, 1024], BF16).ap()
    wvb_d = nc.dram_tensor("wvb", [L, 4, 128, 2048], BF16).ap()
    wob_d = nc.dram_tensor("wob", [L, 8, 128, 2048], BF16).ap()
    dbg_d = {}
    if dbg:
        for nm, shp in dbg.items():
            dbg_d[nm] = nc.dram_tensor("dbg_" + nm, list(shp), F32, kind="ExternalOutput").ap()

    def sb(name, shape, dt=F32):
        return nc.alloc_sbuf_tensor("s_" + name, list(shape), dt)

    cF = sb("cF", [128, C_TOT]);            BcF = S.buf("cF")
    cB = sb("cB", [128, C_TOT], BF16);      BcB = S.buf("cB")
    maskb = sb("maskb", [128, 4, 128], BF16); Bmask = S.buf("mask")
    pv = sb("pv", [128, L * PV_PER]);       Bpv = S.buf("pv")
    gb = sb("gb", [4, L * 2]);              Bgb = S.buf("gb")
    nbf = sb("nbf", [4, L]);                Bnbf = S.buf("nbf")
    wgf = sb("wgf", [128, L * 64]);         Bwgf = S.buf("wgf")
    wgb = sb("wgb", [128, L * 64], BF16);   Bwgb = S.buf("wgb")

    xst = [sb("xst%d" % i, [128, 1024]) for i in range(2)]
    Bxst = [S.buf("xst%d" % i) for i in range(2)]
    ost = [sb("ost%d" % i, [128, 1024]) for i in range(2)]
    Bost = [S.buf("ost%d" % i) for i in range(2)]
    xT = sb("xT", [128, 8, NT]);            BxT = [S.buf("xT%d" % k) for k in range(8)]
    hT = sb("hT", [128, 8, NT], BF16);      BhT = [S.buf("hT%d" % k) for k in range(8)]
    NSQ = 3
    sq = [sb("sq%d" % i, [128, NT], BF16) for i in range(NSQ)]
    Bsq = [S.buf("sq%d" % i) for i in range(NSQ)]
    sd = sb("sd", [128, NT]);               Bsd = S.buf("sd")
    rstd = sb("rstd", [128, NT]);           Brstd = S.buf("rstd")
    NWS = 6
    wsl = [sb("wsl%d" % i, [128, 1024], BF16) for i in range(NWS)]
    Bwsl = [S.buf("wsl%d" % i) for i in range(NWS)]
    NW4 = 4
    w4k = [sb("w4k%d" % i, [128, 2048], BF16) for i in range(NW4)]
    Bw4k = [S.buf("w4k%d" % i) for i in range(NW4)]
    ARENA_B = max(4096 + 4096 + 4096 + NB * 4 * 257 * 2, 8 * NT * 4)
    arena = sb("arena", [128, ARENA_B // 2], BF16)
    qT = arena[:, 0:2048].rearrange("p (h t) -> p h t", h=4)
    kT = arena[:, 2048:4096].rearrange("p (h t) -> p h t", h=4)
    ktok = arena[:, 4096:6144].rearrange("p (b c) -> p b c", b=NB)
    vw = arena[:, 6144:6144 + NB * 4 * 257].rearrange("p (b h v) -> p b h v", b=NB, h=4)
    osb = arena[:, 0:8 * NT * 2].bitcast(F32).rearrange("p (k t) -> p k t", k=8)
    BqT = [S.buf("qT%d" % h) for h in range(4)]
    BkT = [S.buf("kT%d" % h) for h in range(4)]
    Bktok = [S.buf("ktok%d" % b) for b in range(NB)]
    Bvw = [S.buf("vw%d" % b) for b in range(NB)]
    Bosb = [S.buf("osb%d" % k) for k in range(8)]
    arena_m = BqT + BkT + Bktok + Bvw
    g_t = sb("g_t", [4, NT]);   Bg = S.buf("g")
    sp_t = sb("sp_t", [4, NT]); Bsp = S.buf("sp")
    nb_t = sb("nb_t", [4, NT]); Bnb = S.buf("nb")
    w_t = sb("w_t", [4, NT]);   Bw = S.buf("w")
    th_t = sb("th_t", [4, NT]); Bth = S.buf("th")
    sm4 = sb("sm4", [4, 64]);   Bsm4 = S.buf("sm4")
    mst = sb("mst", [4, L]);    Bmst = [S.buf("mst%d" % l) for l in range(L)]
    wthr = sb("wthr", [128, 48]); Bwthr = S.buf("wthr")
    Sm = [sb("Sm%d" % i, [128, 512], BF16) for i in range(2)]
    BSm = [S.buf("Sm%d" % i) for i in range(2)]
    Cst = sb("Cst", [128, L, 4, 257])
    BC = [[S.buf("C%d_%d" % (l, h)) for h in range(4)] for l in range(L)]
    Cd = sb("Cd", [128, 4, 257]);           BCd = [S.buf("Cd%d" % h) for h in range(4)]
    Cb = sb("Cb", [128, 4, 257], BF16);     BCb = [S.buf("Cb%d" % h) for h in range(4)]
    numb = sb("numb", [128, NB, 4, 256], BF16); Bnumb = [S.buf("numb%d" % b) for b in range(NB)]
    junk = sb("junk", [128, 256], BF16);    Bjunk = S.buf("junk")
    sm16 = sb("sm16", [128, 16 * 8]);       Bsm16 = S.buf("sm16")
    Bssr = S.buf("ssr"); Bdn = S.buf("dn"); Bsc = S.buf("sc")
    sz = [sb("sz%d" % i, [128, NT], BF16) for i in range(2)]
    Bsz = [S.buf("sz%d" % i) for i in range(2)]
    mixT = sb("mixT", [128, 16, NT], BF16); Bmix = [S.buf("mix%d" % e) for e in range(16)]
    ub = [sb("ub%d" % i, [128, NT], BF16) for i in range(2)];  Bub = [S.buf("ub%d" % i) for i in range(2)]
    cue = [sb("cue%d" % i, [128, NT + 2], BF16) for i in range(2)]; Bcue = [S.buf("cue%d" % i) for i in range(2)]
    Bb = [sb("Bb%d" % i, [128, NT], BF16) for i in range(2)];  BBb = [S.buf("Bb%d" % i) for i in range(2)]
    dg = sb("dg", [128, 8, 3, 128], BF16); Bdg = [S.buf("dg%d" % j) for j in range(8)]
    tails = sb("tails", [128, L, 8, 2], BF16); Btail = [[S.buf("tl%d_%d" % (l, j)) for j in range(8)] for l in range(L)]
    gsd = sb("gsd", [16, NT]);  Bgsd = S.buf("gsd")
    gr = sb("gr", [16, NT]);    Bgr = S.buf("gr")
    ghi = sb("ghi", [16, NT], BF16); Bghi = S.buf("ghi")
    glo = sb("glo", [16, NT], BF16); Bglo = S.buf("glo")
    tmp = [sb("tmp%d" % i, [128, NT]) for i in range(2)]; Btmp = [S.buf("tmp%d" % i) for i in range(2)]

    PS = [nc.alloc_psum_tensor("ps%d" % i, [128, 512], F32) for i in range(8)]
    BPS = [S.buf("ps%d" % i) for i in range(8)]
    for _b in BPS:
        _b.excl = True
    import os as _os
    pstate = {"i": int(_os.environ.get("PS0", "0")), "pinned": set()}

    POOLS = {'s': [0, 1, 2], 'c': [3, 4, 5, 6, 7], 'a': list(range(8))}
    pidx = {'s': 0, 'c': 0, 'a': 0}

    def psum(pool='a', pin=False):
        lst = POOLS[pool]
        while True:
            i = lst[pidx[pool] % len(lst)]
            pidx[pool] += 1
            if i not in pstate["pinned"]:
                break
        if pin:
            pstate["pinned"].add(i)
        return i

    def unpin(i):
        pstate["pinned"].discard(i)

    rr = {"ws": 0, "w4": 0, "sq": 0, "ev": 0}

    def nxt(key, n):
        i = rr[key]
        rr[key] = (i + 1) % n
        return i

    identF = cF[:, C_IDENT:C_IDENT + 128]
    identB = cB[:, C_IDENT:C_IDENT + 128]
    onesB = cB[:, C_ONES:C_ONES + 128]
    ones4 = cF[0:4, C_ONES:C_ONES + 128]
    indA = cB[:, C_INDA:C_INDA + 128].rearrange("p (j g) -> p j g", j=8)
    indB = cB[0:16, C_INDB:C_INDB + 1024].rearrange("p (j c) -> p j c", j=8)

    pe, act, dve, pool, sp = S.pe, S.act, S.dve, S.pool, S.sp
    T = nc.tensor
    A = nc.scalar
    V = nc.vector
    G = nc.gpsimd

    def pvcol(l, off):
        return pv[:, l * PV_PER + off: l * PV_PER + off + 1]

    S.dma(sp, cF[:], cst_d[:, :], BcF, writes=[BcF])
    S.dma(sp, pv[:], pv_d[:, :], Bpv, writes=[Bpv])
    S.dma(sp, gb[:], gb_d[:, :], Bgb, writes=[Bgb])
    S.dma(sp, wgf[:], wg_d[:, :], Bwgf, writes=[Bwgf])
    S.op(dve, lambda: V.tensor_copy(out=cB[:], in_=cF[:]), reads=[BcF], writes=[BcB])
    for h in range(4):
        S.op(dve, lambda h=h: V.tensor_copy(out=maskb[:, h, :], in_=cF[:, C_MASK:C_MASK + 128]),
             reads=[BcF], writes=[Bmask])
    S.op(dve, lambda: V.tensor_copy(out=wgb[:], in_=wgf[:]), reads=[Bwgf], writes=[Bwgb])
    for l in range(L):
        S.op(dve, lambda l=l: V.tensor_scalar(out=nbf[:, l:l + 1], in0=gb[:, 2 * l + 1:2 * l + 2], scalar1=-1.0,
                                               scalar2=None, op0=ALU.mult), reads=[Bgb], writes=[Bnbf])

    Bwc = [[S.buf("wconv%d_%d" % (l, i)) for i in range(9)] for l in range(L)]

    def convert_layer(l):
        for pi, s0 in enumerate(range(0, NSLAB, 8)):
            S.dma(pool, wsb_d[l, s0:s0 + 8].rearrange("s p f -> (s p) f"),
                  ws_d[l, s0:s0 + 8].rearrange("s p f -> (s p) f"), Bwc[l][pi], writes=[Bwc[l][pi]])
        S.dma(pool, wvb_d[l].rearrange("s p f -> (s p) f"), wv_d[l].rearrange("s p f -> (s p) f"), Bwc[l][7],
              writes=[Bwc[l][7]])
        S.dma(pool, wob_d[l].rearrange("s p f -> (s p) f"), wo_d[l].rearrange("s p f -> (s p) f"), Bwc[l][8],
              writes=[Bwc[l][8]])

    convert_layer(0)

    def evac_eng():
        i = nxt("ev", 2)
        return i

    def copy_on(which, out, in_, reads, writes, scale=None):
        if which == 0:
            if scale is None:
                return S.op(act, lambda: A.activation(out=out, in_=in_, func=AF.Copy), reads=reads, writes=writes)
            return S.op(act, lambda: A.activation(out=out, in_=in_, func=AF.Copy, scale=scale), reads=reads,
                        writes=writes)
        if scale is None:
            return S.op(dve, lambda: V.tensor_copy(out=out, in_=in_), reads=reads, writes=writes)
        return S.op(dve, lambda: V.tensor_scalar(out=out, in0=in_, scalar1=scale, scalar2=None, op0=ALU.mult),
                    reads=reads, writes=writes)

    def load_slab(l, s):
        r = nxt("ws", NWS)
        S.dma(sp, wsl[r][:], wsb_d[l, s], Bwsl[r], reads=[Bwc[l][s // 8]], writes=[Bwsl[r]])
        return r

    def slab_mm(l, s, M=128):
        r = load_slab(l, s)
        b = psum('s')
        for k in range(8):
            S.op(pe, lambda k=k: T.matmul(PS[b][0:M, :], lhsT=wsl[r][:, k * 128:k * 128 + M], rhs=hT[:, k, :],
                                          start=(k == 0), stop=(k == 7)),
                 reads=[Bwsl[r], BhT[k]], writes=[BPS[b]])
        return b

    dbg_bufs = []

    def dump(nm, src_ap, bufs):
        if nm in dbg_d:
            t = S.buf("dbgtmp_" + nm)
            S.dma(pool, dbg_d[nm], src_ap, t, reads=bufs, writes=[t])
            dbg_bufs.append(t)

    def layer(l, ti, first_in_seq):
        if ti == 0 and l + 1 < L:
            convert_layer(l + 1)
        for j in range(8):
            for tap in range(3):
                S.op(pool, lambda: G.tensor_scalar(out=dg[:, j, tap, :], in0=identB,
                                                   scalar1=pvcol(l, PV_CONVW + j * 3 + tap), scalar2=None,
                                                   op0=ALU.mult), reads=[BcB, Bpv], writes=[Bdg[j]])
        bss = psum('c', pin=True)
        for k in range(8):
            i = nxt("sq", NSQ)
            S.op(act, lambda: A.activation(out=sq[i][:], in_=xT[:, k, :], func=AF.Square),
                 reads=[BxT[k]], writes=[Bsq[i]])
            S.op(pe, lambda: T.matmul(PS[bss][:], lhsT=onesB, rhs=sq[i][:], start=(k == 0), stop=(k == 7)),
                 reads=[Bsq[i], BcB], writes=[BPS[bss]])
        S.op(act, lambda: A.activation(out=sd[:], in_=PS[bss][:], func=AF.Sqrt, scale=1.0 / 1024, bias=EPSB),
             reads=[BPS[bss], Beps], writes=[Bsd])
        S.op(dve, lambda: V.reciprocal(out=PS[bss][:], in_=sd[:]), reads=[Bsd], writes=[BPS[bss]])
        for k in range(8):
            S.op(dve, lambda: V.scalar_tensor_tensor(out=hT[:, k, :], in0=xT[:, k, :], scalar=pvcol(l, PV_GPRE + k),
                                                     in1=PS[bss][:], op0=ALU.mult, op1=ALU.mult),
                 reads=[BxT[k], BPS[bss], Bpv], writes=[BhT[k]])
        unpin(bss)

        bi = psum('c')
        bf = psum('c')
        for (bb, c0) in ((bi, 0), (bf, 4)):
            for k in range(8):
                S.op(pe, lambda: T.matmul(PS[bb][0:4, :], lhsT=wgb[:, l * 64 + k * 8 + c0: l * 64 + k * 8 + c0 + 4],
                                          rhs=hT[:, k, :], start=(k == 0), stop=(k == 7)),
                     reads=[Bwgb, BhT[k]], writes=[BPS[bb]])
        S.op(dve, lambda: V.tensor_scalar(out=g_t[:], in0=PS[bi][0:4, :], scalar1=gb[:, 2 * l:2 * l + 1], scalar2=None,
                                          op0=ALU.add), reads=[BPS[bi], Bgb], writes=[Bg])
        S.op(act, lambda: A.activation(out=sp_t[:], in_=PS[bf][0:4, :], func=AF.Exp, scale=-1.0, bias=nbf[:, l:l + 1]),
             reads=[BPS[bf], Bnbf], writes=[Bsp])
        S.op(act, lambda: A.activation(out=sp_t[:], in_=sp_t[:], func=AF.Ln, bias=ONEB[0:4, :]), reads=[Bsp, Beps],
             writes=[Bsp])
        def q_slab(h):
            b = slab_mm(l, h)
            S.op(act, lambda: A.activation(out=qT[:, h, :], in_=PS[b][:], func=AF.Copy, scale=float(128 ** -0.5)),
                 reads=[BPS[b]], writes=[BqT[h]] + Bosb)

        q_slab(0)
        for blk in range(NB):
            S.op(dve, lambda: V.tensor_tensor_scan(out=nb_t[:, blk * 128:(blk + 1) * 128], data0=ones4,
                                                   data1=sp_t[:, blk * 128:(blk + 1) * 128], initial=0.0,
                                                   op0=ALU.mult, op1=ALU.add),
                 reads=[Bsp, BcF], writes=[Bnb])
        S.op(dve, lambda: V.tensor_tensor(out=g_t[:], in0=g_t[:], in1=nb_t[:], op=ALU.add), reads=[Bg, Bnb],
             writes=[Bg])
        S.op(dve, lambda: V.tensor_reduce(out=sm4[:, 0:4], in_=g_t[:].rearrange("p (b t) -> p b t", b=NB),
                                          op=ALU.max, axis=AX.X), reads=[Bg], writes=[Bsm4])
        for blk in range(NB):
            mprev = mst[:, l:l + 1] if blk == 0 else sm4[:, 12 + blk - 1:12 + blk]
            S.op(dve, lambda: V.tensor_tensor(out=sm4[:, 4 + blk:5 + blk], in0=mprev, in1=sm4[:, blk:blk + 1],
                                              op=ALU.max), reads=[Bsm4, Bmst[l]], writes=[Bsm4])
            S.op(dve, lambda: V.tensor_tensor(out=sm4[:, 8 + blk:9 + blk], in0=mprev, in1=sm4[:, 4 + blk:5 + blk],
                                              op=ALU.subtract), reads=[Bsm4, Bmst[l]], writes=[Bsm4])
            S.op(dve, lambda: V.tensor_tensor(out=sm4[:, 12 + blk:13 + blk], in0=sm4[:, 4 + blk:5 + blk],
                                              in1=nb_t[:, blk * 128 + 127:blk * 128 + 128], op=ALU.subtract),
                 reads=[Bsm4, Bnb], writes=[Bsm4])
        S.op(dve, lambda: V.tensor_copy(out=mst[:, l:l + 1], in_=sm4[:, 15:16]), reads=[Bsm4], writes=[Bmst[l]])
        S.op(dve, lambda: V.tensor_scalar(out=sm4[:, 16:20], in0=sm4[:, 4:8], scalar1=-1.0, scalar2=None,
                                          op0=ALU.mult), reads=[Bsm4], writes=[Bsm4])
        for h in range(1, 4):
            q_slab(h)
        for blk in range(NB):
            sl = slice(blk * 128, (blk + 1) * 128)
            S.op(act, lambda: A.activation(out=w_t[:, sl], in_=g_t[:, sl], func=AF.Exp,
                                           bias=sm4[:, 16 + blk:17 + blk]), reads=[Bg, Bsm4], writes=[Bw])
            S.op(act, lambda: A.activation(out=th_t[:, sl], in_=nb_t[:, sl], func=AF.Exp,
                                           bias=sm4[:, 16 + blk:17 + blk]), reads=[Bnb, Bsm4], writes=[Bth])
        S.op(act, lambda: A.activation(out=sm4[:, 20:24], in_=sm4[:, 8:12], func=AF.Exp), reads=[Bsm4],
             writes=[Bsm4])
        for h in range(4):
            b = slab_mm(l, 4 + h)
            S.op(dve, lambda: V.tensor_copy(out=kT[:, h, :], in_=PS[b][:]), reads=[BPS[b]], writes=[BkT[h]] + Bosb)
        for blk in range(NB):
            S.op(dve, lambda: V.tensor_scalar(out=sm4[:, 32 + blk * 4:36 + blk * 4], in0=cF[0:4, C_IDENT:C_IDENT + 4],
                                              scalar1=sm4[:, 20 + blk:21 + blk], scalar2=None, op0=ALU.mult),
                 reads=[Bsm4, BcF], writes=[Bsm4])

        for blk in range(NB):
            b = psum('c')
            pb = PS[b][:].bitcast(BF16)
            for h in range(4):
                S.op(pe, lambda: T.transpose(out=pb[:, h * 128:(h + 1) * 128], in_=kT[:, h, blk * 128:(blk + 1) * 128],
                                             identity=identB), reads=[BkT[h], BcB], writes=[BPS[b]])
            copy_on(blk % 2, ktok[:, blk, :], pb[:, 0:512], [BPS[b]], [Bktok[blk]] + Bosb)
        bw = psum('c')
        for blk in range(NB):
            sl = slice(blk * 128, (blk + 1) * 128)
            S.op(pe, lambda: T.transpose(out=PS[bw][:, blk * 8:blk * 8 + 4], in_=w_t[:, sl],
                                         identity=cF[0:4, C_IDENT:C_IDENT + 4]), reads=[Bw, BcF], writes=[BPS[bw]])
            S.op(pe, lambda: T.transpose(out=PS[bw][:, blk * 8 + 4:blk * 8 + 8], in_=th_t[:, sl],
                                         identity=cF[0:4, C_IDENT:C_IDENT + 4]), reads=[Bth, BcF], writes=[BPS[bw]])
        S.op(pe, lambda: T.matmul(PS[bw][:, 32:48], lhsT=ones4, rhs=sm4[:, 32:48], start=True, stop=True),
             reads=[Bsm4, BcF], writes=[BPS[bw]])
        S.op(dve, lambda: V.tensor_copy(out=wthr[:, 0:48], in_=PS[bw][:, 0:48]), reads=[BPS[bw]], writes=[Bwthr])
        S.op(dve, lambda: V.tensor_copy(out=vw[:, :, :, 256],
                                        in_=wthr[:, 0:32].rearrange("p (b e) -> p b e", e=8)[:, :, 0:4]),
             reads=[Bwthr], writes=Bvw + Bosb)
        for h in range(4):
            r = nxt("w4", NW4)
            S.dma(sp, w4k[r][:], wvb_d[l, h], Bw4k[r], reads=[Bwc[l][7]], writes=[Bw4k[r]])
            for blk in range(NB):
                b = psum('s')
                for k in range(8):
                    S.op(pe, lambda: T.matmul(PS[b][:, 0:256], lhsT=hT[:, k, blk * 128:(blk + 1) * 128],
                                              rhs=w4k[r][:, k * 256:(k + 1) * 256], start=(k == 0), stop=(k == 7)),
                         reads=[Bw4k[r], BhT[k]], writes=[BPS[b]])
                S.op(dve, lambda: V.tensor_scalar(out=vw[:, blk, h, 0:256], in0=PS[b][:, 0:256],
                                                  scalar1=wthr[:, blk * 8 + h:blk * 8 + h + 1], scalar2=None,
                                                  op0=ALU.mult), reads=[BPS[b], Bwthr], writes=[Bvw[blk]])

        def slab_o(e):
            b = slab_mm(l, 8 + e)
            S.op(act, lambda: A.activation(out=mixT[:, e, :], in_=PS[b][:], func=AF.Sigmoid), reads=[BPS[b]],
                 writes=[Bmix[e]])

        def slab_z(e):
            b = slab_mm(l, 16 + e)
            i = e % 2
            S.op(act, lambda: A.activation(out=sz[i][:], in_=PS[b][:], func=AF.Silu), reads=[BPS[b]], writes=[Bsz[i]])
            S.op(dve, lambda: V.tensor_tensor(out=mixT[:, e, :], in0=mixT[:, e, :], in1=sz[i][:], op=ALU.mult),
                 reads=[Bmix[e], Bsz[i]], writes=[Bmix[e]])

        fill = [(lambda e=e: slab_o(e)) for e in range(8)] + [(lambda e=e: slab_z(e)) for e in range(8)]

        def filler(n):
            for _ in range(n):
                if fill:
                    fill.pop(0)()

        for blk in range(NB):
            sl = slice(blk * 128, (blk + 1) * 128)
            for h in range(4):
                S.op(dve, lambda: V.tensor_scalar(out=Cd[:, h, :], in0=Cst[:, l, h, :],
                                                  scalar1=wthr[:, 32 + blk * 4 + h:33 + blk * 4 + h], scalar2=None,
                                                  op0=ALU.mult), reads=[BC[l][h], Bwthr], writes=[BCd[h]])
                S.op(act, lambda: A.activation(out=Cb[:, h, :], in_=Cd[:, h, :], func=AF.Copy), reads=[BCd[h]],
                     writes=[BCb[h]])
            bS = psum('c')
            for h in range(4):
                S.op(pe, lambda: T.matmul(PS[bS][:, h * 128:(h + 1) * 128], lhsT=kT[:, h, sl], rhs=qT[:, h, sl],
                                          start=True, stop=True), reads=[BkT[h], BqT[h]], writes=[BPS[bS]])
            si = blk % 2
            S.op(dve, lambda: V.tensor_tensor(out=Sm[si][:], in0=PS[bS][:], in1=maskb[:].rearrange("p h t -> p (h t)"),
                                              op=ALU.mult), reads=[BPS[bS], Bmask], writes=[BSm[si]])
            filler(2)
            bN = [psum('c'), psum('c')]
            bD = psum('c')
            for h in range(4):
                on = PS[bN[h // 2]][:, (h % 2) * 256:(h % 2) * 256 + 256]
                S.op(pe, lambda: T.matmul(on, lhsT=Sm[si][:, h * 128:(h + 1) * 128], rhs=vw[:, blk, h, 0:256],
                                          start=True, stop=False), reads=[BSm[si], Bvw[blk]], writes=[BPS[bN[h // 2]]])
                S.op(pe, lambda: T.matmul(on, lhsT=qT[:, h, sl], rhs=Cb[:, h, 0:256], start=False, stop=True),
                     reads=[BqT[h], BCb[h]], writes=[BPS[bN[h // 2]]])
                S.op(pe, lambda: T.matmul(PS[bD][:, h:h + 1], lhsT=Sm[si][:, h * 128:(h + 1) * 128],
                                          rhs=vw[:, blk, h, 256:257], start=True, stop=False),
                     reads=[BSm[si], Bvw[blk]], writes=[BPS[bD]])
                S.op(pe, lambda: T.matmul(PS[bD][:, h:h + 1], lhsT=qT[:, h, sl], rhs=Cb[:, h, 256:257], start=False,
                                          stop=True), reads=[BqT[h], BCb[h]], writes=[BPS[bD]])
            bC = [bS, psum('c')]
            for h in range(4):
                S.op(pe, lambda: T.matmul(PS[bC[h // 2]][:, (h % 2) * 256:(h % 2) * 256 + 256],
                                          lhsT=ktok[:, blk, h * 128:(h + 1) * 128], rhs=vw[:, blk, h, 0:256],
                                          start=True, stop=True), reads=[Bktok[blk], Bvw[blk]],
                     writes=[BPS[bC[h // 2]]])
                S.op(pe, lambda: T.matmul(PS[bD][:, 4 + h:5 + h], lhsT=ktok[:, blk, h * 128:(h + 1) * 128],
                                          rhs=vw[:, blk, h, 256:257], start=True, stop=True),
                     reads=[Bktok[blk], Bvw[blk]], writes=[BPS[bD]])
            for h in range(4):
                S.op(dve, lambda: V.tensor_tensor(out=Cst[:, l, h, 0:256], in0=Cd[:, h, 0:256],
                                                  in1=PS[bC[h // 2]][:, (h % 2) * 256:(h % 2) * 256 + 256],
                                                  op=ALU.add), reads=[BCd[h], BPS[bC[h // 2]]], writes=[BC[l][h]])
            S.op(dve, lambda: V.tensor_tensor(out=Cst[:, l, :, 256], in0=Cd[:, :, 256], in1=PS[bD][:, 4:8],
                                              op=ALU.add), reads=BCd + [BPS[bD]], writes=BC[l])
            S.op(act, lambda: A.activation(out=sm16[:, blk * 4:blk * 4 + 4], in_=PS[bD][:, 0:4], func=AF.Abs),
                 reads=[BPS[bD]], writes=[Bdn])
            S.op(dve, lambda: V.tensor_tensor(out=sm16[:, blk * 4:blk * 4 + 4], in0=sm16[:, blk * 4:blk * 4 + 4],
                                              in1=wthr[:, blk * 8 + 4:blk * 8 + 8], op=ALU.max),
                 reads=[Bdn, Bwthr], writes=[Bdn])
            for h in range(4):
                S.op(act, lambda: A.activation(out=junk[:], in_=PS[bN[h // 2]][:, (h % 2) * 256:(h % 2) * 256 + 256],
                                               func=AF.Square,
                                               accum_out=sm16[:, 16 + blk * 4 + h:17 + blk * 4 + h]),
                     reads=[BPS[bN[h // 2]]], writes=[Bjunk, Bssr])
            for pr in range(2):
                S.op(dve, lambda: V.tensor_copy(out=numb[:, blk, 2 * pr:2 * pr + 2, :].rearrange("p a v -> p (a v)"),
                                                in_=PS[bN[pr]][:]), reads=[BPS[bN[pr]]], writes=[Bnumb[blk]])
            filler(2)
        S.op(dve, lambda: V.reciprocal(out=sm16[:, 32:48], in_=sm16[:, 0:16]), reads=[Bdn], writes=[Bsm16])
        S.op(dve, lambda: V.tensor_tensor(out=sm16[:, 48:64], in0=sm16[:, 32:48], in1=sm16[:, 32:48], op=ALU.mult),
             reads=[Bsm16], writes=[Bsm16])
        S.op(dve, lambda: V.tensor_tensor(out=sm16[:, 64:80], in0=sm16[:, 48:64], in1=sm16[:, 16:32], op=ALU.mult),
             reads=[Bsm16, Bssr], writes=[Bsm16])
        S.op(act, lambda: A.activation(out=sm16[:, 80:96], in_=sm16[:, 64:80], func=AF.Sqrt, scale=1.0 / 256,
                                       bias=EPSB), reads=[Bsm16, Beps], writes=[Bsm16])
        S.op(dve, lambda: V.reciprocal(out=sm16[:, 96:112], in_=sm16[:, 80:96]), reads=[Bsm16], writes=[Bsm16])
        S.op(dve, lambda: V.tensor_tensor(out=sm16[:, 112:128], in0=sm16[:, 96:112], in1=sm16[:, 32:48],
                                          op=ALU.mult), reads=[Bsm16], writes=[Bsc])
        for blk in range(NB):
            S.op(dve, lambda: V.tensor_tensor(out=numb[:, blk, :, :], in0=numb[:, blk, :, :],
                                              in1=sm16[:, 112 + blk * 4:116 + blk * 4].unsqueeze(2).broadcast_to(
                                                  [128, 4, 256]), op=ALU.mult),
                 reads=[Bnumb[blk], Bsc], writes=[Bnumb[blk]])
        filler(100)

        bg = psum('c', pin=True)

        def conv_a(j):
            i = j % 2
            S.op(pool, lambda: G.tensor_copy(out=cue[i][:, 0:2], in_=tails[:, l, j, :]), reads=[Btail[l][j]],
                 writes=[Bcue[i]])
            bu = slab_mm(l, 24 + 4 * j + 0)
            S.op(act, lambda: A.activation(out=ub[i][:], in_=PS[bu][:], func=AF.Copy), reads=[BPS[bu]],
                 writes=[Bub[i]])
            bc = slab_mm(l, 24 + 4 * j + 1)
            S.op(dve, lambda: V.tensor_tensor(out=cue[i][:, 2:NT + 2], in0=PS[bc][:], in1=ub[i][:], op=ALU.mult),
                 reads=[BPS[bc], Bub[i]], writes=[Bcue[i]])
            S.op(pool, lambda: G.tensor_copy(out=tails[:, l, j, :], in_=cue[i][:, NT:NT + 2]), reads=[Bcue[i]],
                 writes=[Btail[l][j]])
            bB = slab_mm(l, 24 + 4 * j + 2)
            S.op(act, lambda: A.activation(out=Bb[i][:], in_=PS[bB][:], func=AF.Copy), reads=[BPS[bB]],
                 writes=[BBb[i]])

        def conv_b(j):
            i = j % 2
            by = psum('c')
            for tap in range(3):
                S.op(pe, lambda: T.matmul(PS[by][:], lhsT=dg[:, j, tap, :], rhs=cue[i][:, tap:tap + NT],
                                          start=(tap == 0), stop=(tap == 2)), reads=[Bdg[j], Bcue[i]],
                     writes=[BPS[by]])
            S.op(dve, lambda: V.tensor_tensor(out=mixT[:, 8 + j, :], in0=PS[by][:], in1=Bb[i][:], op=ALU.mult),
                 reads=[BPS[by], BBb[i]], writes=[Bmix[8 + j]])
            qi = nxt("sq", NSQ)
            S.op(act, lambda: A.activation(out=sq[qi][:], in_=mixT[:, 8 + j, :], func=AF.Square),
                 reads=[Bmix[8 + j]], writes=[Bsq[qi]])
            bz = slab_mm(l, 24 + 4 * j + 3)
            S.op(act, lambda: A.activation(out=sz[i][:], in_=PS[bz][:], func=AF.Silu), reads=[BPS[bz]],
                 writes=[Bsz[i]])
            S.op(pe, lambda: T.matmul(PS[bg][0:16, :], lhsT=indA[:, j, :], rhs=sq[qi][:], start=(j == 0),
                                      stop=(j == 7)), reads=[Bsq[qi], BcB], writes=[BPS[bg]])
            S.op(dve, lambda: V.tensor_tensor(out=mixT[:, 8 + j, :], in0=mixT[:, 8 + j, :], in1=sz[i][:],
                                              op=ALU.mult), reads=[Bmix[8 + j], Bsz[i]], writes=[Bmix[8 + j]])

        def ym(e):
            h, half = e // 2, e % 2
            b = psum('c')
            pb = PS[b][:].bitcast(BF16)
            for blk in range(NB):
                S.op(pe, lambda: T.transpose(out=pb[:, blk * 128:(blk + 1) * 128],
                                             in_=numb[:, blk, h, half * 128:(half + 1) * 128], identity=identB),
                     reads=[Bnumb[blk], BcB], writes=[BPS[b]])
            S.op(dve, lambda: V.scalar_tensor_tensor(out=mixT[:, e, :], in0=pb[:, 0:512],
                                                     scalar=pvcol(l, PV_GHEAD + e), in1=mixT[:, e, :], op0=ALU.mult,
                                                     op1=ALU.mult), reads=[BPS[b], Bmix[e], Bpv], writes=[Bmix[e]])

        for j in range(8):
            conv_a(j)
            ym(j)
            conv_b(j)
        S.op(act, lambda: A.activation(out=gsd[:], in_=PS[bg][0:16, :], func=AF.Sqrt, scale=1.0 / 64,
                                       bias=EPSB[0:16, :]), reads=[BPS[bg], Beps], writes=[Bgsd])
        unpin(bg)
        S.op(dve, lambda: V.reciprocal(out=gr[:], in_=gsd[:]), reads=[Bgsd], writes=[Bgr])
        S.op(dve, lambda: V.tensor_copy(out=ghi[:], in_=gr[:]), reads=[Bgr], writes=[Bghi])
        S.op(dve, lambda: V.tensor_tensor(out=glo[:], in0=gr[:], in1=ghi[:], op=ALU.subtract), reads=[Bgr, Bghi],
             writes=[Bglo])
        bss = psum('c', pin=True)
        oslab = {}

        def op_load(dc):
            r = nxt("w4", NW4)
            S.dma(sp, w4k[r][:], wob_d[l, dc], Bw4k[r], reads=[Bwc[l][8]], writes=[Bw4k[r]])
            oslab[dc] = (r, psum('s'))

        def op_mm(dc, e0, e1):
            r, b = oslab[dc]
            for e in range(e0, e1):
                S.op(pe, lambda: T.matmul(PS[b][:], lhsT=w4k[r][:, e * 128:(e + 1) * 128], rhs=mixT[:, e, :],
                                          start=(e == 0), stop=(e == 15)), reads=[Bw4k[r], Bmix[e]],
                     writes=[BPS[b]])

        def op_evac(dc):
            r, b = oslab[dc]
            S.op(act, lambda: A.activation(out=osb[:, dc, :], in_=PS[b][:], func=AF.Copy), reads=[BPS[b]],
                 writes=[Bosb[dc]] + arena_m)
            qi = nxt("sq", NSQ)
            S.op(dve, lambda: V.tensor_tensor(out=sq[qi][:], in0=PS[b][:], in1=osb[:, dc, :], op=ALU.mult),
                 reads=[BPS[b], Bosb[dc]], writes=[Bsq[qi]])
            oslab[dc] = (r, b, qi)

        def op_ss(dc):
            qi = oslab[dc][2]
            S.op(pe, lambda: T.matmul(PS[bss][:], lhsT=onesB, rhs=sq[qi][:], start=(dc == 0), stop=(dc == 7)),
                 reads=[Bsq[qi], BcB], writes=[BPS[bss]])

        for dc in range(3):
            op_load(dc)
            op_mm(dc, 0, 8)
        for j in range(8):
            b = psum('c')
            S.op(pe, lambda: T.matmul(PS[b][:], lhsT=indB[:, j, :], rhs=ghi[:], start=True, stop=False),
                 reads=[Bghi, BcB], writes=[BPS[b]])
            S.op(pe, lambda: T.matmul(PS[b][:], lhsT=indB[:, j, :], rhs=glo[:], start=False, stop=True),
                 reads=[Bglo, BcB], writes=[BPS[b]])
            S.op(dve, lambda: V.scalar_tensor_tensor(out=mixT[:, 8 + j, :], in0=mixT[:, 8 + j, :],
                                                     scalar=pvcol(l, PV_GCONV + j), in1=PS[b][:], op0=ALU.mult,
                                                     op1=ALU.mult), reads=[Bmix[8 + j], BPS[b], Bpv],
                 writes=[Bmix[8 + j]])
        for dc in range(8):
            if dc >= 3:
                op_load(dc)
                op_mm(dc, 0, 8)
            op_mm(dc, 8, 16)
            op_evac(dc)
            if dc > 0:
                op_ss(dc - 1)
        op_ss(7)
        S.op(act, lambda: A.activation(out=sd[:], in_=PS[bss][:], func=AF.Sqrt, scale=1.0 / 1024, bias=EPSB),
             reads=[BPS[bss], Beps], writes=[Bsd])
        S.op(dve, lambda: V.reciprocal(out=PS[bss][:], in_=sd[:]), reads=[Bsd], writes=[BPS[bss]])
        for k in range(8):
            i = k % 2
            S.op(dve, lambda: V.scalar_tensor_tensor(out=tmp[i][:], in0=osb[:, k, :], scalar=pvcol(l, PV_GPOST + k),
                                                     in1=PS[bss][:], op0=ALU.mult, op1=ALU.mult),
                 reads=[Bosb[k], BPS[bss], Bpv], writes=[Btmp[i]])
            S.op(dve, lambda: V.tensor_tensor(out=xT[:, k, :], in0=xT[:, k, :], in1=tmp[i][:], op=ALU.add),
                 reads=[BxT[k], Btmp[i]], writes=[BxT[k]])
        unpin(bss)

    epsb = sb("epsb", [128, 2])
    Beps = S.buf("eps")
    S.op(dve, lambda: V.memset(epsb[:, 0:1], EPS), writes=[Beps])
    S.op(dve, lambda: V.memset(epsb[:, 1:2], 1.0), writes=[Beps])
    EPSB = epsb[:, 0:1]
    ONEB = epsb[:, 1:2]

    for ti in range(NTILES):
        tis = ti % TPS
        tok0 = ti * NT
        if tis == 0:
            S.op(pool, lambda: G.memset(Cst[:].rearrange("p l h v -> p (l h v)"), 0.0),
                 writes=[b for bl in BC for b in bl])
            S.op(pool, lambda: G.memset(mst[:], 0.0), writes=Bmst)
            S.op(pool, lambda: G.memset(tails[:].rearrange("p l j t -> p (l j t)"), 0.0),
                 writes=[b for bl in Btail for b in bl])
        for blk in range(NB):
            i = blk % 2
            S.dma(sp, xst[i][:], x_d[tok0 + blk * 128: tok0 + (blk + 1) * 128, :], Bxst[i], writes=[Bxst[i]])
            for half in range(2):
                b = psum()
                for kk in range(4):
                    k = half * 4 + kk
                    S.op(pe, lambda: T.transpose(out=PS[b][:, kk * 128:(kk + 1) * 128],
                                                 in_=xst[i][:, k * 128:(k + 1) * 128], identity=identF),
                         reads=[Bxst[i], BcF], writes=[BPS[b]])
                copy_on(half, xT[:, half * 4:half * 4 + 4, blk * 128:(blk + 1) * 128],
                        PS[b][:].rearrange("p (a t) -> p a t", a=4), [BPS[b]], BxT[half * 4:half * 4 + 4])
        for l in range(L):
            layer(l, ti, tis == 0)
        for blk in range(NB):
            i = blk % 2
            for half in range(2):
                b = psum()
                for kk in range(4):
                    k = half * 4 + kk
                    S.op(pe, lambda: T.transpose(out=PS[b][:, kk * 128:(kk + 1) * 128],
                                                 in_=xT[:, k, blk * 128:(blk + 1) * 128], identity=identF),
                         reads=[BxT[k], BcF], writes=[BPS[b]])
                copy_on(half, ost[i][:, half * 512:(half + 1) * 512], PS[b][:], [BPS[b]], [Bost[i]])
            S.dma(pool, y_d[tok0 + blk * 128: tok0 + (blk + 1) * 128, :], ost[i][:], Bost[i], reads=[Bost[i]])
    S.wait_all(pool, Bost)
    S.wait_all(sp, Bost + dbg_bufs)
    return nc, S


def _slab_cols():
    cols = []
    for h in range(4):
        cols.append(0 + 128 * h)
    for h in range(4):
        cols.append(512 + 128 * h)
    for e in range(8):
        cols.append(2048 + 128 * e)
    for e in range(8):
        cols.append(3072 + 128 * e)
    for j in range(8):
        cols.append(4104 + 128 * j)
        cols.append(6152 + 128 * j)
        cols.append(5128 + 128 * j)
        cols.append(7176 + 128 * j)
    return cols


def _consts():
    c = np.zeros((128, C_TOT), np.float32)
    c[:, C_IDENT:C_IDENT + 128] = np.eye(128, dtype=np.float32)
    c[:, C_ONES:C_ONES + 128] = 1.0
    s = np.arange(128)
    c[:, C_MASK:C_MASK + 128] = (s[:, None] <= s[None, :]).astype(np.float32)
    indA = np.zeros((128, 8, 16), np.float32)
    indB = np.zeros((128, 8, 128), np.float32)
    for j in range(8):
        for p in range(128):
            g = 2 * j + (1 if p >= 64 else 0)
            indA[p, j, g] = 1.0
            indB[g, j, p] = 1.0
    c[:, C_INDA:C_INDA + 128] = indA.reshape(128, 128)
    c[:, C_INDB:C_INDB + 1024] = indB.reshape(128, 1024)
    return c


def _prep_layers(layers, norm_pre, norm_post, w_in, b_igate, b_fgate, head_norm, conv_w, conv_norm, w_out):
    L = len(layers)
    cols = np.array(_slab_cols())
    colidx = (cols[:, None] + np.arange(128)[None, :])
    ws = np.empty((L, NSLAB, 128, 1024), np.float32)
    wv = np.empty((L, 4, 128, 2048), np.float32)
    wo = np.empty((L, 8, 128, 2048), np.float32)
    wg = np.empty((128, L * 64), np.float32)
    pv = np.empty((128, L * PV_PER), np.float32)
    gb = np.empty((4, L * 2), np.float32)
    for li, l in enumerate(layers):
        W = np.asarray(w_in[l]).reshape(8, 128, 8200)
        ws[li] = W[:, :, colidx].transpose(2, 1, 0, 3).reshape(NSLAB, 128, 1024)
        wv[li] = W[:, :, 1024:2048].reshape(8, 128, 4, 256).transpose(2, 1, 0, 3).reshape(4, 128, 2048)
        wo[li] = np.asarray(w_out[l]).reshape(16, 128, 8, 128).transpose(2, 1, 0, 3).reshape(8, 128, 2048)
        wg[:, li * 64:(li + 1) * 64] = W[:, :, 4096:4104].transpose(1, 0, 2).reshape(128, 64)
        o = li * PV_PER
        pv[:, o + PV_GPRE:o + PV_GPRE + 8] = np.asarray(norm_pre[l]).reshape(8, 128).T
        pv[:, o + PV_GPOST:o + PV_GPOST + 8] = np.asarray(norm_post[l]).reshape(8, 128).T
        pv[:, o + PV_GHEAD:o + PV_GHEAD + 8] = np.asarray(head_norm[l]).reshape(8, 128).T
        pv[:, o + PV_GCONV:o + PV_GCONV + 8] = np.asarray(conv_norm[l]).reshape(8, 128).T
        pv[:, o + PV_CONVW:o + PV_CONVW + 24] = np.asarray(conv_w[l]).reshape(3, 8, 128).transpose(2, 1, 0).reshape(128, 24)
        gb[:, 2 * li] = np.asarray(b_igate[l])
        gb[:, 2 * li + 1] = np.asarray(b_fgate[l])
    return dict(ws=ws, wv=wv, wo=wo, wg=wg, pv=pv, gb=gb, cst=_consts())


_PROG_CACHE = {}


def _get_prog(L, NTILES, TPS):
    key = (L, NTILES, TPS)
    if key not in _PROG_CACHE:
        _PROG_CACHE[key] = build(L, NTILES, TPS)[0]
    return _PROG_CACHE[key]


FUSED = True


def kernel(x, norm_pre, norm_post, w_in, b_igate, b_fgate, head_norm, conv_w, conv_norm, w_out):
    x = np.asarray(x, dtype=np.float32)
    Bt, Sq, D = x.shape
    per = Bt // NCORES
    TPS = Sq // NT
    NTILES = per * TPS
    xs = [np.ascontiguousarray(x[c * per:(c + 1) * per].reshape(per * Sq, D)) for c in range(NCORES)]
    groups = [list(range(4))] if FUSED else [[l] for l in range(4)]
    for layers in groups:
        prm = _prep_layers(layers, norm_pre, norm_post, w_in, b_igate, b_fgate, head_norm, conv_w, conv_norm, w_out)
        nc = _get_prog(len(layers), NTILES, TPS)
        in_maps = [dict(prm, x=xs[c]) for c in range(NCORES)]
        res = run_bass_kernel_spmd(nc, in_maps, core_ids=list(range(NCORES)))
        xs = [np.asarray(res.results[c]["y"], dtype=np.float32) for c in range(NCORES)]
    out = np.stack([xc.reshape(per, Sq, D) for xc in xs], axis=0).reshape(Bt, Sq, D)
    return out
```

```python
import numpy as np
import concourse.bass as bass
import concourse.mybir as mybir
from concourse.bass_utils import run_bass_kernel_spmd

F32 = mybir.dt.float32
BF16 = mybir.dt.bfloat16
AF = mybir.ActivationFunctionType
ALU = mybir.AluOpType
AX = mybir.AxisListType

EPS = 1e-6
NT = 512
NB = 4
NSLAB = 56
NCORES = 8
S_EPOCH = 30000


class Buf:
    __slots__ = ("name", "writers", "readers", "dsem", "dcount", "excl")

    def __init__(self, name):
        self.name = name
        self.excl = False
        self.writers = []
        self.readers = []
        self.dsem = None
        self.dcount = 0


class Eng:
    def __init__(self, S, name, h):
        self.S = S
        self.name = name
        self.h = h
        self.sems = []
        self.count = 0
        self.seen = {}

    def cur_sem(self):
        if not self.sems or self.count >= S_EPOCH:
            self.sems.append(self.S.nc.alloc_semaphore("%s_e%d" % (self.name, len(self.sems))))
            self.count = 0
        return self.sems[-1]


class Sched:
    def __init__(self, nc):
        self.nc = nc
        self.pe = Eng(self, "pe", nc.tensor)
        self.act = Eng(self, "act", nc.scalar)
        self.dve = Eng(self, "dve", nc.vector)
        self.pool = Eng(self, "pool", nc.gpsimd)
        self.sp = Eng(self, "sp", nc.sync)
        self.nbuf = 0
        self.n_ins = 0
        self.n_wait = 0

    def buf(self, name=None):
        self.nbuf += 1
        return Buf(name or "b%d" % self.nbuf)

    def _wait(self, eng, tok):
        sem, val, src = tok
        key = id(sem)
        if eng.seen.get(key, 0) >= val:
            return
        eng.h.wait_ge(sem, val)
        eng.seen[key] = val
        self.n_wait += 1

    def _deps(self, reads, writes):
        deps = []
        for b in reads:
            deps.extend(b.writers)
            if b.excl:
                deps.extend(b.readers)
        for b in writes:
            deps.extend(b.writers)
            deps.extend(b.readers)
        return deps

    def _commit(self, tok, reads, writes):
        for b in reads:
            b.readers.append(tok)
        for b in writes:
            b.writers = [tok]
            b.readers = []

    def op(self, eng, fn, reads=(), writes=()):
        for tok in self._deps(reads, writes):
            if tok[2] is eng and eng is self.pe:
                continue
            self._wait(eng, tok)
        sem = eng.cur_sem()
        ins = fn()
        ins.then_inc(sem, 1)
        eng.count += 1
        tok = (sem, eng.count, eng)
        self.n_ins += 1
        self._commit(tok, reads, writes)
        return tok

    def dma(self, eng, out, in_, sb, reads=(), writes=(), **kw):
        for tok in self._deps(reads, writes):
            self._wait(eng, tok)
        if sb.dsem is None:
            sb.dsem = self.nc.alloc_semaphore("d_%s" % sb.name)
        ins = eng.h.dma_start(out=out, in_=in_, **kw)
        ins.then_inc(sb.dsem, 16)
        sb.dcount += 16
        tok = (sb.dsem, sb.dcount, None)
        self.n_ins += 1
        self._commit(tok, reads, writes)
        return tok

    def wait_all(self, eng, bufs):
        for b in bufs:
            for tok in b.writers + b.readers:
                self._wait(eng, tok)


PV_GPRE, PV_GPOST, PV_GHEAD, PV_GCONV, PV_CONVW, PV_PER = 0, 8, 16, 24, 32, 56
C_IDENT, C_ONES, C_MASK, C_INDA, C_INDB, C_TOT = 0, 128, 256, 384, 512, 1536


def build(L, NTILES, TPS, dbg=None):
    nc = bass.Bass("TRN2", target_bir_lowering=False)
    S = Sched(nc)
    NTOK = NTILES * NT

    x_d = nc.dram_tensor("x", [NTOK, 1024], F32, kind="ExternalInput").ap()
    ws_d = nc.dram_tensor("ws", [L, NSLAB, 128, 1024], F32, kind="ExternalInput").ap()
    wv_d = nc.dram_tensor("wv", [L, 4, 128, 2048], F32, kind="ExternalInput").ap()
    wo_d = nc.dram_tensor("wo", [L, 8, 128, 2048], F32, kind="ExternalInput").ap()
    wg_d = nc.dram_tensor("wg", [128, L * 64], F32, kind="ExternalInput").ap()
    pv_d = nc.dram_tensor("pv", [128, L * PV_PER], F32, kind="ExternalInput").ap()
    gb_d = nc.dram_tensor("gb", [4, L * 2], F32, kind="ExternalInput").ap()
    cst_d = nc.dram_tensor("cst", [128, C_TOT], F32, kind="ExternalInput").ap()
    y_d = nc.dram_tensor("y", [NTOK, 1024], F32, kind="ExternalOutput").ap()
    wsb_d = nc.dram_tensor("wsb", [L, NSLAB, 128, 1024], BF16).ap()
    wvb_d = nc.dram_tensor("wvb", [L, 4, 128, 2048], BF16).ap()
    wob_d = nc.dram_tensor("wob", [L, 8, 128, 2048], BF16).ap()
    dbg_d = {}
    if dbg:
        for nm, shp in dbg.items():
            dbg_d[nm] = nc.dram_tensor("dbg_" + nm, list(shp), F32, kind="ExternalOutput").ap()

    def sb(name, shape, dt=F32):
        return nc.alloc_sbuf_tensor("s_" + name, list(shape), dt)

    cF = sb("cF", [128, C_TOT]);            BcF = S.buf("cF")
    cB = sb("cB", [128, C_TOT], BF16);      BcB = S.buf("cB")
    maskb = sb("maskb", [128, 4, 128], BF16); Bmask = S.buf("mask")
    pv = sb("pv", [128, L * PV_PER]);       Bpv = S.buf("pv")
    gb = sb("gb", [4, L * 2]);              Bgb = S.buf("gb")
    nbf = sb("nbf", [4, L]);                Bnbf = S.buf("nbf")
    wgf = sb("wgf", [128, L * 64]);         Bwgf = S.buf("wgf")
    wgb = sb("wgb", [128, L * 64], BF16);   Bwgb = S.buf("wgb")

    xst = [sb("xst%d" % i, [128, 1024]) for i in range(2)]
    Bxst = [S.buf("xst%d" % i) for i in range(2)]
    ost = [sb("ost%d" % i, [128, 1024]) for i in range(2)]
    Bost = [S.buf("ost%d" % i) for i in range(2)]
    xT = sb("xT", [128, 8, NT]);            BxT = [S.buf("xT%d" % k) for k in range(8)]
    hT = sb("hT", [128, 8, NT], BF16);      BhT = [S.buf("hT%d" % k) for k in range(8)]
    NSQ = 3
    sq = [sb("sq%d" % i, [128, NT], BF16) for i in range(NSQ)]
    Bsq = [S.buf("sq%d" % i) for i in range(NSQ)]
    sd = sb("sd", [128, NT]);               Bsd = S.buf("sd")
    rstd = sb("rstd", [128, NT]);           Brstd = S.buf("rstd")
    NWS = 6
    wsl = [sb("wsl%d" % i, [128, 1024], BF16) for i in range(NWS)]
    Bwsl = [S.buf("wsl%d" % i) for i in range(NWS)]
    NW4 = 4
    w4k = [sb("w4k%d" % i, [128, 2048], BF16) for i in range(NW4)]
    Bw4k = [S.buf("w4k%d" % i) for i in range(NW4)]
    ARENA_B = max(4096 + 4096 + 4096 + NB * 4 * 257 * 2, 8 * NT * 4)
    arena = sb("arena", [128, ARENA_B // 2], BF16)
    qT = arena[:, 0:2048].rearrange("p (h t) -> p h t", h=4)
    kT = arena[:, 2048:4096].rearrange("p (h t) -> p h t", h=4)
    ktok = arena[:, 4096:6144].rearrange("p (b c) -> p b c", b=NB)
    vw = arena[:, 6144:6144 + NB * 4 * 257].rearrange("p (b h v) -> p b h v", b=NB, h=4)
    osb = arena[:, 0:8 * NT * 2].bitcast(F32).rearrange("p (k t) -> p k t", k=8)
    BqT = [S.buf("qT%d" % h) for h in range(4)]
    BkT = [S.buf("kT%d" % h) for h in range(4)]
    Bktok = [S.buf("ktok%d" % b) for b in range(NB)]
    Bvw = [S.buf("vw%d" % b) for b in range(NB)]
    Bosb = [S.buf("osb%d" % k) for k in range(8)]
    arena_m = BqT + BkT + Bktok + Bvw
    g_t = sb("g_t", [4, NT]);   Bg = S.buf("g")
    sp_t = sb("sp_t", [4, NT]); Bsp = S.buf("sp")
    nb_t = sb("nb_t", [4, NT]); Bnb = S.buf("nb")
    w_t = sb("w_t", [4, NT]);   Bw = S.buf("w")
    th_t = sb("th_t", [4, NT]); Bth = S.buf("th")
    sm4 = sb("sm4", [4, 64]);   Bsm4 = S.buf("sm4")
    mst = sb("mst", [4, L]);    Bmst = [S.buf("mst%d" % l) for l in range(L)]
    wthr = sb("wthr", [128, 48]); Bwthr = S.buf("wthr")
    Sm = [sb("Sm%d" % i, [128, 512], BF16) for i in range(2)]
    BSm = [S.buf("Sm%d" % i) for i in range(2)]
    Cst = sb("Cst", [128, L, 4, 257])
    BC = [[S.buf("C%d_%d" % (l, h)) for h in range(4)] for l in range(L)]
    Cd = sb("Cd", [128, 4, 257]);           BCd = [S.buf("Cd%d" % h) for h in range(4)]
    Cb = sb("Cb", [128, 4, 257], BF16);     BCb = [S.buf("Cb%d" % h) for h in range(4)]
    numb = sb("numb", [128, NB, 4, 256], BF16); Bnumb = [S.buf("numb%d" % b) for b in range(NB)]
    junk = sb("junk", [128, 256], BF16);    Bjunk = S.buf("junk")
    sm16 = sb("sm16", [128, 16 * 8]);       Bsm16 = S.buf("sm16")
    Bssr = S.buf("ssr"); Bdn = S.buf("dn"); Bsc = S.buf("sc")
    sz = [sb("sz%d" % i, [128, NT], BF16) for i in range(2)]
    Bsz = [S.buf("sz%d" % i) for i in range(2)]
    mixT = sb("mixT", [128, 16, NT], BF16); Bmix = [S.buf("mix%d" % e) for e in range(16)]
    ub = [sb("ub%d" % i, [128, NT], BF16) for i in range(2)];  Bub = [S.buf("ub%d" % i) for i in range(2)]
    cue = [sb("cue%d" % i, [128, NT + 2], BF16) for i in range(2)]; Bcue = [S.buf("cue%d" % i) for i in range(2)]
    Bb = [sb("Bb%d" % i, [128, NT], BF16) for i in range(2)];  BBb = [S.buf("Bb%d" % i) for i in range(2)]
    dg = sb("dg", [128, 8, 3, 128], BF16); Bdg = [S.buf("dg%d" % j) for j in range(8)]
    tails = sb("tails", [128, L, 8, 2], BF16); Btail = [[S.buf("tl%d_%d" % (l, j)) for j in range(8)] for l in range(L)]
    gsd = sb("gsd", [16, NT]);  Bgsd = S.buf("gsd")
    gr = sb("gr", [16, NT]);    Bgr = S.buf("gr")
    ghi = sb("ghi", [16, NT], BF16); Bghi = S.buf("ghi")
    glo = sb("glo", [16, NT], BF16); Bglo = S.buf("glo")
    tmp = [sb("tmp%d" % i, [128, NT]) for i in range(2)]; Btmp = [S.buf("tmp%d" % i) for i in range(2)]

    PS = [nc.alloc_psum_tensor("ps%d" % i, [128, 512], F32) for i in range(8)]
    BPS = [S.buf("ps%d" % i) for i in range(8)]
    for _b in BPS:
        _b.excl = True
    import os as _os
    pstate = {"i": int(_os.environ.get("PS0", "0")), "pinned": set()}

    POOLS = {'s': [0, 1, 2], 'c': [3, 4, 5, 6, 7], 'a': list(range(8))}
    pidx = {'s': 0, 'c': 0, 'a': 0}

    def psum(pool='a', pin=False):
        lst = POOLS[pool]
        while True:
            i = lst[pidx[pool] % len(lst)]
            pidx[pool] += 1
            if i not in pstate["pinned"]:
                break
        if pin:
            pstate["pinned"].add(i)
        return i

    def unpin(i):
        pstate["pinned"].discard(i)

    rr = {"ws": 0, "w4": 0, "sq": 0, "ev": 0}

    def nxt(key, n):
        i = rr[key]
        rr[key] = (i + 1) % n
        return i

    identF = cF[:, C_IDENT:C_IDENT + 128]
    identB = cB[:, C_IDENT:C_IDENT + 128]
    onesB = cB[:, C_ONES:C_ONES + 128]
    ones4 = cF[0:4, C_ONES:C_ONES + 128]
    indA = cB[:, C_INDA:C_INDA + 128].rearrange("p (j g) -> p j g", j=8)
    indB = cB[0:16, C_INDB:C_INDB + 1024].rearrange("p (j c) -> p j c", j=8)

    pe, act, dve, pool, sp = S.pe, S.act, S.dve, S.pool, S.sp
    T = nc.tensor
    A = nc.scalar
    V = nc.vector
    G = nc.gpsimd

    def pvcol(l, off):
        return pv[:, l * PV_PER + off: l * PV_PER + off + 1]

    S.dma(sp, cF[:], cst_d[:, :], BcF, writes=[BcF])
    S.dma(sp, pv[:], pv_d[:, :], Bpv, writes=[Bpv])
    S.dma(sp, gb[:], gb_d[:, :], Bgb, writes=[Bgb])
    S.dma(sp, wgf[:], wg_d[:, :], Bwgf, writes=[Bwgf])
    S.op(dve, lambda: V.tensor_copy(out=cB[:], in_=cF[:]), reads=[BcF], writes=[BcB])
    for h in range(4):
        S.op(dve, lambda h=h: V.tensor_copy(out=maskb[:, h, :], in_=cF[:, C_MASK:C_MASK + 128]),
             reads=[BcF], writes=[Bmask])
    S.op(dve, lambda: V.tensor_copy(out=wgb[:], in_=wgf[:]), reads=[Bwgf], writes=[Bwgb])
    for l in range(L):
        S.op(dve, lambda l=l: V.tensor_scalar(out=nbf[:, l:l + 1], in0=gb[:, 2 * l + 1:2 * l + 2], scalar1=-1.0,
                                               scalar2=None, op0=ALU.mult), reads=[Bgb], writes=[Bnbf])

    Bwc = [[S.buf("wconv%d_%d" % (l, i)) for i in range(9)] for l in range(L)]

    def convert_layer(l):
        for pi, s0 in enumerate(range(0, NSLAB, 8)):
            S.dma(pool, wsb_d[l, s0:s0 + 8].rearrange("s p f -> (s p) f"),
                  ws_d[l, s0:s0 + 8].rearrange("s p f -> (s p) f"), Bwc[l][pi], writes=[Bwc[l][pi]])
        S.dma(pool, wvb_d[l].rearrange("s p f -> (s p) f"), wv_d[l].rearrange("s p f -> (s p) f"), Bwc[l][7],
              writes=[Bwc[l][7]])
        S.dma(pool, wob_d[l].rearrange("s p f -> (s p) f"), wo_d[l].rearrange("s p f -> (s p) f"), Bwc[l][8],
              writes=[Bwc[l][8]])

    convert_layer(0)

    def evac_eng():
        i = nxt("ev", 2)
        return i

    def copy_on(which, out, in_, reads, writes, scale=None):
        if which == 0:
            if scale is None:
                return S.op(act, lambda: A.activation(out=out, in_=in_, func=AF.Copy), reads=reads, writes=writes)
            return S.op(act, lambda: A.activation(out=out, in_=in_, func=AF.Copy, scale=scale), reads=reads,
                        writes=writes)
        if scale is None:
            return S.op(dve, lambda: V.tensor_copy(out=out, in_=in_), reads=reads, writes=writes)
        return S.op(dve, lambda: V.tensor_scalar(out=out, in0=in_, scalar1=scale, scalar2=None, op0=ALU.mult),
                    reads=reads, writes=writes)

    def load_slab(l, s):
        r = nxt("ws", NWS)
        S.dma(sp, wsl[r][:], wsb_d[l, s], Bwsl[r], reads=[Bwc[l][s // 8]], writes=[Bwsl[r]])
        return r

    def slab_mm(l, s, M=128):
        r = load_slab(l, s)
        b = psum('s')
        for k in range(8):
            S.op(pe, lambda k=k: T.matmul(PS[b][0:M, :], lhsT=wsl[r][:, k * 128:k * 128 + M], rhs=hT[:, k, :],
                                          start=(k == 0), stop=(k == 7)),
                 reads=[Bwsl[r], BhT[k]], writes=[BPS[b]])
        return b

    dbg_bufs = []

    def dump(nm, src_ap, bufs):
        if nm in dbg_d:
            t = S.buf("dbgtmp_" + nm)
            S.dma(pool, dbg_d[nm], src_ap, t, reads=bufs, writes=[t])
            dbg_bufs.append(t)

    def layer(l, ti, first_in_seq):
        if ti == 0 and l + 1 < L:
            convert_layer(l + 1)
        for j in range(8):
            for tap in range(3):
                S.op(pool, lambda: G.tensor_scalar(out=dg[:, j, tap, :], in0=identB,
                                                   scalar1=pvcol(l, PV_CONVW + j * 3 + tap), scalar2=1.0,
                                                   op0=ALU.mult, op1=ALU.mult), reads=[BcB, Bpv], writes=[Bdg[j]])
        bss = psum('s', pin=True)
        for k in range(8):
            i = nxt("sq", NSQ)
            S.op(act, lambda: A.activation(out=sq[i][:], in_=xT[:, k, :], func=AF.Square),
                 reads=[BxT[k]], writes=[Bsq[i]])
            S.op(pe, lambda: T.matmul(PS[bss][:], lhsT=onesB, rhs=sq[i][:], start=(k == 0), stop=(k == 7)),
                 reads=[Bsq[i], BcB], writes=[BPS[bss]])
        S.op(act, lambda: A.activation(out=sd[:], in_=PS[bss][:], func=AF.Ln, scale=1.0 / 1024, bias=EPSB),
             reads=[BPS[bss], Beps], writes=[Bsd])
        S.op(act, lambda: A.activation(out=PS[bss][:], in_=sd[:], func=AF.Exp, scale=-0.5), reads=[Bsd],
             writes=[BPS[bss]])
        for k in range(8):
            S.op(dve, lambda: V.scalar_tensor_tensor(out=hT[:, k, :], in0=xT[:, k, :], scalar=pvcol(l, PV_GPRE + k),
                                                     in1=PS[bss][:], op0=ALU.mult, op1=ALU.mult),
                 reads=[BxT[k], BPS[bss], Bpv], writes=[BhT[k]])
        unpin(bss)

        bi = psum('c')
        bf = psum('c')
        for (bb, c0) in ((bi, 0), (bf, 4)):
            for k in range(8):
                S.op(pe, lambda: T.matmul(PS[bb][0:4, :], lhsT=wgb[:, l * 64 + k * 8 + c0: l * 64 + k * 8 + c0 + 4],
                                          rhs=hT[:, k, :], start=(k == 0), stop=(k == 7)),
                     reads=[Bwgb, BhT[k]], writes=[BPS[bb]])
        S.op(dve, lambda: V.tensor_scalar(out=g_t[:], in0=PS[bi][0:4, :], scalar1=gb[:, 2 * l:2 * l + 1], scalar2=None,
                                          op0=ALU.add), reads=[BPS[bi], Bgb], writes=[Bg])
        S.op(act, lambda: A.activation(out=sp_t[:], in_=PS[bf][0:4, :], func=AF.Exp, scale=-1.0, bias=nbf[:, l:l + 1]),
             reads=[BPS[bf], Bnbf], writes=[Bsp])
        S.op(act, lambda: A.activation(out=sp_t[:], in_=sp_t[:], func=AF.Ln, bias=ONEB[0:4, :]), reads=[Bsp, Beps],
             writes=[Bsp])
        def q_slab(h):
            b = slab_mm(l, h)
            S.op(act, lambda: A.activation(out=qT[:, h, :], in_=PS[b][:], func=AF.Copy, scale=float(128 ** -0.5)),
                 reads=[BPS[b]], writes=[BqT[h]] + Bosb)

        q_slab(0)
        for blk in range(NB):
            S.op(dve, lambda: V.tensor_tensor_scan(out=nb_t[:, blk * 128:(blk + 1) * 128], data0=ones4,
                                                   data1=sp_t[:, blk * 128:(blk + 1) * 128], initial=0.0,
                                                   op0=ALU.mult, op1=ALU.add),
                 reads=[Bsp, BcF], writes=[Bnb])
        S.op(dve, lambda: V.tensor_tensor(out=g_t[:], in0=g_t[:], in1=nb_t[:], op=ALU.add), reads=[Bg, Bnb],
             writes=[Bg])
        S.op(dve, lambda: V.tensor_reduce(out=sm4[:, 0:4], in_=g_t[:].rearrange("p (b t) -> p b t", b=NB),
                                          op=ALU.max, axis=AX.X), reads=[Bg], writes=[Bsm4])
        for blk in range(NB):
            mprev = mst[:, l:l + 1] if blk == 0 else sm4[:, 12 + blk - 1:12 + blk]
            S.op(dve, lambda: V.tensor_tensor(out=sm4[:, 4 + blk:5 + blk], in0=mprev, in1=sm4[:, blk:blk + 1],
                                              op=ALU.max), reads=[Bsm4, Bmst[l]], writes=[Bsm4])
            S.op(dve, lambda: V.tensor_tensor(out=sm4[:, 8 + blk:9 + blk], in0=mprev, in1=sm4[:, 4 + blk:5 + blk],
                                              op=ALU.subtract), reads=[Bsm4, Bmst[l]], writes=[Bsm4])
            S.op(dve, lambda: V.tensor_tensor(out=sm4[:, 12 + blk:13 + blk], in0=sm4[:, 4 + blk:5 + blk],
                                              in1=nb_t[:, blk * 128 + 127:blk * 128 + 128], op=ALU.subtract),
                 reads=[Bsm4, Bnb], writes=[Bsm4])
        S.op(dve, lambda: V.tensor_copy(out=mst[:, l:l + 1], in_=sm4[:, 15:16]), reads=[Bsm4], writes=[Bmst[l]])
        S.op(dve, lambda: V.tensor_scalar(out=sm4[:, 16:20], in0=sm4[:, 4:8], scalar1=-1.0, scalar2=None,
                                          op0=ALU.mult), reads=[Bsm4], writes=[Bsm4])
        for h in range(1, 4):
            q_slab(h)
        for blk in range(NB):
            sl = slice(blk * 128, (blk + 1) * 128)
            S.op(act, lambda: A.activation(out=w_t[:, sl], in_=g_t[:, sl], func=AF.Exp,
                                           bias=sm4[:, 16 + blk:17 + blk]), reads=[Bg, Bsm4], writes=[Bw])
            S.op(act, lambda: A.activation(out=th_t[:, sl], in_=nb_t[:, sl], func=AF.Exp,
                                           bias=sm4[:, 16 + blk:17 + blk]), reads=[Bnb, Bsm4], writes=[Bth])
        S.op(act, lambda: A.activation(out=sm4[:, 20:24], in_=sm4[:, 8:12], func=AF.Exp), reads=[Bsm4],
             writes=[Bsm4])
        for h in range(4):
            b = slab_mm(l, 4 + h)
            S.op(dve, lambda: V.tensor_copy(out=kT[:, h, :], in_=PS[b][:]), reads=[BPS[b]], writes=[BkT[h]] + Bosb)
        for blk in range(NB):
            S.op(dve, lambda: V.tensor_scalar(out=sm4[:, 32 + blk * 4:36 + blk * 4], in0=cF[0:4, C_IDENT:C_IDENT + 4],
                                              scalar1=sm4[:, 20 + blk:21 + blk], scalar2=None, op0=ALU.mult),
                 reads=[Bsm4, BcF], writes=[Bsm4])

        for blk in range(NB):
            b = psum('c')
            pb = PS[b][:].bitcast(BF16)
            for h in range(4):
                S.op(pe, lambda: T.transpose(out=pb[:, h * 128:(h + 1) * 128], in_=kT[:, h, blk * 128:(blk + 1) * 128],
                                             identity=identB), reads=[BkT[h], BcB], writes=[BPS[b]])
            copy_on(blk % 2, ktok[:, blk, :], pb[:, 0:512], [BPS[b]], [Bktok[blk]] + Bosb)
        bw = psum('c')
        for blk in range(NB):
            sl = slice(blk * 128, (blk + 1) * 128)
            S.op(pe, lambda: T.transpose(out=PS[bw][:, blk * 8:blk * 8 + 4], in_=w_t[:, sl],
                                         identity=cF[0:4, C_IDENT:C_IDENT + 4]), reads=[Bw, BcF], writes=[BPS[bw]])
            S.op(pe, lambda: T.transpose(out=PS[bw][:, blk * 8 + 4:blk * 8 + 8], in_=th_t[:, sl],
                                         identity=cF[0:4, C_IDENT:C_IDENT + 4]), reads=[Bth, BcF], writes=[BPS[bw]])
        S.op(pe, lambda: T.matmul(PS[bw][:, 32:48], lhsT=ones4, rhs=sm4[:, 32:48], start=True, stop=True),
             reads=[Bsm4, BcF], writes=[BPS[bw]])
        S.op(dve, lambda: V.tensor_copy(out=wthr[:, 0:48], in_=PS[bw][:, 0:48]), reads=[BPS[bw]], writes=[Bwthr])
        S.op(dve, lambda: V.tensor_copy(out=vw[:, :, :, 256],
                                        in_=wthr[:, 0:32].rearrange("p (b e) -> p b e", e=8)[:, :, 0:4]),
             reads=[Bwthr], writes=Bvw + Bosb)
        for h in range(4):
            r = nxt("w4", NW4)
            S.dma(sp, w4k[r][:], wvb_d[l, h], Bw4k[r], reads=[Bwc[l][7]], writes=[Bw4k[r]])
            for blk in range(NB):
                b = psum('s')
                for k in range(8):
                    S.op(pe, lambda: T.matmul(PS[b][:, 0:256], lhsT=hT[:, k, blk * 128:(blk + 1) * 128],
                                              rhs=w4k[r][:, k * 256:(k + 1) * 256], start=(k == 0), stop=(k == 7)),
                         reads=[Bw4k[r], BhT[k]], writes=[BPS[b]])
                S.op(dve, lambda: V.tensor_scalar(out=vw[:, blk, h, 0:256], in0=PS[b][:, 0:256],
                                                  scalar1=wthr[:, blk * 8 + h:blk * 8 + h + 1], scalar2=None,
                                                  op0=ALU.mult), reads=[BPS[b], Bwthr], writes=[Bvw[blk]])

        def slab_o(e):
            b = slab_mm(l, 8 + e)
            S.op(act, lambda: A.activation(out=mixT[:, e, :], in_=PS[b][:], func=AF.Sigmoid), reads=[BPS[b]],
                 writes=[Bmix[e]])

        def slab_z(e):
            b = slab_mm(l, 16 + e)
            i = e % 2
            S.op(act, lambda: A.activation(out=sz[i][:], in_=PS[b][:], func=AF.Silu), reads=[BPS[b]], writes=[Bsz[i]])
            S.op(dve, lambda: V.tensor_tensor(out=mixT[:, e, :], in0=mixT[:, e, :], in1=sz[i][:], op=ALU.mult),
                 reads=[Bmix[e], Bsz[i]], writes=[Bmix[e]])

        fill = [(lambda e=e: slab_o(e)) for e in range(8)] + [(lambda e=e: slab_z(e)) for e in range(8)]

        def filler(n):
            for _ in range(n):
                if fill:
                    fill.pop(0)()

        for blk in range(NB):
            sl = slice(blk * 128, (blk + 1) * 128)
            for h in range(4):
                S.op(dve, lambda: V.tensor_scalar(out=Cd[:, h, :], in0=Cst[:, l, h, :],
                                                  scalar1=wthr[:, 32 + blk * 4 + h:33 + blk * 4 + h], scalar2=None,
                                                  op0=ALU.mult), reads=[BC[l][h], Bwthr], writes=[BCd[h]])
                S.op(act, lambda: A.activation(out=Cb[:, h, :], in_=Cd[:, h, :], func=AF.Copy), reads=[BCd[h]],
                     writes=[BCb[h]])
            bS = psum('c')
            for h in range(4):
                S.op(pe, lambda: T.matmul(PS[bS][:, h * 128:(h + 1) * 128], lhsT=kT[:, h, sl], rhs=qT[:, h, sl],
                                          start=True, stop=True), reads=[BkT[h], BqT[h]], writes=[BPS[bS]])
            si = blk % 2
            S.op(dve, lambda: V.tensor_tensor(out=Sm[si][:], in0=PS[bS][:], in1=maskb[:].rearrange("p h t -> p (h t)"),
                                              op=ALU.mult), reads=[BPS[bS], Bmask], writes=[BSm[si]])
            filler(2)
            bN = [psum('c'), psum('c')]
            bD = psum('c')
            for h in range(4):
                on = PS[bN[h // 2]][:, (h % 2) * 256:(h % 2) * 256 + 256]
                S.op(pe, lambda: T.matmul(on, lhsT=Sm[si][:, h * 128:(h + 1) * 128], rhs=vw[:, blk, h, 0:256],
                                          start=True, stop=False), reads=[BSm[si], Bvw[blk]], writes=[BPS[bN[h // 2]]])
                S.op(pe, lambda: T.matmul(on, lhsT=qT[:, h, sl], rhs=Cb[:, h, 0:256], start=False, stop=True),
                     reads=[BqT[h], BCb[h]], writes=[BPS[bN[h // 2]]])
                S.op(pe, lambda: T.matmul(PS[bD][:, h:h + 1], lhsT=Sm[si][:, h * 128:(h + 1) * 128],
                                          rhs=vw[:, blk, h, 256:257], start=True, stop=False),
                     reads=[BSm[si], Bvw[blk]], writes=[BPS[bD]])
                S.op(pe, lambda: T.matmul(PS[bD][:, h:h + 1], lhsT=qT[:, h, sl], rhs=Cb[:, h, 256:257], start=False,
                                          stop=True), reads=[BqT[h], BCb[h]], writes=[BPS[bD]])
            bC = [bS, psum('c')]
            for h in range(4):
                S.op(pe, lambda: T.matmul(PS[bC[h // 2]][:, (h % 2) * 256:(h % 2) * 256 + 256],
                                          lhsT=ktok[:, blk, h * 128:(h + 1) * 128], rhs=vw[:, blk, h, 0:256],
                                          start=True, stop=True), reads=[Bktok[blk], Bvw[blk]],
                     writes=[BPS[bC[h // 2]]])
                S.op(pe, lambda: T.matmul(PS[bD][:, 4 + h:5 + h], lhsT=ktok[:, blk, h * 128:(h + 1) * 128],
                                          rhs=vw[:, blk, h, 256:257], start=True, stop=True),
                     reads=[Bktok[blk], Bvw[blk]], writes=[BPS[bD]])
            for h in range(4):
                S.op(dve, lambda: V.tensor_tensor(out=Cst[:, l, h, 0:256], in0=Cd[:, h, 0:256],
                                                  in1=PS[bC[h // 2]][:, (h % 2) * 256:(h % 2) * 256 + 256],
                                                  op=ALU.add), reads=[BCd[h], BPS[bC[h // 2]]], writes=[BC[l][h]])
            S.op(dve, lambda: V.tensor_tensor(out=Cst[:, l, :, 256], in0=Cd[:, :, 256], in1=PS[bD][:, 4:8],
                                              op=ALU.add), reads=BCd + [BPS[bD]], writes=BC[l])
            S.op(act, lambda: A.activation(out=sm16[:, blk * 4:blk * 4 + 4], in_=PS[bD][:, 0:4], func=AF.Abs),
                 reads=[BPS[bD]], writes=[Bdn])
            S.op(dve, lambda: V.tensor_tensor(out=sm16[:, blk * 4:blk * 4 + 4], in0=sm16[:, blk * 4:blk * 4 + 4],
                                              in1=wthr[:, blk * 8 + 4:blk * 8 + 8], op=ALU.max),
                 reads=[Bdn, Bwthr], writes=[Bdn])
            for h in range(4):
                S.op(act, lambda: A.activation(out=junk[:], in_=PS[bN[h // 2]][:, (h % 2) * 256:(h % 2) * 256 + 256],
                                               func=AF.Square,
                                               accum_out=sm16[:, 16 + blk * 4 + h:17 + blk * 4 + h]),
                     reads=[BPS[bN[h // 2]]], writes=[Bjunk, Bssr])
            for pr in range(2):
                S.op(dve, lambda: V.tensor_copy(out=numb[:, blk, 2 * pr:2 * pr + 2, :].rearrange("p a v -> p (a v)"),
                                                in_=PS[bN[pr]][:]), reads=[BPS[bN[pr]]], writes=[Bnumb[blk]])
            filler(2)
        S.op(dve, lambda: V.reciprocal(out=sm16[:, 32:48], in_=sm16[:, 0:16]), reads=[Bdn], writes=[Bsm16])
        S.op(dve, lambda: V.tensor_tensor(out=sm16[:, 48:64], in0=sm16[:, 32:48], in1=sm16[:, 32:48], op=ALU.mult),
             reads=[Bsm16], writes=[Bsm16])
        S.op(dve, lambda: V.tensor_tensor(out=sm16[:, 64:80], in0=sm16[:, 48:64], in1=sm16[:, 16:32], op=ALU.mult),
             reads=[Bsm16, Bssr], writes=[Bsm16])
        S.op(act, lambda: A.activation(out=sm16[:, 80:96], in_=sm16[:, 64:80], func=AF.Ln, scale=1.0 / 256,
                                       bias=EPSB), reads=[Bsm16, Beps], writes=[Bsm16])
        S.op(act, lambda: A.activation(out=sm16[:, 96:112], in_=sm16[:, 80:96], func=AF.Exp, scale=-0.5),
             reads=[Bsm16], writes=[Bsm16])
        S.op(dve, lambda: V.tensor_tensor(out=sm16[:, 112:128], in0=sm16[:, 96:112], in1=sm16[:, 32:48],
                                          op=ALU.mult), reads=[Bsm16], writes=[Bsc])
        for blk in range(NB):
            S.op(dve, lambda: V.tensor_tensor(out=numb[:, blk, :, :], in0=numb[:, blk, :, :],
                                              in1=sm16[:, 112 + blk * 4:116 + blk * 4].unsqueeze(2).broadcast_to(
                                                  [128, 4, 256]), op=ALU.mult),
                 reads=[Bnumb[blk], Bsc], writes=[Bnumb[blk]])
        filler(100)

        bg = psum('c', pin=True)

        def conv_a(j):
            i = j % 2
            S.op(pool, lambda: G.tensor_copy(out=cue[i][:, 0:2], in_=tails[:, l, j, :]), reads=[Btail[l][j]],
                 writes=[Bcue[i]])
            bu = slab_mm(l, 24 + 4 * j + 0)
            S.op(act, lambda: A.activation(out=ub[i][:], in_=PS[bu][:], func=AF.Copy), reads=[BPS[bu]],
                 writes=[Bub[i]])
            bc = slab_mm(l, 24 + 4 * j + 1)
            S.op(dve, lambda: V.tensor_tensor(out=cue[i][:, 2:NT + 2], in0=PS[bc][:], in1=ub[i][:], op=ALU.mult),
                 reads=[BPS[bc], Bub[i]], writes=[Bcue[i]])
            S.op(pool, lambda: G.tensor_copy(out=tails[:, l, j, :], in_=cue[i][:, NT:NT + 2]), reads=[Bcue[i]],
                 writes=[Btail[l][j]])
            bB = slab_mm(l, 24 + 4 * j + 2)
            S.op(act, lambda: A.activation(out=Bb[i][:], in_=PS[bB][:], func=AF.Copy), reads=[BPS[bB]],
                 writes=[BBb[i]])

        def conv_b(j):
            i = j % 2
            by = psum('c')
            for tap in range(3):
                S.op(pe, lambda: T.matmul(PS[by][:], lhsT=dg[:, j, tap, :], rhs=cue[i][:, tap:tap + NT],
                                          start=(tap == 0), stop=(tap == 2)), reads=[Bdg[j], Bcue[i]],
                     writes=[BPS[by]])
            S.op(dve, lambda: V.tensor_tensor(out=mixT[:, 8 + j, :], in0=PS[by][:], in1=Bb[i][:], op=ALU.mult),
                 reads=[BPS[by], BBb[i]], writes=[Bmix[8 + j]])
            qi = nxt("sq", NSQ)
            S.op(act, lambda: A.activation(out=sq[qi][:], in_=mixT[:, 8 + j, :], func=AF.Square),
                 reads=[Bmix[8 + j]], writes=[Bsq[qi]])
            bz = slab_mm(l, 24 + 4 * j + 3)
            S.op(act, lambda: A.activation(out=sz[i][:], in_=PS[bz][:], func=AF.Silu), reads=[BPS[bz]],
                 writes=[Bsz[i]])
            S.op(pe, lambda: T.matmul(PS[bg][0:16, :], lhsT=indA[:, j, :], rhs=sq[qi][:], start=(j == 0),
                                      stop=(j == 7)), reads=[Bsq[qi], BcB], writes=[BPS[bg]])
            S.op(dve, lambda: V.tensor_tensor(out=mixT[:, 8 + j, :], in0=mixT[:, 8 + j, :], in1=sz[i][:],
                                              op=ALU.mult), reads=[Bmix[8 + j], Bsz[i]], writes=[Bmix[8 + j]])

        def ym(e):
            h, half = e // 2, e % 2
            b = psum('c')
            pb = PS[b][:].bitcast(BF16)
            for blk in range(NB):
                S.op(pe, lambda: T.transpose(out=pb[:, blk * 128:(blk + 1) * 128],
                                             in_=numb[:, blk, h, half * 128:(half + 1) * 128], identity=identB),
                     reads=[Bnumb[blk], BcB], writes=[BPS[b]])
            S.op(dve, lambda: V.scalar_tensor_tensor(out=mixT[:, e, :], in0=pb[:, 0:512],
                                                     scalar=pvcol(l, PV_GHEAD + e), in1=mixT[:, e, :], op0=ALU.mult,
                                                     op1=ALU.mult), reads=[BPS[b], Bmix[e], Bpv], writes=[Bmix[e]])

        for j in range(8):
            conv_a(j)
            ym(j)
            conv_b(j)
        S.op(act, lambda: A.activation(out=gsd[:], in_=PS[bg][0:16, :], func=AF.Ln, scale=1.0 / 64,
                                       bias=EPSB[0:16, :]), reads=[BPS[bg], Beps], writes=[Bgsd])
        unpin(bg)
        S.op(act, lambda: A.activation(out=gr[:], in_=gsd[:], func=AF.Exp, scale=-0.5), reads=[Bgsd], writes=[Bgr])
        S.op(dve, lambda: V.tensor_copy(out=ghi[:], in_=gr[:]), reads=[Bgr], writes=[Bghi])
        S.op(dve, lambda: V.tensor_tensor(out=glo[:], in0=gr[:], in1=ghi[:], op=ALU.subtract), reads=[Bgr, Bghi],
             writes=[Bglo])
        bss = psum('c', pin=True)
        oslab = {}

        def op_load(dc):
            r = nxt("w4", NW4)
            S.dma(sp, w4k[r][:], wob_d[l, dc], Bw4k[r], reads=[Bwc[l][8]], writes=[Bw4k[r]])
            oslab[dc] = (r, psum('s'))

        def op_mm(dc, e0, e1):
            r, b = oslab[dc]
            for e in range(e0, e1):
                S.op(pe, lambda: T.matmul(PS[b][:], lhsT=w4k[r][:, e * 128:(e + 1) * 128], rhs=mixT[:, e, :],
                                          start=(e == 0), stop=(e == 15)), reads=[Bw4k[r], Bmix[e]],
                     writes=[BPS[b]])

        def op_evac(dc):
            r, b = oslab[dc]
            S.op(act, lambda: A.activation(out=osb[:, dc, :], in_=PS[b][:], func=AF.Copy), reads=[BPS[b]],
                 writes=[Bosb[dc]] + arena_m)
            qi = nxt("sq", NSQ)
            S.op(dve, lambda: V.tensor_tensor(out=sq[qi][:], in0=PS[b][:], in1=osb[:, dc, :], op=ALU.mult),
                 reads=[BPS[b], Bosb[dc]], writes=[Bsq[qi]])
            oslab[dc] = (r, b, qi)

        def op_ss(dc):
            qi = oslab[dc][2]
            S.op(pe, lambda: T.matmul(PS[bss][:], lhsT=onesB, rhs=sq[qi][:], start=(dc == 0), stop=(dc == 7)),
                 reads=[Bsq[qi], BcB], writes=[BPS[bss]])

        for dc in range(3):
            op_load(dc)
            op_mm(dc, 0, 8)
        for j in range(8):
            b = psum('c')
            S.op(pe, lambda: T.matmul(PS[b][:], lhsT=indB[:, j, :], rhs=ghi[:], start=True, stop=False),
                 reads=[Bghi, BcB], writes=[BPS[b]])
            S.op(pe, lambda: T.matmul(PS[b][:], lhsT=indB[:, j, :], rhs=glo[:], start=False, stop=True),
                 reads=[Bglo, BcB], writes=[BPS[b]])
            S.op(dve, lambda: V.scalar_tensor_tensor(out=mixT[:, 8 + j, :], in0=mixT[:, 8 + j, :],
                                                     scalar=pvcol(l, PV_GCONV + j), in1=PS[b][:], op0=ALU.mult,
                                                     op1=ALU.mult), reads=[Bmix[8 + j], BPS[b], Bpv],
                 writes=[Bmix[8 + j]])
        for dc in range(8):
            if dc >= 3:
                op_load(dc)
                op_mm(dc, 0, 8)
            op_mm(dc, 8, 16)
            op_evac(dc)
            if dc > 0:
                op_ss(dc - 1)
        op_ss(7)
        S.op(act, lambda: A.activation(out=sd[:], in_=PS[bss][:], func=AF.Ln, scale=1.0 / 1024, bias=EPSB),
             reads=[BPS[bss], Beps], writes=[Bsd])
        S.op(act, lambda: A.activation(out=PS[bss][:], in_=sd[:], func=AF.Exp, scale=-0.5), reads=[Bsd],
             writes=[BPS[bss]])
        for k in range(8):
            i = k % 2
            S.op(dve, lambda: V.scalar_tensor_tensor(out=tmp[i][:], in0=osb[:, k, :], scalar=pvcol(l, PV_GPOST + k),
                                                     in1=PS[bss][:], op0=ALU.mult, op1=ALU.mult),
                 reads=[Bosb[k], BPS[bss], Bpv], writes=[Btmp[i]])
            S.op(dve, lambda: V.tensor_tensor(out=xT[:, k, :], in0=xT[:, k, :], in1=tmp[i][:], op=ALU.add),
                 reads=[BxT[k], Btmp[i]], writes=[BxT[k]])
        unpin(bss)

    epsb = sb("epsb", [128, 2])
    Beps = S.buf("eps")
    S.op(dve, lambda: V.memset(epsb[:, 0:1], EPS), writes=[Beps])
    S.op(dve, lambda: V.memset(epsb[:, 1:2], 1.0), writes=[Beps])
    EPSB = epsb[:, 0:1]
    ONEB = epsb[:, 1:2]

    for ti in range(NTILES):
        tis = ti % TPS
        tok0 = ti * NT
        if tis == 0:
            S.op(pool, lambda: G.memset(Cst[:].rearrange("p l h v -> p (l h v)"), 0.0),
                 writes=[b for bl in BC for b in bl])
            S.op(pool, lambda: G.memset(mst[:], 0.0), writes=Bmst)
            S.op(pool, lambda: G.memset(tails[:].rearrange("p l j t -> p (l j t)"), 0.0),
                 writes=[b for bl in Btail for b in bl])
        for blk in range(NB):
            i = blk % 2
            S.dma(sp, xst[i][:], x_d[tok0 + blk * 128: tok0 + (blk + 1) * 128, :], Bxst[i], writes=[Bxst[i]])
            for half in range(2):
                b = psum()
                for kk in range(4):
                    k = half * 4 + kk
                    S.op(pe, lambda: T.transpose(out=PS[b][:, kk * 128:(kk + 1) * 128],
                                                 in_=xst[i][:, k * 128:(k + 1) * 128], identity=identF),
                         reads=[Bxst[i], BcF], writes=[BPS[b]])
                copy_on(half, xT[:, half * 4:half * 4 + 4, blk * 128:(blk + 1) * 128],
                        PS[b][:].rearrange("p (a t) -> p a t", a=4), [BPS[b]], BxT[half * 4:half * 4 + 4])
        for l in range(L):
            layer(l, ti, tis == 0)
        for blk in range(NB):
            i = blk % 2
            for half in range(2):
                b = psum()
                for kk in range(4):
                    k = half * 4 + kk
                    S.op(pe, lambda: T.transpose(out=PS[b][:, kk * 128:(kk + 1) * 128],
                                                 in_=xT[:, k, blk * 128:(blk + 1) * 128], identity=identF),
                         reads=[BxT[k], BcF], writes=[BPS[b]])
                copy_on(half, ost[i][:, half * 512:(half + 1) * 512], PS[b][:], [BPS[b]], [Bost[i]])
            S.dma(pool, y_d[tok0 + blk * 128: tok0 + (blk + 1) * 128, :], ost[i][:], Bost[i], reads=[Bost[i]])
    S.wait_all(pool, Bost)
    S.wait_all(sp, Bost + dbg_bufs)
    return nc, S


def _slab_cols():
    cols = []
    for h in range(4):
        cols.append(0 + 128 * h)
    for h in range(4):
        cols.append(512 + 128 * h)
    for e in range(8):
        cols.append(2048 + 128 * e)
    for e in range(8):
        cols.append(3072 + 128 * e)
    for j in range(8):
        cols.append(4104 + 128 * j)
        cols.append(6152 + 128 * j)
        cols.append(5128 + 128 * j)
        cols.append(7176 + 128 * j)
    return cols


def _consts():
    c = np.zeros((128, C_TOT), np.float32)
    c[:, C_IDENT:C_IDENT + 128] = np.eye(128, dtype=np.float32)
    c[:, C_ONES:C_ONES + 128] = 1.0
    s = np.arange(128)
    c[:, C_MASK:C_MASK + 128] = (s[:, None] <= s[None, :]).astype(np.float32)
    indA = np.zeros((128, 8, 16), np.float32)
    indB = np.zeros((128, 8, 128), np.float32)
    for j in range(8):
        for p in range(128):
            g = 2 * j + (1 if p >= 64 else 0)
            indA[p, j, g] = 1.0
            indB[g, j, p] = 1.0
    c[:, C_INDA:C_INDA + 128] = indA.reshape(128, 128)
    c[:, C_INDB:C_INDB + 1024] = indB.reshape(128, 1024)
    return c


def _prep_layers(layers, norm_pre, norm_post, w_in, b_igate, b_fgate, head_norm, conv_w, conv_norm, w_out):
    L = len(layers)
    cols = np.array(_slab_cols())
    colidx = (cols[:, None] + np.arange(128)[None, :])
    ws = np.empty((L, NSLAB, 128, 1024), np.float32)
    wv = np.empty((L, 4, 128, 2048), np.float32)
    wo = np.empty((L, 8, 128, 2048), np.float32)
    wg = np.empty((128, L * 64), np.float32)
    pv = np.empty((128, L * PV_PER), np.float32)
    gb = np.empty((4, L * 2), np.float32)
    for li, l in enumerate(layers):
        W = np.asarray(w_in[l]).reshape(8, 128, 8200)
        ws[li] = W[:, :, colidx].transpose(2, 1, 0, 3).reshape(NSLAB, 128, 1024)
        wv[li] = W[:, :, 1024:2048].reshape(8, 128, 4, 256).transpose(2, 1, 0, 3).reshape(4, 128, 2048)
        wo[li] = np.asarray(w_out[l]).reshape(16, 128, 8, 128).transpose(2, 1, 0, 3).reshape(8, 128, 2048)
        wg[:, li * 64:(li + 1) * 64] = W[:, :, 4096:4104].transpose(1, 0, 2).reshape(128, 64)
        o = li * PV_PER
        pv[:, o + PV_GPRE:o + PV_GPRE + 8] = np.asarray(norm_pre[l]).reshape(8, 128).T
        pv[:, o + PV_GPOST:o + PV_GPOST + 8] = np.asarray(norm_post[l]).reshape(8, 128).T
        pv[:, o + PV_GHEAD:o + PV_GHEAD + 8] = np.asarray(head_norm[l]).reshape(8, 128).T
        pv[:, o + PV_GCONV:o + PV_GCONV + 8] = np.asarray(conv_norm[l]).reshape(8, 128).T
        pv[:, o + PV_CONVW:o + PV_CONVW + 24] = np.asarray(conv_w[l]).reshape(3, 8, 128).transpose(2, 1, 0).reshape(128, 24)
        gb[:, 2 * li] = np.asarray(b_igate[l])
        gb[:, 2 * li + 1] = np.asarray(b_fgate[l])
    return dict(ws=ws, wv=wv, wo=wo, wg=wg, pv=pv, gb=gb, cst=_consts())


_PROG_CACHE = {}


def _get_prog(L, NTILES, TPS):
    key = (L, NTILES, TPS)
    if key not in _PROG_CACHE:
        _PROG_CACHE[key] = build(L, NTILES, TPS)[0]
    return _PROG_CACHE[key]


FUSED = True


def kernel(x, norm_pre, norm_post, w_in, b_igate, b_fgate, head_norm, conv_w, conv_norm, w_out):
    x = np.asarray(x, dtype=np.float32)
    Bt, Sq, D = x.shape
    per = Bt // NCORES
    TPS = Sq // NT
    NTILES = per * TPS
    xs = [np.ascontiguousarray(x[c * per:(c + 1) * per].reshape(per * Sq, D)) for c in range(NCORES)]
    groups = [list(range(4))] if FUSED else [[l] for l in range(4)]
    for layers in groups:
        prm = _prep_layers(layers, norm_pre, norm_post, w_in, b_igate, b_fgate, head_norm, conv_w, conv_norm, w_out)
        nc = _get_prog(len(layers), NTILES, TPS)
        in_maps = [dict(prm, x=xs[c]) for c in range(NCORES)]
        res = run_bass_kernel_spmd(nc, in_maps, core_ids=list(range(NCORES)))
        xs = [np.asarray(res.results[c]["y"], dtype=np.float32) for c in range(NCORES)]
    out = np.stack([xc.reshape(per, Sq, D) for xc in xs], axis=0).reshape(Bt, Sq, D)
    return out
```

```python
import numpy as np
import concourse.bass as bass
import concourse.mybir as mybir
from concourse.bass_utils import run_bass_kernel_spmd

F32 = mybir.dt.float32
BF16 = mybir.dt.bfloat16
AF = mybir.ActivationFunctionType
ALU = mybir.AluOpType
AX = mybir.AxisListType

EPS = 1e-6
NT = 512
NB = 4
NSLAB = 56
NCORES = 8
S_EPOCH = 30000


class Buf:
    __slots__ = ("name", "writers", "readers", "dsem", "dcount", "excl")

    def __init__(self, name):
        self.name = name
        self.excl = False
        self.writers = []
        self.readers = []
        self.dsem = None
        self.dcount = 0


class Eng:
    def __init__(self, S, name, h):
        self.S = S
        self.name = name
        self.h = h
        self.sems = []
        self.count = 0
        self.seen = {}

    def cur_sem(self):
        if not self.sems or self.count >= S_EPOCH:
            self.sems.append(self.S.nc.alloc_semaphore("%s_e%d" % (self.name, len(self.sems))))
            self.count = 0
        return self.sems[-1]


class Sched:
    def __init__(self, nc):
        self.nc = nc
        self.pe = Eng(self, "pe", nc.tensor)
        self.act = Eng(self, "act", nc.scalar)
        self.dve = Eng(self, "dve", nc.vector)
        self.pool = Eng(self, "pool", nc.gpsimd)
        self.sp = Eng(self, "sp", nc.sync)
        self.nbuf = 0
        self.n_ins = 0
        self.n_wait = 0

    def buf(self, name=None):
        self.nbuf += 1
        return Buf(name or "b%d" % self.nbuf)

    def _wait(self, eng, tok):
        sem, val, src = tok
        key = id(sem)
        if eng.seen.get(key, 0) >= val:
            return
        eng.h.wait_ge(sem, val)
        eng.seen[key] = val
        self.n_wait += 1

    def _deps(self, reads, writes):
        deps = []
        for b in reads:
            deps.extend(b.writers)
            if b.excl:
                deps.extend(b.readers)
        for b in writes:
            deps.extend(b.writers)
            deps.extend(b.readers)
        return deps

    def _commit(self, tok, reads, writes):
        for b in reads:
            b.readers.append(tok)
        for b in writes:
            b.writers = [tok]
            b.readers = []

    def op(self, eng, fn, reads=(), writes=()):
        for tok in self._deps(reads, writes):
            if tok[2] is eng and eng is self.pe:
                continue
            self._wait(eng, tok)
        sem = eng.cur_sem()
        ins = fn()
        ins.then_inc(sem, 1)
        eng.count += 1
        tok = (sem, eng.count, eng)
        self.n_ins += 1
        self._commit(tok, reads, writes)
        return tok

    def dma(self, eng, out, in_, sb, reads=(), writes=(), **kw):
        for tok in self._deps(reads, writes):
            self._wait(eng, tok)
        if sb.dsem is None:
            sb.dsem = self.nc.alloc_semaphore("d_%s" % sb.name)
        ins = eng.h.dma_start(out=out, in_=in_, **kw)
        ins.then_inc(sb.dsem, 16)
        sb.dcount += 16
        tok = (sb.dsem, sb.dcount, None)
        self.n_ins += 1
        self._commit(tok, reads, writes)
        return tok

    def wait_all(self, eng, bufs):
        for b in bufs:
            for tok in b.writers + b.readers:
                self._wait(eng, tok)


PV_GPRE, PV_GPOST, PV_GHEAD, PV_GCONV, PV_CONVW, PV_PER = 0, 8, 16, 24, 32, 56
C_IDENT, C_ONES, C_MASK, C_INDA, C_INDB, C_TOT = 0, 128, 256, 384, 512, 1536


def build(L, NTILES, TPS, dbg=None):
    nc = bass.Bass("TRN2", target_bir_lowering=False)
    S = Sched(nc)
    NTOK = NTILES * NT

    x_d = nc.dram_tensor("x", [NTOK, 1024], F32, kind="ExternalInput").ap()
    ws_d = nc.dram_tensor("ws", [L, NSLAB, 128, 1024], F32, kind="ExternalInput").ap()
    wv_d = nc.dram_tensor("wv", [L, 4, 128, 2048], F32, kind="ExternalInput").ap()
    wo_d = nc.dram_tensor("wo", [L, 8, 128, 2048], F32, kind="ExternalInput").ap()
    wg_d = nc.dram_tensor("wg", [128, L * 64], F32, kind="ExternalInput").ap()
    pv_d = nc.dram_tensor("pv", [128, L * PV_PER], F32, kind="ExternalInput").ap()
    gb_d = nc.dram_tensor("gb", [4, L * 2], F32, kind="ExternalInput").ap()
    cst_d = nc.dram_tensor("cst", [128, C_TOT], F32, kind="ExternalInput").ap()
    y_d = nc.dram_tensor("y", [NTOK, 1024], F32, kind="ExternalOutput").ap()
    wsb_d = nc.dram_tensor("wsb", [L, NSLAB, 128, 1024], BF16).ap()
    wvb_d = nc.dram_tensor("wvb", [L, 4, 128, 2048], BF16).ap()
    wob_d = nc.dram_tensor("wob", [L, 8, 128, 2048], BF16).ap()
    dbg_d = {}
    if dbg:
        for nm, shp in dbg.items():
            dbg_d[nm] = nc.dram_tensor("dbg_" + nm, list(shp), F32, kind="ExternalOutput").ap()

    def sb(name, shape, dt=F32):
        return nc.alloc_sbuf_tensor("s_" + name, list(shape), dt)

    cF = sb("cF", [128, C_TOT]);            BcF = S.buf("cF")
    cB = sb("cB", [128, C_TOT], BF16);      BcB = S.buf("cB")
    maskb = sb("maskb", [128, 4, 128], BF16); Bmask = S.buf("mask")
    pv = sb("pv", [128, L * PV_PER]);       Bpv = S.buf("pv")
    gb = sb("gb", [4, L * 2]);              Bgb = S.buf("gb")
    nbf = sb("nbf", [4, L]);                Bnbf = S.buf("nbf")
    wgf = sb("wgf", [128, L * 64]);         Bwgf = S.buf("wgf")
    wgb = sb("wgb", [128, L * 64], BF16);   Bwgb = S.buf("wgb")

    xst = [sb("xst%d" % i, [128, 1024]) for i in range(NB)]
    Bxst = [S.buf("xst%d" % i) for i in range(NB)]
    ost = [sb("ost%d" % i, [128, 1024]) for i in range(2)]
    Bost = [S.buf("ost%d" % i) for i in range(2)]
    xT = sb("xT", [128, 8, NT]);            BxT = [S.buf("xT%d" % k) for k in range(8)]
    hT = sb("hT", [128, 8, NT], BF16);      BhT = [S.buf("hT%d" % k) for k in range(8)]
    NSQ = 3
    sq = [sb("sq%d" % i, [128, NT], BF16) for i in range(NSQ)]
    Bsq = [S.buf("sq%d" % i) for i in range(NSQ)]
    sd = sb("sd", [128, NT]);               Bsd = S.buf("sd")
    NWS = 6
    wsl = [sb("wsl%d" % i, [128, 1024], BF16) for i in range(NWS)]
    Bwsl = [S.buf("wsl%d" % i) for i in range(NWS)]
    NW4 = 4
    w4k = [sb("w4k%d" % i, [128, 2048], BF16) for i in range(NW4)]
    Bw4k = [S.buf("w4k%d" % i) for i in range(NW4)]
    ARENA_B = max(4096 + 4096 + 4096 + NB * 4 * 257 * 2, 8 * NT * 4)
    arena = sb("arena", [128, ARENA_B // 2], BF16)
    qT = arena[:, 0:2048].rearrange("p (h t) -> p h t", h=4)
    kT = arena[:, 2048:4096].rearrange("p (h t) -> p h t", h=4)
    ktok = arena[:, 4096:6144].rearrange("p (b c) -> p b c", b=NB)
    vw = arena[:, 6144:6144 + NB * 4 * 257].rearrange("p (b h v) -> p b h v", b=NB, h=4)
    osb = arena[:, 0:8 * NT * 2].bitcast(F32).rearrange("p (k t) -> p k t", k=8)
    BqT = [S.buf("qT%d" % h) for h in range(4)]
    BkT = [S.buf("kT%d" % h) for h in range(4)]
    Bktok = [S.buf("ktok%d" % b) for b in range(NB)]
    Bvw = [S.buf("vw%d" % b) for b in range(NB)]
    Bosb = [S.buf("osb%d" % k) for k in range(8)]
    arena_m = BqT + BkT + Bktok + Bvw
    g_t = sb("g_t", [4, NT]);   Bg = S.buf("g")
    sp_t = sb("sp_t", [4, NT]); Bsp = S.buf("sp")
    nb_t = sb("nb_t", [4, NT]); Bnb = S.buf("nb")
    w_t = sb("w_t", [4, NT]);   Bw = S.buf("w")
    th_t = sb("th_t", [4, NT]); Bth = S.buf("th")
    sm4 = sb("sm4", [4, 64]);   Bsm4 = S.buf("sm4")
    mst = sb("mst", [4, L]);    Bmst = [S.buf("mst%d" % l) for l in range(L)]
    wthr = sb("wthr", [128, 48]); Bwthr = S.buf("wthr")
    Sm = [sb("Sm%d" % i, [128, 512], BF16) for i in range(2)]
    BSm = [S.buf("Sm%d" % i) for i in range(2)]
    Cst = sb("Cst", [128, L, 4, 257])
    BC = [[S.buf("C%d_%d" % (l, h)) for h in range(4)] for l in range(L)]
    Cd = sb("Cd", [128, 4, 257]);           BCd = [S.buf("Cd%d" % h) for h in range(4)]
    Cb = sb("Cb", [128, 4, 257], BF16);     BCb = [S.buf("Cb%d" % h) for h in range(4)]
    numb = sb("numb", [128, NB, 4, 256], BF16); Bnumb = [S.buf("numb%d" % b) for b in range(NB)]
    junk = sb("junk", [128, 256], BF16);    Bjunk = S.buf("junk")
    sm16 = sb("sm16", [128, 16 * 8]);       Bsm16 = S.buf("sm16")
    Bssr = S.buf("ssr"); Bdn = S.buf("dn"); Bsc = S.buf("sc")
    sz = [sb("sz%d" % i, [128, NT], BF16) for i in range(2)]
    Bsz = [S.buf("sz%d" % i) for i in range(2)]
    mixT = sb("mixT", [128, 16, NT], BF16); Bmix = [S.buf("mix%d" % e) for e in range(16)]
    ub = [sb("ub%d" % i, [128, NT], BF16) for i in range(3)];  Bub = [S.buf("ub%d" % i) for i in range(3)]
    cue = [sb("cue%d" % i, [128, NT + 2], BF16) for i in range(3)]; Bcue = [S.buf("cue%d" % i) for i in range(3)]
    Bb = [sb("Bb%d" % i, [128, NT], BF16) for i in range(3)];  BBb = [S.buf("Bb%d" % i) for i in range(3)]
    dg = sb("dg", [128, 8, 3, 128], BF16); Bdg = [S.buf("dg%d" % j) for j in range(8)]
    tails = sb("tails", [128, L, 8, 2], BF16); Btail = [[S.buf("tl%d_%d" % (l, j)) for j in range(8)] for l in range(L)]
    gsd = sb("gsd", [16, NT]);  Bgsd = S.buf("gsd")
    gr = sb("gr", [16, NT]);    Bgr = S.buf("gr")
    ghi = sb("ghi", [16, NT], BF16); Bghi = S.buf("ghi")
    glo = sb("glo", [16, NT], BF16); Bglo = S.buf("glo")
    tmp = [sb("tmp%d" % i, [128, NT]) for i in range(2)]; Btmp = [S.buf("tmp%d" % i) for i in range(2)]

    PS = [nc.alloc_psum_tensor("ps%d" % i, [128, 512], F32) for i in range(8)]
    BPS = [S.buf("ps%d" % i) for i in range(8)]
    for _b in BPS:
        _b.excl = True
    import os as _os
    pstate = {"i": int(_os.environ.get("PS0", "0")), "pinned": set()}

    POOLS = {'s': [0, 1, 2], 'c': [3, 4, 5, 6, 7], 'a': list(range(8))}
    pidx = {'s': 0, 'c': 0, 'a': 0}

    def psum(pool='a', pin=False):
        lst = POOLS[pool]
        while True:
            i = lst[pidx[pool] % len(lst)]
            pidx[pool] += 1
            if i not in pstate["pinned"]:
                break
        if pin:
            pstate["pinned"].add(i)
        return i

    def unpin(i):
        pstate["pinned"].discard(i)

    rr = {"ws": 0, "w4": 0, "sq": 0, "ev": 0}

    def nxt(key, n):
        i = rr[key]
        rr[key] = (i + 1) % n
        return i

    identF = cF[:, C_IDENT:C_IDENT + 128]
    identB = cB[:, C_IDENT:C_IDENT + 128]
    onesB = cB[:, C_ONES:C_ONES + 128]
    ones4 = cF[0:4, C_ONES:C_ONES + 128]
    indA = cB[:, C_INDA:C_INDA + 128].rearrange("p (j g) -> p j g", j=8)
    indB = cB[0:16, C_INDB:C_INDB + 1024].rearrange("p (j c) -> p j c", j=8)

    pe, act, dve, pool, sp = S.pe, S.act, S.dve, S.pool, S.sp
    T = nc.tensor
    A = nc.scalar
    V = nc.vector
    G = nc.gpsimd

    def pvcol(l, off):
        return pv[:, l * PV_PER + off: l * PV_PER + off + 1]

    S.dma(sp, cF[:], cst_d[:, :], BcF, writes=[BcF])
    S.dma(sp, pv[:], pv_d[:, :], Bpv, writes=[Bpv])
    S.dma(sp, gb[:], gb_d[:, :], Bgb, writes=[Bgb])
    S.dma(sp, wgf[:], wg_d[:, :], Bwgf, writes=[Bwgf])
    S.op(dve, lambda: V.tensor_copy(out=cB[:], in_=cF[:]), reads=[BcF], writes=[BcB])
    for h in range(4):
        S.op(dve, lambda h=h: V.tensor_copy(out=maskb[:, h, :], in_=cF[:, C_MASK:C_MASK + 128]),
             reads=[BcF], writes=[Bmask])
    S.op(dve, lambda: V.tensor_copy(out=wgb[:], in_=wgf[:]), reads=[Bwgf], writes=[Bwgb])
    for l in range(L):
        S.op(dve, lambda l=l: V.tensor_scalar(out=nbf[:, l:l + 1], in0=gb[:, 2 * l + 1:2 * l + 2], scalar1=-1.0,
                                               scalar2=None, op0=ALU.mult), reads=[Bgb], writes=[Bnbf])

    Bwc = [[S.buf("wconv%d_%d" % (l, i)) for i in range(9)] for l in range(L)]

    def convert_layer(l):
        for pi, s0 in enumerate(range(0, NSLAB, 8)):
            S.dma(pool, wsb_d[l, s0:s0 + 8].rearrange("s p f -> (s p) f"),
                  ws_d[l, s0:s0 + 8].rearrange("s p f -> (s p) f"), Bwc[l][pi], writes=[Bwc[l][pi]])
        S.dma(pool, wvb_d[l].rearrange("s p f -> (s p) f"), wv_d[l].rearrange("s p f -> (s p) f"), Bwc[l][7],
              writes=[Bwc[l][7]])
        S.dma(pool, wob_d[l].rearrange("s p f -> (s p) f"), wo_d[l].rearrange("s p f -> (s p) f"), Bwc[l][8],
              writes=[Bwc[l][8]])

    convert_layer(0)

    def evac_eng():
        i = nxt("ev", 2)
        return i

    def copy_on(which, out, in_, reads, writes, scale=None):
        if which == 0:
            if scale is None:
                return S.op(act, lambda: A.activation(out=out, in_=in_, func=AF.Copy), reads=reads, writes=writes)
            return S.op(act, lambda: A.activation(out=out, in_=in_, func=AF.Copy, scale=scale), reads=reads,
                        writes=writes)
        if scale is None:
            return S.op(dve, lambda: V.tensor_copy(out=out, in_=in_), reads=reads, writes=writes)
        return S.op(dve, lambda: V.tensor_scalar(out=out, in0=in_, scalar1=scale, scalar2=None, op0=ALU.mult),
                    reads=reads, writes=writes)

    def load_slab(l, s):
        r = nxt("ws", NWS)
        S.dma(sp, wsl[r][:], wsb_d[l, s], Bwsl[r], reads=[Bwc[l][s // 8]], writes=[Bwsl[r]])
        return r

    def slab_mm(l, s, M=128):
        r = load_slab(l, s)
        b = psum('s')
        for k in range(8):
            S.op(pe, lambda k=k: T.matmul(PS[b][0:M, :], lhsT=wsl[r][:, k * 128:k * 128 + M], rhs=hT[:, k, :],
                                          start=(k == 0), stop=(k == 7)),
                 reads=[Bwsl[r], BhT[k]], writes=[BPS[b]])
        return b

    dbg_bufs = []

    def dump(nm, src_ap, bufs):
        if nm in dbg_d:
            t = S.buf("dbgtmp_" + nm)
            S.dma(pool, dbg_d[nm], src_ap, t, reads=bufs, writes=[t])
            dbg_bufs.append(t)

    def layer(l, ti, first_in_seq):
        if ti == 0 and l + 1 < L:
            convert_layer(l + 1)
        if l == L - 1 and ti + 1 < NTILES:
            load_x(ti + 1)
        for j in range(8):
            for tap in range(3):
                S.op(pool, lambda: G.tensor_scalar(out=dg[:, j, tap, :], in0=identB,
                                                   scalar1=pvcol(l, PV_CONVW + j * 3 + tap), scalar2=1.0,
                                                   op0=ALU.mult, op1=ALU.mult), reads=[BcB, Bpv], writes=[Bdg[j]])
        bss = psum('s', pin=True)
        for k in range(8):
            i = nxt("sq", NSQ)
            S.op(act, lambda: A.activation(out=sq[i][:], in_=xT[:, k, :], func=AF.Square),
                 reads=[BxT[k]], writes=[Bsq[i]])
            S.op(pe, lambda: T.matmul(PS[bss][:], lhsT=onesB, rhs=sq[i][:], start=(k == 0), stop=(k == 7)),
                 reads=[Bsq[i], BcB], writes=[BPS[bss]])
        S.op(act, lambda: A.activation(out=sd[:], in_=PS[bss][:], func=AF.Ln, scale=1.0 / 1024, bias=EPSB),
             reads=[BPS[bss], Beps], writes=[Bsd])
        S.op(act, lambda: A.activation(out=PS[bss][:], in_=sd[:], func=AF.Exp, scale=-0.5), reads=[Bsd],
             writes=[BPS[bss]])
        for k in range(8):
            S.op(dve, lambda: V.scalar_tensor_tensor(out=hT[:, k, :], in0=xT[:, k, :], scalar=pvcol(l, PV_GPRE + k),
                                                     in1=PS[bss][:], op0=ALU.mult, op1=ALU.mult),
                 reads=[BxT[k], BPS[bss], Bpv], writes=[BhT[k]])
        unpin(bss)

        bi = psum('c')
        bf = psum('c')
        for (bb, c0) in ((bi, 0), (bf, 4)):
            for k in range(8):
                S.op(pe, lambda: T.matmul(PS[bb][0:4, :], lhsT=wgb[:, l * 64 + k * 8 + c0: l * 64 + k * 8 + c0 + 4],
                                          rhs=hT[:, k, :], start=(k == 0), stop=(k == 7)),
                     reads=[Bwgb, BhT[k]], writes=[BPS[bb]])
        S.op(dve, lambda: V.tensor_scalar(out=g_t[:], in0=PS[bi][0:4, :], scalar1=gb[:, 2 * l:2 * l + 1], scalar2=None,
                                          op0=ALU.add), reads=[BPS[bi], Bgb], writes=[Bg])
        S.op(act, lambda: A.activation(out=sp_t[:], in_=PS[bf][0:4, :], func=AF.Exp, scale=-1.0, bias=nbf[:, l:l + 1]),
             reads=[BPS[bf], Bnbf], writes=[Bsp])
        S.op(act, lambda: A.activation(out=sp_t[:], in_=sp_t[:], func=AF.Ln, bias=ONEB[0:4, :]), reads=[Bsp, Beps],
             writes=[Bsp])
        def q_slab(h):
            b = slab_mm(l, h)
            S.op(act, lambda: A.activation(out=qT[:, h, :], in_=PS[b][:], func=AF.Copy, scale=float(128 ** -0.5)),
                 reads=[BPS[b]], writes=[BqT[h]] + Bosb)

        q_slab(0)
        for blk in range(NB):
            S.op(dve, lambda: V.tensor_tensor_scan(out=nb_t[:, blk * 128:(blk + 1) * 128], data0=ones4,
                                                   data1=sp_t[:, blk * 128:(blk + 1) * 128], initial=0.0,
                                                   op0=ALU.mult, op1=ALU.add),
                 reads=[Bsp, BcF], writes=[Bnb])
        S.op(dve, lambda: V.tensor_tensor(out=g_t[:], in0=g_t[:], in1=nb_t[:], op=ALU.add), reads=[Bg, Bnb],
             writes=[Bg])
        S.op(dve, lambda: V.tensor_reduce(out=sm4[:, 0:4], in_=g_t[:].rearrange("p (b t) -> p b t", b=NB),
                                          op=ALU.max, axis=AX.X), reads=[Bg], writes=[Bsm4])
        for blk in range(NB):
            mprev = mst[:, l:l + 1] if blk == 0 else sm4[:, 12 + blk - 1:12 + blk]
            S.op(dve, lambda: V.tensor_tensor(out=sm4[:, 4 + blk:5 + blk], in0=mprev, in1=sm4[:, blk:blk + 1],
                                              op=ALU.max), reads=[Bsm4, Bmst[l]], writes=[Bsm4])
            S.op(dve, lambda: V.tensor_tensor(out=sm4[:, 8 + blk:9 + blk], in0=mprev, in1=sm4[:, 4 + blk:5 + blk],
                                              op=ALU.subtract), reads=[Bsm4, Bmst[l]], writes=[Bsm4])
            S.op(dve, lambda: V.tensor_tensor(out=sm4[:, 12 + blk:13 + blk], in0=sm4[:, 4 + blk:5 + blk],
                                              in1=nb_t[:, blk * 128 + 127:blk * 128 + 128], op=ALU.subtract),
                 reads=[Bsm4, Bnb], writes=[Bsm4])
        S.op(dve, lambda: V.tensor_copy(out=mst[:, l:l + 1], in_=sm4[:, 15:16]), reads=[Bsm4], writes=[Bmst[l]])
        S.op(dve, lambda: V.tensor_scalar(out=sm4[:, 16:20], in0=sm4[:, 4:8], scalar1=-1.0, scalar2=None,
                                          op0=ALU.mult), reads=[Bsm4], writes=[Bsm4])
        for h in range(1, 4):
            q_slab(h)
        for blk in range(NB):
            sl = slice(blk * 128, (blk + 1) * 128)
            S.op(act, lambda: A.activation(out=w_t[:, sl], in_=g_t[:, sl], func=AF.Exp,
                                           bias=sm4[:, 16 + blk:17 + blk]), reads=[Bg, Bsm4], writes=[Bw])
            S.op(act, lambda: A.activation(out=th_t[:, sl], in_=nb_t[:, sl], func=AF.Exp,
                                           bias=sm4[:, 16 + blk:17 + blk]), reads=[Bnb, Bsm4], writes=[Bth])
        S.op(act, lambda: A.activation(out=sm4[:, 20:24], in_=sm4[:, 8:12], func=AF.Exp), reads=[Bsm4],
             writes=[Bsm4])
        for h in range(4):
            b = slab_mm(l, 4 + h)
            S.op(dve, lambda: V.tensor_copy(out=kT[:, h, :], in_=PS[b][:]), reads=[BPS[b]], writes=[BkT[h]] + Bosb)
        for blk in range(NB):
            S.op(dve, lambda: V.tensor_scalar(out=sm4[:, 32 + blk * 4:36 + blk * 4], in0=cF[0:4, C_IDENT:C_IDENT + 4],
                                              scalar1=sm4[:, 20 + blk:21 + blk], scalar2=None, op0=ALU.mult),
                 reads=[Bsm4, BcF], writes=[Bsm4])

        for blk in range(NB):
            b = psum('c')
            pb = PS[b][:].bitcast(BF16)
            for h in range(4):
                S.op(pe, lambda: T.transpose(out=pb[:, h * 128:(h + 1) * 128], in_=kT[:, h, blk * 128:(blk + 1) * 128],
                                             identity=identB), reads=[BkT[h], BcB], writes=[BPS[b]])
            copy_on(blk % 2, ktok[:, blk, :], pb[:, 0:512], [BPS[b]], [Bktok[blk]] + Bosb)
        bw = psum('c')
        for blk in range(NB):
            sl = slice(blk * 128, (blk + 1) * 128)
            S.op(pe, lambda: T.transpose(out=PS[bw][:, blk * 8:blk * 8 + 4], in_=w_t[:, sl],
                                         identity=cF[0:4, C_IDENT:C_IDENT + 4]), reads=[Bw, BcF], writes=[BPS[bw]])
            S.op(pe, lambda: T.transpose(out=PS[bw][:, blk * 8 + 4:blk * 8 + 8], in_=th_t[:, sl],
                                         identity=cF[0:4, C_IDENT:C_IDENT + 4]), reads=[Bth, BcF], writes=[BPS[bw]])
        S.op(pe, lambda: T.matmul(PS[bw][:, 32:48], lhsT=ones4, rhs=sm4[:, 32:48], start=True, stop=True),
             reads=[Bsm4, BcF], writes=[BPS[bw]])
        S.op(dve, lambda: V.tensor_copy(out=wthr[:, 0:48], in_=PS[bw][:, 0:48]), reads=[BPS[bw]], writes=[Bwthr])
        S.op(dve, lambda: V.tensor_copy(out=vw[:, :, :, 256],
                                        in_=wthr[:, 0:32].rearrange("p (b e) -> p b e", e=8)[:, :, 0:4]),
             reads=[Bwthr], writes=Bvw + Bosb)
        for h in range(4):
            r = nxt("w4", NW4)
            S.dma(sp, w4k[r][:], wvb_d[l, h], Bw4k[r], reads=[Bwc[l][7]], writes=[Bw4k[r]])
            for blk in range(NB):
                b = psum('s')
                for k in range(8):
                    S.op(pe, lambda: T.matmul(PS[b][:, 0:256], lhsT=hT[:, k, blk * 128:(blk + 1) * 128],
                                              rhs=w4k[r][:, k * 256:(k + 1) * 256], start=(k == 0), stop=(k == 7)),
                         reads=[Bw4k[r], BhT[k]], writes=[BPS[b]])
                S.op(dve, lambda: V.tensor_scalar(out=vw[:, blk, h, 0:256], in0=PS[b][:, 0:256],
                                                  scalar1=wthr[:, blk * 8 + h:blk * 8 + h + 1], scalar2=None,
                                                  op0=ALU.mult), reads=[BPS[b], Bwthr], writes=[Bvw[blk]])

        def slab_o(e):
            b = slab_mm(l, 8 + e)
            S.op(act, lambda: A.activation(out=mixT[:, e, :], in_=PS[b][:], func=AF.Sigmoid), reads=[BPS[b]],
                 writes=[Bmix[e]])

        def slab_z(e):
            b = slab_mm(l, 16 + e)
            i = e % 2
            S.op(act, lambda: A.activation(out=sz[i][:], in_=PS[b][:], func=AF.Silu), reads=[BPS[b]], writes=[Bsz[i]])
            S.op(dve, lambda: V.tensor_tensor(out=mixT[:, e, :], in0=mixT[:, e, :], in1=sz[i][:], op=ALU.mult),
                 reads=[Bmix[e], Bsz[i]], writes=[Bmix[e]])

        fill = [(lambda e=e: slab_o(e)) for e in range(8)] + [(lambda e=e: slab_z(e)) for e in range(8)]

        def filler(n):
            for _ in range(n):
                if fill:
                    fill.pop(0)()

        for blk in range(NB):
            sl = slice(blk * 128, (blk + 1) * 128)
            for h in range(4):
                S.op(dve, lambda: V.tensor_scalar(out=Cd[:, h, :], in0=Cst[:, l, h, :],
                                                  scalar1=wthr[:, 32 + blk * 4 + h:33 + blk * 4 + h], scalar2=None,
                                                  op0=ALU.mult), reads=[BC[l][h], Bwthr], writes=[BCd[h]])
                S.op(act, lambda: A.activation(out=Cb[:, h, :], in_=Cd[:, h, :], func=AF.Copy), reads=[BCd[h]],
                     writes=[BCb[h]])
            bS = psum('c')
            for h in range(4):
                S.op(pe, lambda: T.matmul(PS[bS][:, h * 128:(h + 1) * 128], lhsT=kT[:, h, sl], rhs=qT[:, h, sl],
                                          start=True, stop=True), reads=[BkT[h], BqT[h]], writes=[BPS[bS]])
            si = blk % 2
            S.op(dve, lambda: V.tensor_tensor(out=Sm[si][:], in0=PS[bS][:], in1=maskb[:].rearrange("p h t -> p (h t)"),
                                              op=ALU.mult), reads=[BPS[bS], Bmask], writes=[BSm[si]])
            filler(2)
            bN = [psum('c'), psum('c')]
            bD = psum('c')
            for h in range(4):
                on = PS[bN[h // 2]][:, (h % 2) * 256:(h % 2) * 256 + 256]
                S.op(pe, lambda: T.matmul(on, lhsT=Sm[si][:, h * 128:(h + 1) * 128], rhs=vw[:, blk, h, 0:256],
                                          start=True, stop=False), reads=[BSm[si], Bvw[blk]], writes=[BPS[bN[h // 2]]])
                S.op(pe, lambda: T.matmul(on, lhsT=qT[:, h, sl], rhs=Cb[:, h, 0:256], start=False, stop=True),
                     reads=[BqT[h], BCb[h]], writes=[BPS[bN[h // 2]]])
                S.op(pe, lambda: T.matmul(PS[bD][:, h:h + 1], lhsT=Sm[si][:, h * 128:(h + 1) * 128],
                                          rhs=vw[:, blk, h, 256:257], start=True, stop=False),
                     reads=[BSm[si], Bvw[blk]], writes=[BPS[bD]])
                S.op(pe, lambda: T.matmul(PS[bD][:, h:h + 1], lhsT=qT[:, h, sl], rhs=Cb[:, h, 256:257], start=False,
                                          stop=True), reads=[BqT[h], BCb[h]], writes=[BPS[bD]])
            bC = [bS, psum('c')]
            for h in range(4):
                S.op(pe, lambda: T.matmul(PS[bC[h // 2]][:, (h % 2) * 256:(h % 2) * 256 + 256],
                                          lhsT=ktok[:, blk, h * 128:(h + 1) * 128], rhs=vw[:, blk, h, 0:256],
                                          start=True, stop=True), reads=[Bktok[blk], Bvw[blk]],
                     writes=[BPS[bC[h // 2]]])
                S.op(pe, lambda: T.matmul(PS[bD][:, 4 + h:5 + h], lhsT=ktok[:, blk, h * 128:(h + 1) * 128],
                                          rhs=vw[:, blk, h, 256:257], start=True, stop=True),
                     reads=[Bktok[blk], Bvw[blk]], writes=[BPS[bD]])
            for h in range(4):
                S.op(dve, lambda: V.tensor_tensor(out=Cst[:, l, h, 0:256], in0=Cd[:, h, 0:256],
                                                  in1=PS[bC[h // 2]][:, (h % 2) * 256:(h % 2) * 256 + 256],
                                                  op=ALU.add), reads=[BCd[h], BPS[bC[h // 2]]], writes=[BC[l][h]])
            S.op(dve, lambda: V.tensor_tensor(out=Cst[:, l, :, 256], in0=Cd[:, :, 256], in1=PS[bD][:, 4:8],
                                              op=ALU.add), reads=BCd + [BPS[bD]], writes=BC[l])
            S.op(act, lambda: A.activation(out=sm16[:, blk * 4:blk * 4 + 4], in_=PS[bD][:, 0:4], func=AF.Abs),
                 reads=[BPS[bD]], writes=[Bdn])
            S.op(dve, lambda: V.tensor_tensor(out=sm16[:, blk * 4:blk * 4 + 4], in0=sm16[:, blk * 4:blk * 4 + 4],
                                              in1=wthr[:, blk * 8 + 4:blk * 8 + 8], op=ALU.max),
                 reads=[Bdn, Bwthr], writes=[Bdn])
            for h in range(4):
                S.op(act, lambda: A.activation(out=junk[:], in_=PS[bN[h // 2]][:, (h % 2) * 256:(h % 2) * 256 + 256],
                                               func=AF.Square,
                                               accum_out=sm16[:, 16 + blk * 4 + h:17 + blk * 4 + h]),
                     reads=[BPS[bN[h // 2]]], writes=[Bjunk, Bssr])
            for pr in range(2):
                S.op(dve, lambda: V.tensor_copy(out=numb[:, blk, 2 * pr:2 * pr + 2, :].rearrange("p a v -> p (a v)"),
                                                in_=PS[bN[pr]][:]), reads=[BPS[bN[pr]]], writes=[Bnumb[blk]])
            filler(2)
        S.op(dve, lambda: V.reciprocal(out=sm16[:, 32:48], in_=sm16[:, 0:16]), reads=[Bdn], writes=[Bsm16])
        S.op(dve, lambda: V.tensor_tensor(out=sm16[:, 48:64], in0=sm16[:, 32:48], in1=sm16[:, 32:48], op=ALU.mult),
             reads=[Bsm16], writes=[Bsm16])
        S.op(dve, lambda: V.tensor_tensor(out=sm16[:, 64:80], in0=sm16[:, 48:64], in1=sm16[:, 16:32], op=ALU.mult),
             reads=[Bsm16, Bssr], writes=[Bsm16])
        S.op(act, lambda: A.activation(out=sm16[:, 80:96], in_=sm16[:, 64:80], func=AF.Ln, scale=1.0 / 256,
                                       bias=EPSB), reads=[Bsm16, Beps], writes=[Bsm16])
        S.op(act, lambda: A.activation(out=sm16[:, 96:112], in_=sm16[:, 80:96], func=AF.Exp, scale=-0.5),
             reads=[Bsm16], writes=[Bsm16])
        S.op(dve, lambda: V.tensor_tensor(out=sm16[:, 112:128], in0=sm16[:, 96:112], in1=sm16[:, 32:48],
                                          op=ALU.mult), reads=[Bsm16], writes=[Bsc])
        for blk in range(NB):
            S.op(dve, lambda: V.tensor_tensor(out=numb[:, blk, :, :], in0=numb[:, blk, :, :],
                                              in1=sm16[:, 112 + blk * 4:116 + blk * 4].unsqueeze(2).broadcast_to(
                                                  [128, 4, 256]), op=ALU.mult),
                 reads=[Bnumb[blk], Bsc], writes=[Bnumb[blk]])
        filler(100)

        bg = psum('c', pin=True)

        def conv_a(j):
            i = j % 3
            S.op(pool, lambda: G.tensor_copy(out=cue[i][:, 0:2], in_=tails[:, l, j, :]), reads=[Btail[l][j]],
                 writes=[Bcue[i]])
            bu = slab_mm(l, 24 + 4 * j + 0)
            S.op(act, lambda: A.activation(out=ub[i][:], in_=PS[bu][:], func=AF.Copy), reads=[BPS[bu]],
                 writes=[Bub[i]])
            bc = slab_mm(l, 24 + 4 * j + 1)
            S.op(dve, lambda: V.tensor_tensor(out=cue[i][:, 2:NT + 2], in0=PS[bc][:], in1=ub[i][:], op=ALU.mult),
                 reads=[BPS[bc], Bub[i]], writes=[Bcue[i]])
            S.op(pool, lambda: G.tensor_copy(out=tails[:, l, j, :], in_=cue[i][:, NT:NT + 2]), reads=[Bcue[i]],
                 writes=[Btail[l][j]])
            bB = slab_mm(l, 24 + 4 * j + 2)
            S.op(act, lambda: A.activation(out=Bb[i][:], in_=PS[bB][:], func=AF.Copy), reads=[BPS[bB]],
                 writes=[BBb[i]])

        def conv_b(j):
            i = j % 3
            by = psum('c')
            for tap in range(3):
                S.op(pe, lambda: T.matmul(PS[by][:], lhsT=dg[:, j, tap, :], rhs=cue[i][:, tap:tap + NT],
                                          start=(tap == 0), stop=(tap == 2)), reads=[Bdg[j], Bcue[i]],
                     writes=[BPS[by]])
            S.op(dve, lambda: V.tensor_tensor(out=mixT[:, 8 + j, :], in0=PS[by][:], in1=Bb[i][:], op=ALU.mult),
                 reads=[BPS[by], BBb[i]], writes=[Bmix[8 + j]])
            qi = nxt("sq", NSQ)
            S.op(act, lambda: A.activation(out=sq[qi][:], in_=mixT[:, 8 + j, :], func=AF.Square),
                 reads=[Bmix[8 + j]], writes=[Bsq[qi]])
            bz = slab_mm(l, 24 + 4 * j + 3)
            S.op(act, lambda: A.activation(out=sz[j % 2][:], in_=PS[bz][:], func=AF.Silu), reads=[BPS[bz]],
                 writes=[Bsz[j % 2]])
            S.op(pe, lambda: T.matmul(PS[bg][0:16, :], lhsT=indA[:, j, :], rhs=sq[qi][:], start=(j == 0),
                                      stop=(j == 7)), reads=[Bsq[qi], BcB], writes=[BPS[bg]])
            S.op(dve, lambda: V.tensor_tensor(out=mixT[:, 8 + j, :], in0=mixT[:, 8 + j, :], in1=sz[j % 2][:],
                                              op=ALU.mult), reads=[Bmix[8 + j], Bsz[j % 2]], writes=[Bmix[8 + j]])

        def ym2(e0):
            b = psum('c')
            pb = PS[b][:].bitcast(BF16)
            for e in (e0, e0 + 1):
                h, half = e // 2, e % 2
                for blk in range(NB):
                    S.op(pe, lambda: T.transpose(out=pb[:, (e - e0) * 512 + blk * 128:(e - e0) * 512 + (blk + 1) * 128],
                                                 in_=numb[:, blk, h, half * 128:(half + 1) * 128], identity=identB),
                         reads=[Bnumb[blk], BcB], writes=[BPS[b]])
            return b

        def ym2_fin(e0, b):
            pb = PS[b][:].bitcast(BF16)
            for e in (e0, e0 + 1):
                S.op(dve, lambda: V.scalar_tensor_tensor(out=mixT[:, e, :], in0=pb[:, (e - e0) * 512:(e - e0 + 1) * 512],
                                                         scalar=pvcol(l, PV_GHEAD + e), in1=mixT[:, e, :],
                                                         op0=ALU.mult, op1=ALU.mult),
                     reads=[BPS[b], Bmix[e], Bpv], writes=[Bmix[e]])

        conv_a(0)
        conv_a(1)
        ybanks = [(e0, ym2(e0)) for e0 in (0, 2, 4, 6)]
        for e0, yb in ybanks:
            ym2_fin(e0, yb)
        for j in range(8):
            conv_b(j)
            if j + 2 < 8:
                conv_a(j + 2)
        S.op(act, lambda: A.activation(out=gsd[:], in_=PS[bg][0:16, :], func=AF.Ln, scale=1.0 / 64,
                                       bias=EPSB[0:16, :]), reads=[BPS[bg], Beps], writes=[Bgsd])
        unpin(bg)
        S.op(act, lambda: A.activation(out=gr[:], in_=gsd[:], func=AF.Exp, scale=-0.5), reads=[Bgsd], writes=[Bgr])
        S.op(dve, lambda: V.tensor_copy(out=ghi[:], in_=gr[:]), reads=[Bgr], writes=[Bghi])
        S.op(dve, lambda: V.tensor_tensor(out=glo[:], in0=gr[:], in1=ghi[:], op=ALU.subtract), reads=[Bgr, Bghi],
             writes=[Bglo])
        bss = psum('c', pin=True)
        oslab = {}

        def op_load(dc):
            r = nxt("w4", NW4)
            S.dma(sp, w4k[r][:], wob_d[l, dc], Bw4k[r], reads=[Bwc[l][8]], writes=[Bw4k[r]])
            oslab[dc] = (r, psum('s'))

        def op_mm(dc, e0, e1):
            r, b = oslab[dc]
            for e in range(e0, e1):
                S.op(pe, lambda: T.matmul(PS[b][:], lhsT=w4k[r][:, e * 128:(e + 1) * 128], rhs=mixT[:, e, :],
                                          start=(e == 0), stop=(e == 15)), reads=[Bw4k[r], Bmix[e]],
                     writes=[BPS[b]])

        def op_evac(dc):
            r, b = oslab[dc]
            S.op(act, lambda: A.activation(out=osb[:, dc, :], in_=PS[b][:], func=AF.Copy), reads=[BPS[b]],
                 writes=[Bosb[dc]] + arena_m)
            qi = nxt("sq", NSQ)
            S.op(dve, lambda: V.tensor_tensor(out=sq[qi][:], in0=PS[b][:], in1=osb[:, dc, :], op=ALU.mult),
                 reads=[BPS[b], Bosb[dc]], writes=[Bsq[qi]])
            oslab[dc] = (r, b, qi)

        def op_ss(dc):
            qi = oslab[dc][2]
            S.op(pe, lambda: T.matmul(PS[bss][:], lhsT=onesB, rhs=sq[qi][:], start=(dc == 0), stop=(dc == 7)),
                 reads=[Bsq[qi], BcB], writes=[BPS[bss]])

        for dc in range(3):
            op_load(dc)
            op_mm(dc, 0, 8)
        for j in range(8):
            b = psum('c')
            S.op(pe, lambda: T.matmul(PS[b][:], lhsT=indB[:, j, :], rhs=ghi[:], start=True, stop=False),
                 reads=[Bghi, BcB], writes=[BPS[b]])
            S.op(pe, lambda: T.matmul(PS[b][:], lhsT=indB[:, j, :], rhs=glo[:], start=False, stop=True),
                 reads=[Bglo, BcB], writes=[BPS[b]])
            S.op(dve, lambda: V.scalar_tensor_tensor(out=mixT[:, 8 + j, :], in0=mixT[:, 8 + j, :],
                                                     scalar=pvcol(l, PV_GCONV + j), in1=PS[b][:], op0=ALU.mult,
                                                     op1=ALU.mult), reads=[Bmix[8 + j], BPS[b], Bpv],
                 writes=[Bmix[8 + j]])
        for dc in range(8):
            if dc >= 3:
                op_load(dc)
                op_mm(dc, 0, 8)
            op_mm(dc, 8, 16)
            op_evac(dc)
            if dc > 0:
                op_ss(dc - 1)
        op_ss(7)
        S.op(act, lambda: A.activation(out=sd[:], in_=PS[bss][:], func=AF.Ln, scale=1.0 / 1024, bias=EPSB),
             reads=[BPS[bss], Beps], writes=[Bsd])
        S.op(act, lambda: A.activation(out=PS[bss][:], in_=sd[:], func=AF.Exp, scale=-0.5), reads=[Bsd],
             writes=[BPS[bss]])
        for k in range(8):
            i = k % 2
            S.op(dve, lambda: V.scalar_tensor_tensor(out=tmp[i][:], in0=osb[:, k, :], scalar=pvcol(l, PV_GPOST + k),
                                                     in1=PS[bss][:], op0=ALU.mult, op1=ALU.mult),
                 reads=[Bosb[k], BPS[bss], Bpv], writes=[Btmp[i]])
            S.op(dve, lambda: V.tensor_tensor(out=xT[:, k, :], in0=xT[:, k, :], in1=tmp[i][:], op=ALU.add),
                 reads=[BxT[k], Btmp[i]], writes=[BxT[k]])
        unpin(bss)

    epsb = sb("epsb", [128, 2])
    Beps = S.buf("eps")
    S.op(dve, lambda: V.memset(epsb[:, 0:1], EPS), writes=[Beps])
    S.op(dve, lambda: V.memset(epsb[:, 1:2], 1.0), writes=[Beps])
    EPSB = epsb[:, 0:1]
    ONEB = epsb[:, 1:2]

    def load_x(ti):
        for blk in range(NB):
            S.dma(sp, xst[blk][:], x_d[ti * NT + blk * 128: ti * NT + (blk + 1) * 128, :], Bxst[blk],
                  writes=[Bxst[blk]])

    for ti in range(NTILES):
        tis = ti % TPS
        tok0 = ti * NT
        if tis == 0:
            S.op(pool, lambda: G.memset(Cst[:].rearrange("p l h v -> p (l h v)"), 0.0),
                 writes=[b for bl in BC for b in bl])
            S.op(pool, lambda: G.memset(mst[:], 0.0), writes=Bmst)
            S.op(pool, lambda: G.memset(tails[:].rearrange("p l j t -> p (l j t)"), 0.0),
                 writes=[b for bl in Btail for b in bl])
        if ti == 0:
            load_x(0)
        for blk in range(NB):
            i = blk
            for half in range(2):
                b = psum()
                for kk in range(4):
                    k = half * 4 + kk
                    S.op(pe, lambda: T.transpose(out=PS[b][:, kk * 128:(kk + 1) * 128],
                                                 in_=xst[i][:, k * 128:(k + 1) * 128], identity=identF),
                         reads=[Bxst[i], BcF], writes=[BPS[b]])
                copy_on(half, xT[:, half * 4:half * 4 + 4, blk * 128:(blk + 1) * 128],
                        PS[b][:].rearrange("p (a t) -> p a t", a=4), [BPS[b]], BxT[half * 4:half * 4 + 4])
        for l in range(L):
            layer(l, ti, tis == 0)
        for blk in range(NB):
            i = blk % 2
            for half in range(2):
                b = psum()
                for kk in range(4):
                    k = half * 4 + kk
                    S.op(pe, lambda: T.transpose(out=PS[b][:, kk * 128:(kk + 1) * 128],
                                                 in_=xT[:, k, blk * 128:(blk + 1) * 128], identity=identF),
                         reads=[BxT[k], BcF], writes=[BPS[b]])
                copy_on(half, ost[i][:, half * 512:(half + 1) * 512], PS[b][:], [BPS[b]], [Bost[i]])
            S.dma(pool, y_d[tok0 + blk * 128: tok0 + (blk + 1) * 128, :], ost[i][:], Bost[i], reads=[Bost[i]])
    S.wait_all(pool, Bost)
    S.wait_all(sp, Bost + dbg_bufs)
    return nc, S


def _slab_cols():
    cols = []
    for h in range(4):
        cols.append(0 + 128 * h)
    for h in range(4):
        cols.append(512 + 128 * h)
    for e in range(8):
        cols.append(2048 + 128 * e)
    for e in range(8):
        cols.append(3072 + 128 * e)
    for j in range(8):
        cols.append(4104 + 128 * j)
        cols.append(6152 + 128 * j)
        cols.append(5128 + 128 * j)
        cols.append(7176 + 128 * j)
    return cols


def _consts():
    c = np.zeros((128, C_TOT), np.float32)
    c[:, C_IDENT:C_IDENT + 128] = np.eye(128, dtype=np.float32)
    c[:, C_ONES:C_ONES + 128] = 1.0
    s = np.arange(128)
    c[:, C_MASK:C_MASK + 128] = (s[:, None] <= s[None, :]).astype(np.float32)
    indA = np.zeros((128, 8, 16), np.float32)
    indB = np.zeros((128, 8, 128), np.float32)
    for j in range(8):
        for p in range(128):
            g = 2 * j + (1 if p >= 64 else 0)
            indA[p, j, g] = 1.0
            indB[g, j, p] = 1.0
    c[:, C_INDA:C_INDA + 128] = indA.reshape(128, 128)
    c[:, C_INDB:C_INDB + 1024] = indB.reshape(128, 1024)
    return c


def _prep_layers(layers, norm_pre, norm_post, w_in, b_igate, b_fgate, head_norm, conv_w, conv_norm, w_out):
    L = len(layers)
    cols = np.array(_slab_cols())
    colidx = (cols[:, None] + np.arange(128)[None, :])
    ws = np.empty((L, NSLAB, 128, 1024), np.float32)
    wv = np.empty((L, 4, 128, 2048), np.float32)
    wo = np.empty((L, 8, 128, 2048), np.float32)
    wg = np.empty((128, L * 64), np.float32)
    pv = np.empty((128, L * PV_PER), np.float32)
    gb = np.empty((4, L * 2), np.float32)
    for li, l in enumerate(layers):
        W = np.asarray(w_in[l]).reshape(8, 128, 8200)
        ws[li] = W[:, :, colidx].transpose(2, 1, 0, 3).reshape(NSLAB, 128, 1024)
        wv[li] = W[:, :, 1024:2048].reshape(8, 128, 4, 256).transpose(2, 1, 0, 3).reshape(4, 128, 2048)
        wo[li] = np.asarray(w_out[l]).reshape(16, 128, 8, 128).transpose(2, 1, 0, 3).reshape(8, 128, 2048)
        wg[:, li * 64:(li + 1) * 64] = W[:, :, 4096:4104].transpose(1, 0, 2).reshape(128, 64)
        o = li * PV_PER
        pv[:, o + PV_GPRE:o + PV_GPRE + 8] = np.asarray(norm_pre[l]).reshape(8, 128).T
        pv[:, o + PV_GPOST:o + PV_GPOST + 8] = np.asarray(norm_post[l]).reshape(8, 128).T
        pv[:, o + PV_GHEAD:o + PV_GHEAD + 8] = np.asarray(head_norm[l]).reshape(8, 128).T
        pv[:, o + PV_GCONV:o + PV_GCONV + 8] = np.asarray(conv_norm[l]).reshape(8, 128).T
        pv[:, o + PV_CONVW:o + PV_CONVW + 24] = np.asarray(conv_w[l]).reshape(3, 8, 128).transpose(2, 1, 0).reshape(128, 24)
        gb[:, 2 * li] = np.asarray(b_igate[l])
        gb[:, 2 * li + 1] = np.asarray(b_fgate[l])
    return dict(ws=ws, wv=wv, wo=wo, wg=wg, pv=pv, gb=gb, cst=_consts())


_PROG_CACHE = {}


def _get_prog(L, NTILES, TPS):
    key = (L, NTILES, TPS)
    if key not in _PROG_CACHE:
        _PROG_CACHE[key] = build(L, NTILES, TPS)[0]
    return _PROG_CACHE[key]


FUSED = True


def kernel(x, norm_pre, norm_post, w_in, b_igate, b_fgate, head_norm, conv_w, conv_norm, w_out):
    x = np.asarray(x, dtype=np.float32)
    Bt, Sq, D = x.shape
    per = Bt // NCORES
    TPS = Sq // NT
    NTILES = per * TPS
    xs = [np.ascontiguousarray(x[c * per:(c + 1) * per].reshape(per * Sq, D)) for c in range(NCORES)]
    groups = [list(range(4))] if FUSED else [[l] for l in range(4)]
    for layers in groups:
        prm = _prep_layers(layers, norm_pre, norm_post, w_in, b_igate, b_fgate, head_norm, conv_w, conv_norm, w_out)
        nc = _get_prog(len(layers), NTILES, TPS)
        in_maps = [dict(prm, x=xs[c]) for c in range(NCORES)]
        res = run_bass_kernel_spmd(nc, in_maps, core_ids=list(range(NCORES)))
        xs = [np.asarray(res.results[c]["y"], dtype=np.float32) for c in range(NCORES)]
    out = np.stack([xc.reshape(per, Sq, D) for xc in xs], axis=0).reshape(Bt, Sq, D)
    return out
```
